# Optimizing a Trainium2 kernel written in Bass

```python
import jax, jax.numpy as jnp
from jax import lax
import numpy as np

D_MODEL = 1024
BATCH = 8
SEQ = 2048
DEPTH = 1
DEC_BATCH = 32
DEC_SEQ = 1
PAST_LEN = 8192
PAGE_SIZE = 128

MIX_WIDTH = D_MODEL
ATT_WIDTH = MIX_WIDTH // 2
CONV_CH = MIX_WIDTH - ATT_WIDTH
HEAD_DIM = 64
N_HEADS = ATT_WIDTH // HEAD_DIM
ROPE_DIM = HEAD_DIM // 4
ROPE_THETA = 500000.0
MOBA_BLOCK = 256
MOBA_TOPK = 3
Q_CHUNK = 32
ATT_SCALE = HEAD_DIM ** -0.5
CONV_K = 3
PROJ_WIDTH = 3 * ATT_WIDTH + 3 * CONV_CH
SPLIT_POINTS = (ATT_WIDTH, 2 * ATT_WIDTH, 3 * ATT_WIDTH,
                3 * ATT_WIDTH + CONV_CH, 3 * ATT_WIDTH + 2 * CONV_CH)
N_EXPERT_GROUPS = 4
EXPERTS_PER_GROUP = 8
N_EXPERTS = N_EXPERT_GROUPS * EXPERTS_PER_GROUP
EXPERT_TOPK = 2
EXPERT_FF = D_MODEL // 4
RMS_EPS = 1e-6

kernel_name = "hymba_moba_shortconv_hiermoe_step"


def rmsnorm(x, g):
    xf = x.astype(jnp.float32)
    y = xf * lax.rsqrt(jnp.mean(xf * xf, axis=-1, keepdims=True) + RMS_EPS)
    return (y * g.astype(jnp.float32)).astype(x.dtype)


def apply_partial_rope(x, pos):
    half = ROPE_DIM // 2
    inv = ROPE_THETA ** (-jnp.arange(0, ROPE_DIM, 2, dtype=jnp.float32) / ROPE_DIM)
    ang = pos.astype(jnp.float32)[:, None] * inv[None, :]
    cos = jnp.cos(ang)[None, :, None, :]
    sin = jnp.sin(ang)[None, :, None, :]
    xf = x.astype(jnp.float32)
    x1 = xf[..., :half]
    x2 = xf[..., half:ROPE_DIM]
    out = jnp.concatenate([x1 * cos - x2 * sin, x2 * cos + x1 * sin, xf[..., ROPE_DIM:]], axis=-1)
    return out.astype(x.dtype)


def moba_attend(q, k, v, qpos):
    B, H, L, dh = k.shape
    Q = q.shape[2]
    nb = -(-L // MOBA_BLOCK)
    pad = nb * MOBA_BLOCK - L
    k = jnp.pad(k, ((0, 0), (0, 0), (0, pad), (0, 0)))
    v = jnp.pad(v, ((0, 0), (0, 0), (0, pad), (0, 0)))
    kb = k.reshape(B, H, nb, MOBA_BLOCK, dh)
    vb = v.reshape(B, H, nb, MOBA_BLOCK, dh)
    means = jnp.mean(kb.astype(jnp.float32), axis=3)
    n_sel = min(MOBA_TOPK, nb)
    bi = jnp.arange(B)[:, None, None, None]
    hi = jnp.arange(H)[None, :, None, None]
    blk_ids = jnp.arange(nb)
    offs = jnp.arange(MOBA_BLOCK)

    def attend_chunk(q_c, pos_c):
        C = q_c.shape[2]
        own = pos_c // MOBA_BLOCK
        gate = jnp.einsum('bhcd,bhnd->bhcn', q_c.astype(jnp.float32), means)
        fully_past = blk_ids[None, :] < own[:, None]
        gate = jnp.where(fully_past[None, None], gate, -jnp.inf)
        _, sel = lax.top_k(gate, n_sel)
        own_b = jnp.broadcast_to(own[None, None, :, None], (B, H, C, 1)).astype(sel.dtype)
        sel_ok = sel < own_b
        idx = jnp.concatenate([sel, own_b], axis=-1)
        slot_ok = jnp.concatenate([sel_ok, jnp.ones_like(sel_ok[..., :1])], axis=-1)
        kg = kb[bi, hi, idx]
        vg = vb[bi, hi, idx]
        kpos = idx[..., None] * MOBA_BLOCK + offs
        mask = slot_ok[..., None] & (kpos <= pos_c[None, None, :, None, None])
        s = jnp.einsum('bhcd,bhcnkd->bhcnk', q_c, kg).astype(jnp.float32) * ATT_SCALE
        s = jnp.where(mask, s, -jnp.inf)
        n_slot = idx.shape[-1]
        p = jax.nn.softmax(s.reshape(B, H, C, n_slot * MOBA_BLOCK), axis=-1)
        p = p.reshape(B, H, C, n_slot, MOBA_BLOCK).astype(vg.dtype)
        return jnp.einsum('bhcnk,bhcnkd->bhcd', p, vg)

    if Q > Q_CHUNK and Q % Q_CHUNK == 0:
        nc = Q // Q_CHUNK
        qc = jnp.moveaxis(q.reshape(B, H, nc, Q_CHUNK, dh), 2, 0)
        pc = qpos.reshape(nc, Q_CHUNK)
        out = lax.map(lambda a: attend_chunk(a[0], a[1]), (qc, pc))
        return jnp.moveaxis(out, 0, 2).reshape(B, H, Q, dh)
    return attend_chunk(q, qpos)


def short_conv(u, prev, w):
    T = u.shape[1]
    up = jnp.concatenate([prev, u], axis=1)
    out = w[0] * up[:, 0:T]
    for i in range(1, CONV_K):
        out = out + w[i] * up[:, i:i + T]
    return out, up[:, -(CONV_K - 1):]


def hier_moe(x, w_rg, b_rg, w_re, b_re, w_gate, w_up, w_down):
    T = x.shape[0]
    lg = (x @ w_rg).astype(jnp.float32) + b_rg.astype(jnp.float32)
    pg_all = jax.nn.softmax(lg, axis=-1)
    g = jnp.argmax(lg, axis=-1)
    pg = jnp.take_along_axis(pg_all, g[:, None], axis=1)
    le = ((x @ w_re).astype(jnp.float32) + b_re.astype(jnp.float32)).reshape(
        T, N_EXPERT_GROUPS, EXPERTS_PER_GROUP)
    le_g = jnp.take_along_axis(le, g[:, None, None], axis=1)[:, 0]
    top_l, top_i = lax.top_k(le_g, EXPERT_TOPK)
    w_top = jax.nn.softmax(top_l, axis=-1) * pg
    eid = g[:, None] * EXPERTS_PER_GROUP + top_i
    gates = jnp.sum(jax.nn.one_hot(eid, N_EXPERTS, dtype=jnp.float32) * w_top[..., None], axis=1)
    hg = jnp.einsum('td,edf->tef', x, w_gate)
    hu = jnp.einsum('td,edf->tef', x, w_up)
    h = jax.nn.silu(hg) * hu * gates[:, :, None].astype(x.dtype)
    return jnp.einsum('tef,efd->td', h, w_down)


def decoder_layer(x, pos, k_past, v_past, conv_prev, g_mix, w_in, conv_w, w_out, g_ffn,
                  w_rg, b_rg, w_re, b_re, w_gate, w_up, w_down):
    B, T, D = x.shape
    xn = rmsnorm(x, g_mix)
    proj = xn @ w_in
    q, k, v, b_gate, c_gate, h_in = jnp.split(proj, SPLIT_POINTS, axis=-1)
    q = apply_partial_rope(q.reshape(B, T, N_HEADS, HEAD_DIM), pos)
    k = apply_partial_rope(k.reshape(B, T, N_HEADS, HEAD_DIM), pos)
    v = v.reshape(B, T, N_HEADS, HEAD_DIM)
    if k_past is None:
        k_all, v_all = k, v
    else:
        k_all = jnp.concatenate([k_past, k], axis=1)
        v_all = jnp.concatenate([v_past, v], axis=1)
    att = moba_attend(q.transpose(0, 2, 1, 3), k_all.transpose(0, 2, 1, 3),
                      v_all.transpose(0, 2, 1, 3), pos)
    att = att.transpose(0, 2, 1, 3).reshape(B, T, ATT_WIDTH)
    conv_out, conv_new = short_conv(c_gate * h_in, conv_prev, conv_w)
    conv_y = b_gate * conv_out
    hres = x + jnp.concatenate([att, conv_y], axis=-1) @ w_out
    hn = rmsnorm(hres, g_ffn)
    ffn = hier_moe(hn.reshape(B * T, D), w_rg, b_rg, w_re, b_re, w_gate, w_up, w_down)
    return hres + ffn.reshape(B, T, D), k, v, conv_new


def setup_inputs(seed: int = 0) -> dict:
    key = jax.random.key(seed)
    ks = jax.random.split(key, 20)
    f32 = jnp.float32
    n_pages = PAST_LEN // PAGE_SIZE
    n_used = DEC_BATCH * n_pages
    n_phys = n_used + max(1, n_used // 4)
    page_table = jax.random.permutation(ks[0], n_phys)[:n_used].reshape(DEC_BATCH, n_pages).astype(jnp.int32)
    nrm = lambda k, shape, s: jax.random.normal(k, shape, f32) * s
    return {
        "x_prompt": nrm(ks[1], (BATCH, SEQ, D_MODEL), 1.0),
        "x_sample": nrm(ks[2], (DEC_BATCH, DEC_SEQ, D_MODEL), 1.0),
        "cache_k": nrm(ks[3], (DEPTH, n_phys, PAGE_SIZE, N_HEADS, HEAD_DIM), 1.0),
        "cache_v": nrm(ks[4], (DEPTH, n_phys, PAGE_SIZE, N_HEADS, HEAD_DIM), 1.0),
        "state_conv": nrm(ks[5], (DEPTH, DEC_BATCH, CONV_K - 1, CONV_CH), 1.0),
        "page_table": page_table,
        "g_mix": 1.0 + nrm(ks[6], (DEPTH, D_MODEL), 0.02),
        "w_in": nrm(ks[7], (DEPTH, D_MODEL, PROJ_WIDTH), D_MODEL ** -0.5),
        "conv_w": nrm(ks[8], (DEPTH, CONV_K, CONV_CH), CONV_K ** -0.5),
        "w_out": nrm(ks[9], (DEPTH, MIX_WIDTH, D_MODEL), MIX_WIDTH ** -0.5),
        "g_ffn": 1.0 + nrm(ks[10], (DEPTH, D_MODEL), 0.02),
        "w_router_group": nrm(ks[11], (DEPTH, D_MODEL, N_EXPERT_GROUPS), D_MODEL ** -0.5),
        "b_router_group": nrm(ks[12], (DEPTH, N_EXPERT_GROUPS), 0.01),
        "w_router_expert": nrm(ks[13], (DEPTH, D_MODEL, N_EXPERTS), D_MODEL ** -0.5),
        "b_router_expert": nrm(ks[14], (DEPTH, N_EXPERTS), 0.01),
        "w_gate": nrm(ks[15], (DEPTH, N_EXPERTS, D_MODEL, EXPERT_FF), D_MODEL ** -0.5),
        "w_up": nrm(ks[16], (DEPTH, N_EXPERTS, D_MODEL, EXPERT_FF), D_MODEL ** -0.5),
        "w_down": nrm(ks[17], (DEPTH, N_EXPERTS, EXPERT_FF, D_MODEL), EXPERT_FF ** -0.5),
        "g_final": 1.0 + nrm(ks[18], (D_MODEL,), 0.02),
    }


def reference(x_prompt, x_sample, cache_k, cache_v, state_conv, page_table, g_mix, w_in, conv_w,
              w_out, g_ffn, w_router_group, b_router_group, w_router_expert, b_router_expert,
              w_gate, w_up, w_down, g_final):
    n_dec, dec_len = x_sample.shape[0], x_sample.shape[1]
    past_len = page_table.shape[1] * cache_k.shape[2]
    pos_p = jnp.arange(x_prompt.shape[1], dtype=jnp.int32)
    pos_s = past_len + jnp.arange(dec_len, dtype=jnp.int32)
    y_p, y_s = x_prompt, x_sample
    kp_l, vp_l, cp_l, ksl, vsl, csl = [], [], [], [], [], []
    for l in range(DEPTH):
        lw = (g_mix[l], w_in[l], conv_w[l], w_out[l], g_ffn[l], w_router_group[l],
              b_router_group[l], w_router_expert[l], b_router_expert[l], w_gate[l], w_up[l], w_down[l])
        conv0 = jnp.zeros((y_p.shape[0], CONV_K - 1, CONV_CH), y_p.dtype)
        y_p, kp, vp, cp = decoder_layer(y_p, pos_p, None, None, conv0, *lw)
        k_past = cache_k[l][page_table].reshape(n_dec, past_len, N_HEADS, HEAD_DIM)
        v_past = cache_v[l][page_table].reshape(n_dec, past_len, N_HEADS, HEAD_DIM)
        y_s, ks_, vs_, cs_ = decoder_layer(y_s, pos_s, k_past, v_past, state_conv[l], *lw)
        kp_l.append(kp); vp_l.append(vp); cp_l.append(cp)
        ksl.append(ks_); vsl.append(vs_); csl.append(cs_)
    y_prompt = rmsnorm(y_p, g_final)
    y_sample = rmsnorm(y_s, g_final)
    return (y_prompt, y_sample, jnp.stack(kp_l), jnp.stack(vp_l), jnp.stack(cp_l),
            jnp.stack(ksl), jnp.stack(vsl), jnp.stack(csl))
```

```python
import contextlib
import numpy as np
import ml_dtypes
import concourse.bass as bass
import concourse.mybir as mybir
from concourse.bass_utils import run_bass_kernel_spmd

F32 = mybir.dt.float32
BF16 = mybir.dt.bfloat16
I32 = mybir.dt.int32
U8 = mybir.dt.uint8
AF = mybir.ActivationFunctionType
ALU = mybir.AluOpType
AX = mybir.AxisListType

ENGS = ("sync", "scalar", "vector", "gpsimd", "tensor")
NCORES = 8
NT = 17
NE = 32
BIG = 30000.0
KiB = 1024


class Buf:
    __slots__ = ("name", "last_w", "readers")

    def __init__(self, name):
        self.name = name
        self.last_w = None
        self.readers = []


class Ins:
    __slots__ = ("eng", "fn", "deps", "signal", "sigval", "is_dma", "dsem", "dval")

    def __init__(self, eng, fn, is_dma):
        self.eng = eng
        self.fn = fn
        self.deps = []
        self.signal = False
        self.sigval = None
        self.is_dma = is_dma
        self.dsem = None
        self.dval = None


class Prog:
    def __init__(self, nc, ring=8):
        self.nc = nc
        self.q = {e: [] for e in ENGS}
        self.ringd = {e: ring for e in ENGS}
        self.ringd["gpsimd"] = 16
        self.dma_count = {e: 0 for e in ENGS}
        self.dma_hist = {e: [] for e in ENGS}

    def _add(self, ins, r, w):
        deps = []
        for b in r:
            if b.last_w is not None:
                deps.append(b.last_w)
        for b in w:
            if b.last_w is not None:
                deps.append(b.last_w)
            deps.extend(b.readers)
        for b in w:
            b.last_w = ins
            b.readers = []
        for b in r:
            if b.last_w is not ins:
                b.readers.append(ins)
        seen = set()
        for d in deps:
            if d is ins or id(d) in seen:
                continue
            seen.add(id(d))
            if (not d.is_dma) and d.eng == ins.eng and d.eng == "tensor" and not ins.is_dma:
                continue
            ins.deps.append(d)
            if not d.is_dma:
                d.signal = True
        self.q[ins.eng].append(ins)
        return ins

    def op(self, eng, fn, r=(), w=()):
        return self._add(Ins(eng, fn, False), list(r), list(w))

    def dma(self, eng, fn, r=(), w=()):
        ins = Ins(eng, fn, True)
        k = self.dma_count[eng]
        self.dma_count[eng] += 1
        hist = self.dma_hist[eng]
        ring = self.ringd[eng]
        if k >= ring:
            ins.deps.append(hist[k - ring])
        hist.append(ins)
        ins.dsem = (eng, k % ring)
        ins.dval = 16 * (k // ring + 1)
        return self._add(ins, list(r), list(w))

    def alias(self, new, olds):
        for o in olds:
            if o.last_w is not None:
                new.readers.append(o.last_w)
            new.readers.extend(o.readers)

    def run(self):
        nc = self.nc
        with contextlib.ExitStack() as st:
            esem = {e: st.enter_context(nc.semaphore("es_" + e)) for e in ENGS}
            dsem = {}
            for e in ENGS:
                for i in range(min(self.ringd[e], self.dma_count[e])):
                    dsem[(e, i)] = st.enter_context(nc.semaphore("ds_%s%d" % (e, i)))
            for e in ENGS:
                c = 0
                for ins in self.q[e]:
                    if (not ins.is_dma) and ins.signal:
                        c += 1
                        ins.sigval = c
            allsems = list(esem.values()) + list(dsem.values())
            for s_ in allsems:
                nc.gpsimd.sem_clear(s_)
            nc.all_engine_barrier()
            block = nc.Block()
            block.__enter__()

            def make(ename):
                def body(eng):
                    known = {}
                    for ins in self.q[ename]:
                        need = {}
                        for d in ins.deps:
                            if d.is_dma:
                                s, v = dsem[d.dsem], d.dval
                            else:
                                s, v = esem[d.eng], d.sigval
                            key = id(s)
                            if known.get(key, 0) >= v:
                                continue
                            if key not in need or need[key][1] < v:
                                need[key] = (s, v)
                        for key, (s, v) in need.items():
                            eng.wait_ge(s, v)
                            known[key] = v
                        r = ins.fn(eng)
                        if ins.is_dma:
                            r.then_inc(dsem[ins.dsem], 16)
                        elif ins.signal:
                            r.then_inc(esem[ename], 1)
                    for ins in self.dma_hist[ename][-self.ringd[ename]:]:
                        s, v = dsem[ins.dsem], ins.dval
                        if known.get(id(s), 0) < v:
                            eng.wait_ge(s, v)
                            known[id(s)] = v
                return body

            for e in ENGS:
                if self.q[e]:
                    getattr(block, e)(make(e))
            block.__exit__(None, None, None)
            st.pop_all()
            nc.all_engine_barrier()
            for s_ in allsems:
                nc.gpsimd.sem_clear(s_)
            nc.all_engine_barrier()


NFC = 1068


def host_consts():
    c = {}
    c["c_identb"] = np.eye(128, dtype=np.float32).astype(ml_dtypes.bfloat16)
    c["c_identf"] = np.eye(128, dtype=np.float32)
    kk = np.arange(128)[:, None]
    qq = np.arange(128)[None, :]
    c["c_tri"] = (qq >= kk).astype(np.float32).astype(ml_dtypes.bfloat16)
    inv = (500000.0 ** (-np.arange(0, 16, 2, dtype=np.float32) / np.float32(16))).astype(np.float32)
    cs = np.zeros((NT, 128, 128), np.float32)
    for t in range(NT):
        pos = (t * 128 + np.arange(128)) if t < 16 else np.full(128, 8192)
        ang = pos.astype(np.float32)[:, None] * inv[None, :]
        ang = ang.astype(np.float32).astype(np.float64)
        cs[t, :, 0:64] = np.tile(np.cos(ang), (1, 8))
        cs[t, :, 64:128] = np.tile(np.sin(ang), (1, 8))
    c["c_cs"] = cs
    g = np.zeros((16, 128, 192), np.float32)
    for t in range(16):
        own = (t * 128 + np.arange(128)) // 256
        j = np.arange(8)[None, :]
        past = (j < own[:, None]).astype(np.float32)
        ownm = (j == own[:, None]).astype(np.float32)
        g[t, :, 0:64] = np.tile((past - 1.0) * 1e30, (1, 8))
        g[t, :, 64:128] = np.tile(past, (1, 8))
        g[t, :, 128:192] = np.tile(ownm, (1, 8))
    c["c_g"] = g
    blk = np.zeros((8, 2048), np.float32)
    for j in range(8):
        blk[j, j * 256:(j + 1) * 256] = 1.0
    c["c_blk"] = blk.astype(ml_dtypes.bfloat16)
    f = np.zeros((128, NFC), np.float32)
    p = np.arange(128)
    f[p, p // 2] = 1.0
    for h in range(8):
        for b in range(4):
            for r in range(3):
                for hf in range(2):
                    col = 64 + ((h * 4 + b) * 3 + r) * 2 + hf
                    f[:, col] = p * 8 + h
    f[:, 256:288] = np.arange(32)[None, :]
    for pr in range(2):
        for m in range(64):
            f[2 * pr + m // 32, 288 + pr * 64 + m] = 1.0
    for b in range(4):
        f[b, 416 + b * 128: 416 + (b + 1) * 128] = 1.0
    f[0:8, 928:936] = np.eye(8)
    f[0:4, 936:940] = np.eye(4)
    f[:, 940:1068] = 1.0
    c["c_f"] = f
    return c


def build_program(debug=False):
    nc = bass.Bass("TRN2", target_bir_lowering=False)
    P = Prog(nc)

    def din(name, shape, dt=F32):
        return nc.dram_tensor(name, list(shape), dt, kind="ExternalInput").ap()

    def dout(name, shape, dt=F32):
        return nc.dram_tensor(name, list(shape), dt, kind="ExternalOutput").ap()

    xp = din("xp", [2048, 1024]); xs = din("xs", [4, 1024])
    ck = din("ck", [2621440, 64]); cv = din("cv", [2621440, 64])
    sc = din("sc", [8, 512]); pt = din("pt", [4, 64], I32)
    g_mix = din("g_mix", [1024]); w_in = din("w_in", [1024, 3072]); conv_w = din("conv_w", [3, 512])
    w_out = din("w_out", [1024, 1024]); g_ffn = din("g_ffn", [1024])
    w_rg = din("w_rg", [1024, 4]); b_rg = din("b_rg", [4]); w_re = din("w_re", [1024, 32]); b_re = din("b_re", [32])
    w_gate = din("w_gate", [32, 1024, 256]); w_up = din("w_up", [32, 1024, 256]); w_down = din("w_down", [32, 256, 1024])
    g_final = din("g_final", [1024])
    c_identb = din("c_identb", [128, 128], BF16); c_identf = din("c_identf", [128, 128])
    c_tri = din("c_tri", [128, 128], BF16); c_cs = din("c_cs", [NT, 128, 128]); c_g = din("c_g", [16, 128, 192])
    c_blk = din("c_blk", [8, 2048], BF16); c_f = din("c_f", [128, NFC])
    yp = dout("yp", [2048, 1024]); ys = dout("ys", [4, 1024])
    kp = dout("kp", [2048, 512]); vp = dout("vp", [2048, 512]); cp = dout("cp", [2, 512])
    ks = dout("ks", [4, 512]); vs = dout("vs", [4, 512]); cso = dout("cso", [8, 512])
    ckc = ck.rearrange("(a b) d -> a (b d)", b=32)

    st = contextlib.ExitStack()
    with st:
        ARENA = 190 * KiB
        arena = st.enter_context(nc.sbuf_tensor("arena", [128, ARENA], U8))

        def AV(off, shape, dt, p0=0):
            isz = 2 if dt == BF16 else 4
            n = 1
            for s in shape[1:]:
                n *= s
            nb = n * isz
            assert off + nb <= ARENA, (off, nb)
            ap = arena[p0:p0 + shape[0], off:off + nb].bitcast(dt)
            if len(shape) == 3:
                ap = ap.rearrange("p (a b) -> p a b", a=shape[1])
            elif len(shape) == 4:
                ap = ap.rearrange("p (a b c) -> p a b c", a=shape[1], b=shape[2])
            return ap

        def ST(name, shape, dt):
            return st.enter_context(nc.sbuf_tensor(name, list(shape), dt))

        PS = [st.enter_context(nc.psum_tensor("ps%d" % i, [128, 1024], F32)) for i in range(4)]

        def bank(i):
            return PS[i // 2][:, (i % 2) * 512:(i % 2 + 1) * 512]

        bkB = [P_buf for P_buf in (Buf("bank%d" % i) for i in range(8))]

        identb = ST("identb", [128, 128], BF16); identf = ST("identf", [128, 128], F32); tri = ST("tri", [128, 128], BF16)
        cf = ST("cf", [128, NFC], F32)
        gbc = ST("gbc", [128, 1024], F32)
        ssq = ST("ssq", [128, 3 * NT], F32); rt = ST("rt", [128, 3 * NT], F32); rstd = ST("rstd", [128, 3 * NT], F32)
        convw = ST("convw", [128, 12], F32)
        ksum = ST("ksum", [64, 64], F32); ksumhi = ST("ksumhi", [64, 64], BF16); ksumlo = ST("ksumlo", [64, 64], BF16)
        ksumr = ST("ksumr", [64, 64], F32)
        epsb = ST("epsb", [128, 1], F32)
        rb = ST("rb", [128, 36], F32)
        wr = ST("wr", [128, 8, 36], F32)
        B_ = {n: Buf(n) for n in ("identb identf tri cf gbc convw ksum ksumhi ksumlo ksumr epsb rb wr").split()}
        B_ssq = [Buf("ssq%d" % i) for i in range(3 * NT)]

        def dma(q, out, in_, r=(), w=()):
            return P.dma(q, lambda e: e.dma_start(out=out, in_=in_), r, w)

        def act(out, in_, func, r, w, **kw):
            return P.op("scalar", lambda e: e.activation(out=out, in_=in_, func=func, **kw), r, w)

        def mm(out, lhsT, rhs, start, stop, r, w):
            return P.op("tensor", lambda e: e.matmul(out, lhsT=lhsT, rhs=rhs, start=start, stop=stop), r, w)

        def tr(out, in_, ident, r, w):
            return P.op("tensor", lambda e: e.transpose(out=out, in_=in_, identity=ident), r, w)

        def V(name, r, w, eng="vector", **kw):
            return P.op(eng, lambda e: getattr(e, name)(**kw), r, w)

        dma("sync", identb[:], c_identb, w=[B_["identb"]])
        dma("sync", identf[:], c_identf, w=[B_["identf"]])
        dma("sync", tri[:], c_tri, w=[B_["tri"]])
        dma("sync", cf[:], c_f, w=[B_["cf"]])
        dma("sync", gbc[:], g_mix.partition_broadcast(128), w=[B_["gbc"]])
        V("memset", [], [B_["epsb"]], ap=epsb[:], constant=1e-6)
        PairM = cf[:, 0:64]; poshc = cf[:, 64:256]; chunkid = cf[:, 256:288]
        ones_f = cf[:, 940:1068]

        cw12 = ST("cw12", [12, 128], F32); Bcw12 = Buf("cw12")
        dma("sync", cw12[:], conv_w.rearrange("k (t p) -> (k t) p", p=128), w=[Bcw12])
        tr(bank(0)[:, 0:12], cw12[:], identf[0:12, 0:12], [Bcw12, B_["identf"]], [bkB[0]])
        act(convw[:], bank(0)[:, 0:12], AF.Copy, [bkB[0]], [B_["convw"]])
        dma("sync", wr[:, :, 0:4], w_rg.rearrange("(c p) n -> p c n", p=128), w=[B_["wr"]])
        dma("sync", wr[:, :, 4:36], w_re.rearrange("(c p) n -> p c n", p=128), w=[B_["wr"]])
        dma("sync", rb[:, 0:4], b_rg.partition_broadcast(128), w=[B_["rb"]])
        dma("sync", rb[:, 4:36], b_re.partition_broadcast(128), w=[B_["rb"]])

        WinB = AV(0, [128, 8, 3072], BF16); B_win = [Buf("win%d" % c) for c in range(8)]
        W0 = 146 * KiB
        stg = [AV(W0 + i * 12 * KiB, [128, 3072], F32) for i in range(2)]
        B_stg = [Buf("stg%d" % i) for i in range(2)]
        for c in range(8):
            s = c % 2
            dma("sync", stg[s], w_in[c * 128:(c + 1) * 128, :], w=[B_stg[s]])
            V("tensor_copy", [B_stg[s]], [B_win[c]], eng=("vector" if c % 2 == 0 else "gpsimd"), out=WinB[:, c, :], in_=stg[s])

        QT = AV(48 * KiB, [72, 8, 2048], BF16); KT = AV(80 * KiB, [72, 8, 2048], BF16)
        Vb = AV(112 * KiB, [128, 16, 8, 65], BF16)
        convT = AV(129 * KiB, [128, NT, 4, 128], BF16)
        B_qt = [Buf("qt%d" % t) for t in range(16)]; B_kt = [Buf("kt%d" % t) for t in range(16)]
        B_vb = [Buf("vb%d" % t) for t in range(16)]; B_ct = [Buf("convT%d" % t) for t in range(NT)]
        B_qtb = [Buf("qtb%d" % t) for t in range(16)]
        V("memset", [], B_vb, eng="gpsimd", ap=Vb[:, :, :, 64:65], constant=1.0)
        for h in range(8):
            dma("sync", KT[64:72, h, :], c_blk, w=B_kt)
        V("memset", [], [B_ct[16]], eng="gpsimd", ap=convT[:, 16, :, :], constant=0.0)

        o = W0
        qf = AV(o, [128, 512], F32); o += 2 * KiB
        kf = [AV(o + i * 2 * KiB, [128, 512], F32) for i in range(2)]; o += 4 * KiB
        vf = [AV(o, [128, 512], F32)] * 2; o += 2 * KiB
        xt = [AV(o + i * 4 * KiB, [128, 1024], F32) for i in range(2)]; o += 8 * KiB
        xnb = AV(o, [128, 1024], BF16); o += 2 * KiB
        xnTg = AV(o, [128, 8, 512], BF16); o += 8 * KiB
        qb = AV(o, [128, 512], BF16); o += 1 * KiB
        kb = AV(o, [128, 512], BF16); o += 1 * KiB
        rtq = [AV(o + i * 256, [128, 8, 8], F32) for i in range(4)]; o += 1 * KiB
        rtk = [AV(o + i * 256, [128, 8, 8], F32) for i in range(4)]; o += 1 * KiB
        Hs = AV(o, [128, 512], F32); o += 2 * KiB
        ctmp = AV(o, [128, 512], F32); o += 2 * KiB
        ug = AV(o, [128, 4, 514], F32); o += 8224 + 32
        cst = [AV(o + i * 512, [128, 128], F32) for i in range(2)]; o += 1 * KiB
        assert o <= ARENA, o
        Bw = {n: Buf(n) for n in "xnb xnTg qf qb kb rtq rtk Hs ctmp ug".split()}
        B_xt = [Buf("xt0"), Buf("xt1")]; B_kf = [Buf("kf0"), Buf("kf1")]; B_vf = [Buf("vf0")] * 2
        B_cst = [Buf("cst0"), Buf("cst1")]
        for b_ in list(Bw.values()) + B_xt + B_kf + B_vf[:1] + B_cst:
            P.alias(b_, B_stg)
        V("memset", [], [Bw["ug"]], eng="gpsimd", ap=ug[:, :, 0:2], constant=0.0)

        def load_x(t):
            s = t % 2
            if t < 16:
                dma("sync", xt[s], xp[t * 128:(t + 1) * 128, :], w=[B_xt[s]])
            else:
                V("memset", [], [B_xt[s]], eng="gpsimd", ap=xt[s], constant=0.0)
                dma("sync", xt[s][0:4, :], xs, w=[B_xt[s]])
            dma("sync", cst[s], c_cs[t], w=[B_cst[s]])

        def rmsnorm_stats(src, col, rbufs):
            act(junk, src, AF.Square, rbufs, B_junkl + [B_ssq[col]], accum_out=ssq[:, col:col + 1])
            act(rt[:, col:col + 1], ssq[:, col:col + 1], AF.Sqrt, [B_ssq[col], B_["epsb"]], [B_ssq[col]],
                scale=1.0 / 1024.0, bias=epsb[:, 0:1])
            V("reciprocal", [B_ssq[col]], [B_ssq[col]], out=rstd[:, col:col + 1], in_=rt[:, col:col + 1])

        junk = AV(W0 + 30 * KiB, [128, 1024], F32); B_junkl = [Bw["Hs"], Bw["ctmp"]]
        us = ST("us", [128, 4, 4], F32); B_us = Buf("us")
        cs4 = ST("cs4", [128, 4, 4], F32); B_cs4 = Buf("cs4")
        prevT = ST("prevT", [128, 4, 4, 2], F32); B_prevT = Buf("prevT")
        csT = ST("csT", [128, 4, 4, 2], F32); B_csT = Buf("csT")
        misc8 = ST("misc8", [8, 512], F32); B_misc8 = Buf("misc8")
        cpo = misc8[0:2, :]; sc8 = misc8; cso8 = misc8
        B_cpo = B_sc8 = B_cso8 = B_misc8

        def rope(tf, tmps, cs_t, Bt, Btmp, Bcs, eng):
            v = tf.rearrange("p (h d) -> p h d", h=8)
            x1 = v[:, :, 0:8]; x2 = v[:, :, 8:16]
            cosv = cs_t[:, 0:64].rearrange("p (h d) -> p h d", h=8); sinv = cs_t[:, 64:128].rearrange("p (h d) -> p h d", h=8)
            t1, t2, t3, t4 = tmps
            V("tensor_tensor", [Bt, Bcs], [Btmp], eng=eng, out=t1, in0=x1, in1=cosv, op=ALU.mult)
            V("tensor_tensor", [Bt, Bcs], [Btmp], eng=eng, out=t2, in0=x2, in1=sinv, op=ALU.mult)
            V("tensor_tensor", [Bt, Bcs], [Btmp], eng=eng, out=t3, in0=x2, in1=cosv, op=ALU.mult)
            V("tensor_tensor", [Bt, Bcs], [Btmp], eng=eng, out=t4, in0=x1, in1=sinv, op=ALU.mult)
            V("tensor_tensor", [Btmp], [Bt], eng=eng, out=x1, in0=t1, in1=t2, op=ALU.subtract)
            V("tensor_tensor", [Btmp], [Bt], eng=eng, out=x2, in0=t3, in1=t4, op=ALU.add)

        pTx = bank(0).bitcast(BF16).rearrange("p (a b) -> p a b", a=8)
        pTqk = bank(4).bitcast(BF16).rearrange("p (a b) -> p a b", a=8)

        load_x(0)
        groups = [[0, 1, 2, 3], [4, 5, 6, 7], [8, 9, 10, 11], [12, 13, 14, 15], [16]]
        for g, tiles in enumerate(groups):
            ncol = 128 * len(tiles)
            for tt, t in enumerate(tiles):
                s = t % 2
                if t + 1 < NT:
                    load_x(t + 1)
                rmsnorm_stats(xt[s], t, [B_xt[s]])
                V("scalar_tensor_tensor", [B_xt[s], B_ssq[t], B_["gbc"]], [Bw["xnb"]], out=xnb, in0=xt[s],
                  scalar=rstd[:, t:t + 1], in1=gbc[:], op0=ALU.mult, op1=ALU.mult)
                for c in range(8):
                    tr(pTx[:, c, :], xnb[:, c * 128:(c + 1) * 128], identb[:], [Bw["xnb"], B_["identb"]], [bkB[0]])
                act(xnTg[:, :, tt * 128:(tt + 1) * 128], pTx, AF.Copy, [bkB[0]], [Bw["xnTg"]])
                for j in range(3):
                    for c in range(8):
                        mm(bank(1 + j), xnTg[:, c, tt * 128:(tt + 1) * 128], WinB[:, c, j * 512:(j + 1) * 512],
                           c == 0, c == 7, [Bw["xnTg"], B_win[c]], [bkB[1 + j]])
                tq, tk, tv = qf, kf[s], vf[s]
                Bq, Bk, Bv = Bw["qf"], B_kf[s], B_vf[s]
                act(tq, bank(1), AF.Copy, [bkB[1]], [Bq], scale=0.125)
                act(tk, bank(2), AF.Copy, [bkB[2]], [Bk])
                act(tv, bank(3), AF.Copy, [bkB[3]], [Bv])
                rope(tq, rtq, cst[s], Bq, Bw["rtq"], B_cst[s], "vector")
                rope(tk, rtk, cst[s], Bk, Bw["rtk"], B_cst[s], "gpsimd")
                if t < 16:
                    dma("sync", kp[t * 128:(t + 1) * 128, :], tk, r=[Bk])
                    dma("sync", vp[t * 128:(t + 1) * 128, :], tv, r=[Bv])
                    V("tensor_copy", [Bv], [B_vb[t]], eng="gpsimd", out=Vb[:, t, :, 0:64],
                      in_=tv.rearrange("p (h d) -> p h d", h=8))
                    V("tensor_copy", [Bq], [Bw["qb"]], out=qb, in_=tq)
                    V("tensor_copy", [Bk], [Bw["kb"]], eng="gpsimd", out=kb, in_=tk)
                    for h in range(8):
                        tr(pTqk[0:64, h, :], qb[:, h * 64:(h + 1) * 64], identb[:], [Bw["qb"], B_["identb"]], [bkB[4]])
                    act(QT[0:64, :, t * 128:(t + 1) * 128], pTqk[0:64, :, :], AF.Copy, [bkB[4]], [B_qt[t]])
                    for h in range(8):
                        tr(pTqk[0:64, h, :], kb[:, h * 64:(h + 1) * 64], identb[:], [Bw["kb"], B_["identb"]], [bkB[4]])
                    act(KT[0:64, :, t * 128:(t + 1) * 128], pTqk[0:64, :, :], AF.Copy, [bkB[4]], [B_kt[t]])
                else:
                    dma("sync", ks, tk[0:4, :], r=[Bk])
                    dma("sync", vs, tv[0:4, :], r=[Bv])
            for ct in range(4):
                def colsW(j):
                    return slice(1536 + j * 512 + ct * 128, 1536 + j * 512 + (ct + 1) * 128)
                for c in range(8):
                    mm(bank(5)[:, 0:ncol], WinB[:, c, colsW(2)], xnTg[:, c, 0:ncol], c == 0, c == 7, [Bw["xnTg"], B_win[c]], [bkB[5]])
                for c in range(8):
                    mm(bank(6)[:, 0:ncol], WinB[:, c, colsW(1)], xnTg[:, c, 0:ncol], c == 0, c == 7, [Bw["xnTg"], B_win[c]], [bkB[6]])
                act(Hs[:, 0:ncol], bank(5)[:, 0:ncol], AF.Copy, [bkB[5]], [Bw["Hs"]])
                for c in range(8):
                    mm(bank(5)[:, 0:ncol], WinB[:, c, colsW(0)], xnTg[:, c, 0:ncol], c == 0, c == 7, [Bw["xnTg"], B_win[c]], [bkB[5]])
                w0 = convw[:, 0 * 4 + ct:0 * 4 + ct + 1]; w1 = convw[:, 4 + ct:4 + ct + 1]; w2 = convw[:, 8 + ct:8 + ct + 1]
                if g < 4:
                    V("tensor_tensor", [bkB[6], Bw["Hs"]], [Bw["ug"]], out=ug[:, ct, 2:514], in0=bank(6), in1=Hs, op=ALU.mult)
                    V("tensor_scalar", [Bw["ug"], B_["convw"]], [Bw["ctmp"]], out=ctmp, in0=ug[:, ct, 2:514], scalar1=w2, scalar2=None, op0=ALU.mult)
                    V("scalar_tensor_tensor", [Bw["ug"], Bw["ctmp"], B_["convw"]], [Bw["ctmp"]], out=ctmp, in0=ug[:, ct, 1:513], scalar=w1, in1=ctmp, op0=ALU.mult, op1=ALU.add)
                    V("scalar_tensor_tensor", [Bw["ug"], Bw["ctmp"], B_["convw"]], [Bw["ctmp"]], out=ctmp, in0=ug[:, ct, 0:512], scalar=w0, in1=ctmp, op0=ALU.mult, op1=ALU.add)
                    V("tensor_tensor", [bkB[5], Bw["ctmp"]], [B_ct[t_] for t_ in tiles],
                      out=convT[:, tiles[0]:tiles[0] + 4, ct, :], in0=bank(5).rearrange("p (a b) -> p a b", a=4),
                      in1=ctmp.rearrange("p (a b) -> p a b", a=4), op=ALU.mult)
                    V("tensor_copy", [Bw["ug"]], [Bw["ug"]], out=ug[:, ct, 0:2], in_=ug[:, ct, 512:514])
                else:
                    V("tensor_tensor", [bkB[6], Bw["Hs"]], [B_us], out=us[:, ct, :], in0=bank(6)[:, 0:4], in1=Hs[:, 0:4], op=ALU.mult)
                    V("tensor_scalar", [B_us, B_["convw"]], [B_cs4], out=cs4[:, ct, :], in0=us[:, ct, :], scalar1=w2, scalar2=None, op0=ALU.mult)
                    V("scalar_tensor_tensor", [B_prevT, B_cs4, B_["convw"]], [B_cs4], out=cs4[:, ct, :], in0=prevT[:, ct, :, 1], scalar=w1, in1=cs4[:, ct, :], op0=ALU.mult, op1=ALU.add)
                    V("scalar_tensor_tensor", [B_prevT, B_cs4, B_["convw"]], [B_cs4], out=cs4[:, ct, :], in0=prevT[:, ct, :, 0], scalar=w0, in1=cs4[:, ct, :], op0=ALU.mult, op1=ALU.add)
                    V("tensor_tensor", [bkB[5], B_cs4], [B_ct[16]], out=convT[:, 16, ct, 0:4], in0=bank(5)[:, 0:4], in1=cs4[:, ct, :], op=ALU.mult)
            if g == 3:
                for ct in range(4):
                    tr(bank(0)[0:2, ct * 128:(ct + 1) * 128], ug[:, ct, 0:2], identf[:], [Bw["ug"], B_["identf"]], [bkB[0]])
                act(cpo, bank(0)[0:2, :], AF.Copy, [bkB[0]], [B_cpo])
                dma("sync", cp, cpo, r=[B_cpo])
                dma("sync", sc8[:], sc, w=[B_sc8])
                for ct in range(4):
                    tr(bank(0)[:, 512 - 32 + ct * 8: 512 - 32 + (ct + 1) * 8], sc8[:, ct * 128:(ct + 1) * 128], identf[0:8, 0:8], [B_sc8, B_["identf"]], [bkB[0]])
                act(prevT[:].rearrange("p a b c -> p (a b c)"), bank(0)[:, 480:512], AF.Copy, [bkB[0]], [B_prevT])
            if g == 4:
                V("tensor_copy", [B_prevT], [B_csT], out=csT[:, :, :, 0], in_=prevT[:, :, :, 1])
                V("tensor_copy", [B_us], [B_csT], out=csT[:, :, :, 1], in_=us[:, :, :])
                for ct in range(4):
                    tr(bank(0)[0:8, ct * 128:(ct + 1) * 128], csT[:, ct, :, :].rearrange("p a b -> p (a b)"), identf[:], [B_csT, B_["identf"]], [bkB[0]])
                act(cso8[:], bank(0)[0:8, :], AF.Copy, [bkB[0]], [B_cso8])
                dma("sync", cso, cso8[:], r=[B_cso8])

        all_p1 = list(Bw.values()) + B_xt + B_kf + B_vf[:1] + B_cst + B_win
        ATT = AV(0, [128, NT, 8, 128], BF16)
        B_att = [Buf("att%d" % t) for t in range(NT)]
        for b_ in B_att:
            P.alias(b_, B_win)
        Pb = [AV(34 * KiB + i * KiB, [128, 512], BF16) for i in range(4)]; B_P = [Buf("P%d" % i) for i in range(4)]
        osb = [AV(38 * KiB + i * 2 * KiB, [65, 512], F32) for i in range(2)]; B_osb = [Buf("osb%d" % i) for i in range(2)]
        biasp = [AV(42 * KiB + i * 1152, [128, 8, 72], BF16) for i in range(2)]; B_bp = [Buf("bp%d" % i) for i in range(2)]
        gct = [AV(45 * KiB + i * 768, [128, 192], F32) for i in range(2)]; B_gct = [Buf("gct%d" % i) for i in range(2)]
        gm = AV(46 * KiB + 512, [128, 64], F32); top8 = AV(46 * KiB + 768, [128, 8, 8], F32); sel = AV(47 * KiB, [128, 64], F32)
        B_gm = Buf("gm"); B_top8 = Buf("top8"); B_sel = Buf("sel")
        for b_ in B_P + B_osb + B_bp + B_gct + [B_gm, B_top8, B_sel]:
            P.alias(b_, B_win)
        V("memset", [], [B_att[16]], eng="gpsimd", ap=ATT[0:64, 16, :, :], constant=0.0)
        for i in range(2):
            V("memset", [], [B_bp[i]], eng="gpsimd", ap=biasp[i], constant=0.0)
        SAMPLE_BUFS = []

        def sbuf_(name, olds=None):
            b_ = Buf(name)
            P.alias(b_, (all_p1 if olds is None else olds) + SAMPLE_BUFS)
            SAMPLE_BUFS.append(b_)
            return b_
        SB = 154 * KiB
        G = [AV(SB + i * 8 * KiB, [128, 2048], F32) for i in range(2)]
        accs = [AV(SB + 16 * KiB + i * 8 * KiB, [128, 2048], F32) for i in range(2)]
        B_G = [sbuf_("G0"), sbuf_("G1")]; B_accs = [sbuf_("accs0"), sbuf_("accs1")]
        oA = [186 * KiB]

        def smallA(shape, dt):
            isz = 2 if dt == BF16 else 4
            n = 1
            for x_ in shape[1:]:
                n *= x_
            ap = AV(oA[0], shape, dt)
            oA[0] += (n * isz + 31) // 32 * 32
            assert oA[0] <= ARENA
            return ap
        ptI = smallA([128, 2], I32); ptF = smallA([128, 2], F32); idxf = smallA([128, 64], F32); idxI = smallA([128, 64], I32)
        ptb8 = smallA([8, 256], I32); PTf = smallA([8, 256], F32)
        B_s0 = sbuf_("s0")
        for pr in range(2):
            dma("sync", ptI[:, pr:pr + 1], pt[2 * pr:2 * pr + 2, :].rearrange("b (g o) -> (b g) o", o=1), w=[B_s0])
        dma("sync", ptb8, pt.rearrange("b g -> (b g)").partition_broadcast(8), w=[B_s0])
        V("tensor_copy", [B_s0], [B_s0], out=ptF, in_=ptI)
        V("tensor_copy", [B_s0], [B_s0], out=PTf, in_=ptb8)
        V("tensor_scalar", [B_s0], [B_s0], out=ptF, in0=ptF, scalar1=32.0, scalar2=None, op0=ALU.mult)
        for pr in range(2):
            V("tensor_scalar", [B_s0, B_["cf"]], [B_s0], out=idxf[:, pr * 32:(pr + 1) * 32], in0=chunkid, scalar1=ptF[:, pr:pr + 1], scalar2=None, op0=ALU.add)
        V("tensor_scalar", [B_s0], [B_s0], out=idxf, in0=idxf, scalar1=0.0, scalar2=81919.0, op0=ALU.max, op1=ALU.min)
        V("tensor_copy", [B_s0], [B_s0], out=idxI, in_=idxf)

        def s1_steps():
            chunks = [(pr, c) for pr in range(2) for c in range(32)]

            def gather(k_):
                pr, c = chunks[k_]
                gi_ = k_ % 2
                col = pr * 32 + c
                P.dma("gpsimd", lambda e, gi_=gi_, col=col: e.indirect_dma_start(
                    out=G[gi_], out_offset=None, in_=ckc[:, :], in_offset=bass.IndirectOffsetOnAxis(ap=idxI[:, col:col + 1], axis=0)),
                    [B_s0], [B_G[gi_]])

            def accum(k_):
                pr, c = chunks[k_]
                gi_ = k_ % 2
                if c == 0:
                    V("tensor_copy", [B_G[gi_]], [B_accs[pr]], out=accs[pr], in_=G[gi_])
                else:
                    V("tensor_tensor", [B_G[gi_], B_accs[pr]], [B_accs[pr]], out=accs[pr], in0=accs[pr], in1=G[gi_], op=ALU.add)
            gather(0)
            for k_ in range(len(chunks)):
                if k_ + 1 < len(chunks):
                    gather(k_ + 1)
                accum(k_)
                yield
        s1gen = s1_steps()

        def s1_advance(n):
            for _ in range(n):
                try:
                    next(s1gen)
                except StopIteration:
                    return
        for h in range(8):
            V("tensor_reduce", B_kt, [B_["ksum"]], out=ksum[:, h * 8:(h + 1) * 8],
              in_=KT[0:64, h, :].rearrange("p (b k) -> p b k", b=8), axis=AX.X, op=ALU.add)
        V("tensor_copy", [B_["ksum"]], [B_["ksumhi"]], out=ksumhi[:], in_=ksum[:])
        V("tensor_tensor", [B_["ksum"], B_["ksumhi"]], [B_["ksumlo"]], out=ksumlo[:], in0=ksum[:], in1=ksumhi[:], op=ALU.subtract)
        pB = bank(7).bitcast(BF16).rearrange("p (a b) -> p a b", a=8)
        for t in range(16):
            s = t % 2
            s1_advance(4)
            dma("sync", gct[s], c_g[t], w=[B_gct[s]])
            for h in range(8):
                mm(bank(6)[:, h * 8:(h + 1) * 8], QT[0:64, h, t * 128:(t + 1) * 128], ksumhi[:, h * 8:(h + 1) * 8], True, False,
                   [B_qt[t], B_["ksumhi"]], [bkB[6]])
                mm(bank(6)[:, h * 8:(h + 1) * 8], QT[0:64, h, t * 128:(t + 1) * 128], ksumlo[:, h * 8:(h + 1) * 8], False, True,
                   [B_qt[t], B_["ksumlo"]], [bkB[6]])
            V("tensor_tensor", [bkB[6], B_gct[s]], [B_gm], out=gm, in0=bank(6)[:, 0:64], in1=gct[s][:, 0:64], op=ALU.add)
            for h in range(8):
                V("max", [B_gm], [B_top8], out=top8[:, h, :], in_=gm[:, h * 8:(h + 1) * 8])
            for h in range(8):
                V("tensor_scalar", [B_gm, B_top8], [B_sel], out=sel[:, h * 8:(h + 1) * 8], in0=gm[:, h * 8:(h + 1) * 8],
                  scalar1=top8[:, h, 2:3], scalar2=None, op0=ALU.is_ge)
            V("tensor_tensor", [B_sel, B_gct[s]], [B_sel], out=sel, in0=sel, in1=gct[s][:, 64:128], op=ALU.mult)
            V("tensor_tensor", [B_sel, B_gct[s]], [B_sel], out=sel, in0=sel, in1=gct[s][:, 128:192], op=ALU.add)
            V("tensor_scalar", [B_sel], [B_bp[s]], out=biasp[s][:, :, 64:72], in0=sel.rearrange("p (h j) -> p h j", h=8),
              scalar1=-1.0, scalar2=BIG, op0=ALU.add, op1=ALU.mult)
            for h in range(8):
                tr(pB[0:72, h, :], biasp[s][:, h, :], identb[:], [B_bp[s], B_["identb"]], [bkB[7]])
            act(QT[64:72, :, t * 128:(t + 1) * 128], pB[64:72, :, :], AF.Copy, [bkB[7]], [B_qtb[t]])


        def s2_steps():
            def sb_view(off_kib, shape, dt):
                return AV(int(off_kib * KiB), shape, dt)
            pagesum = [sb_view(154 + 2 * i, [128, 512], F32) for i in range(2)]
            B_ps_ = sbuf_("pagesum", [])
            for pr in range(2):
                V("tensor_reduce", [B_accs[pr]], [B_ps_], out=pagesum[pr], in_=accs[pr].rearrange("p (pos f) -> p f pos", pos=4), axis=AX.X, op=ALU.add)
            qbcs = sb_view(158, [64, 512], F32); prod = sb_view(160, [64, 512], F32)
            oB = [162 * KiB]

            def smallB(shape, dt):
                n = 1
                for x_ in shape[1:]:
                    n *= x_
                ap = AV(oB[0], shape, dt)
                oB[0] += (n * 4 + 31) // 32 * 32
                assert oB[0] <= 166 * KiB
                return ap
            gate2 = smallB([64, 16], F32); gateT = smallB([8, 128], F32); top8s = smallB([8, 4, 8], F32); oh = smallB([8, 32], F32)
            junk8 = smallB([8, 32], F32); physf = smallB([8, 24], F32); Dm = smallB([8, 8, 24], F32)
            idxf2 = smallB([128, 192], F32); idxI2 = smallB([128, 192], I32)
            B_s2 = sbuf_("s2", [])
            Bqs = Bw["qf"]; Bks = B_kf[0]; Bvs = B_vf[0]
            for pr in range(2):
                mm(bank(7)[0:64, :], cf[:, 0:64], pagesum[pr], True, True, [B_ps_, B_["cf"]], [bkB[7]])
                mm(bank(6)[0:64, :], cf[0:4, 288 + 64 * pr:288 + 64 * (pr + 1)], qf[0:4, :], True, True, [Bqs, B_["cf"]], [bkB[6]])
                act(qbcs, bank(6)[0:64, :], AF.Copy, [bkB[6]], [B_s2])
                V("tensor_tensor", [bkB[7], B_s2], [B_s2], out=prod, in0=bank(7)[0:64, :], in1=qbcs, op=ALU.mult)
                V("tensor_reduce", [B_s2], [B_s2], out=gate2[:, pr * 8:(pr + 1) * 8], in_=prod.rearrange("p (h d) -> p h d", h=8), axis=AX.X, op=ALU.add)
                tr(bank(7)[0:8, pr * 64:(pr + 1) * 64], gate2[:, pr * 8:(pr + 1) * 8], identf[0:64, 0:64], [B_s2, B_["identf"]], [bkB[7]])
                act(gateT[:, pr * 64:(pr + 1) * 64], bank(7)[0:8, pr * 64:(pr + 1) * 64], AF.Copy, [bkB[7]], [B_s2])
            S2 = [B_s2]
            for b in range(4):
                V("max", S2, S2, out=top8s[:, b, :], in_=gateT[:, b * 32:(b + 1) * 32])
                for r_ in range(3):
                    V("tensor_scalar", S2, S2, out=oh, in0=gateT[:, b * 32:(b + 1) * 32], scalar1=top8s[:, b, r_:r_ + 1], scalar2=None, op0=ALU.is_equal)
                    for hf in range(2):
                        colp = (b * 3 + r_) * 2 + hf
                        V("tensor_tensor", S2 + [B_s0], S2, out=junk8, in0=oh,
                          in1=PTf[:, b * 64:(b + 1) * 64].rearrange("p (j t) -> p j t", t=2)[:, :, hf], op=ALU.mult)
                        V("tensor_reduce", S2, S2, out=physf[:, colp:colp + 1], in_=junk8, axis=AX.X, op=ALU.add)
            for h in range(8):
                V("tensor_scalar", S2 + [B_["cf"]], S2, out=Dm[:, h, :], in0=physf, scalar1=cf[0:8, 928 + h:929 + h], scalar2=None, op0=ALU.mult)
            mm(bank(7)[:, 0:192], cf[0:8, 940:1068], Dm.rearrange("p a b -> p (a b)"), True, True, S2 + [B_["cf"]], [bkB[7]])
            V("scalar_tensor_tensor", [bkB[7], B_["cf"]], S2, out=idxf2, in0=bank(7)[:, 0:192], scalar=1024.0, in1=poshc, op0=ALU.mult, op1=ALU.add)
            V("tensor_scalar", S2, S2, out=idxf2, in0=idxf2, scalar1=0.0, scalar2=2621439.0, op0=ALU.max, op1=ALU.min)
            V("tensor_copy", S2, S2, out=idxI2, in_=idxf2)
            yield
            NSET = 4
            Ks = [sb_view(166 + 6 * i, [128, 12, 64], F32) for i in range(NSET)]
            Vs = [sb_view(169 + 6 * i, [128, 12, 64], F32) for i in range(NSET)]
            B_ks = [sbuf_("ks%d" % i, []) for i in range(NSET)]; B_vs = [sbuf_("vs%d" % i, []) for i in range(NSET)]
            qbu = [sb_view(154 + 0.5 * i, [128, 128], F32) for i in range(2)]; B_qbu = [sbuf_("qbu0", []), sbuf_("qbu1", [])]
            oC = [158 * KiB]

            def smallC(shape):
                n = 1
                for x_ in shape[1:]:
                    n *= x_
                ap = AV(oC[0], shape, F32)
                oC[0] += (n * 4 + 31) // 32 * 32
                assert oC[0] <= 162 * KiB
                return ap
            STs = smallC([128, 192]); Es = smallC([128, 192]); denp = smallC([64, 32]); sself = smallC([4, 8])
            eself = smallC([4, 8]); D2 = smallC([4, 32]); ebvt = smallC([64, 96]); numt = smallC([64, 32]); dent = smallC([64, 32])
            B_s6 = sbuf_("s6", [])
            S6 = [B_s6]
            units = [(b, hp) for b in range(4) for hp in range(4)]

            def u_gather(u):
                b, hp = units[u]
                st_ = u % NSET
                for jj in range(12):
                    h = 2 * hp + jj // 6
                    col = h * 24 + b * 6 + (jj % 6)
                    P.dma("gpsimd", lambda e, jj=jj, col=col, st_=st_: e.indirect_dma_start(
                        out=Ks[st_][:, jj, :], out_offset=None, in_=ck[:, :], in_offset=bass.IndirectOffsetOnAxis(ap=idxI2[:, col:col + 1], axis=0)),
                        S2, [B_ks[st_]])
                    P.dma("gpsimd", lambda e, jj=jj, col=col, st_=st_: e.indirect_dma_start(
                        out=Vs[st_][:, jj, :], out_offset=None, in_=cv[:, :], in_offset=bass.IndirectOffsetOnAxis(ap=idxI2[:, col:col + 1], axis=0)),
                        S2, [B_vs[st_]])

            def u_compute(u):
                b, hp = units[u]
                st_ = u % NSET
                qs_ = u % 2
                mm(bank(6)[:, 0:128], cf[0:4, 416 + 128 * b:416 + 128 * (b + 1)], qf[0:4, hp * 128:(hp + 1) * 128], True, True, [Bqs, B_["cf"]], [bkB[6]])
                act(qbu[qs_], bank(6)[:, 0:128], AF.Copy, [bkB[6]], [B_qbu[qs_]])
                for jj in range(12):
                    hh = jj // 6
                    V("tensor_tensor", [B_ks[st_], B_qbu[qs_]], [B_ks[st_]], out=Ks[st_][:, jj, :], in0=Ks[st_][:, jj, :],
                      in1=qbu[qs_][:, hh * 64:(hh + 1) * 64], op=ALU.mult)
                c0 = b * 48 + hp * 12
                V("tensor_reduce", [B_ks[st_]], S6, out=STs[:, c0:c0 + 12], in_=Ks[st_], axis=AX.X, op=ALU.add)
                act(Es[:, c0:c0 + 12], STs[:, c0:c0 + 12], AF.Exp, S6, S6)
                for hh in range(2):
                    h = 2 * hp + hh
                    for s6 in range(6):
                        cc = c0 + hh * 6 + s6
                        mm(bank(7)[0:64, 480 + b * 8 + h:480 + b * 8 + h + 1], Vs[st_][:, hh * 6 + s6, :], Es[:, cc:cc + 1], s6 == 0, s6 == 5, S6 + [B_vs[st_]], [bkB[7]])
            LAG = NSET - 1
            for u in range(16 + LAG):
                if u < 16:
                    u_gather(u)
                if u - LAG >= 0:
                    u_compute(u - LAG)
                yield
            mm(bank(7)[0:64, 0:192], cf[:, 940:1004], Es, True, True, S6 + [B_["cf"]], [bkB[7]])
            V("tensor_reduce", [bkB[7]], S6, out=denp, in_=bank(7)[0:64, 0:192].rearrange("p (g s) -> p g s", s=6), axis=AX.X, op=ALU.add)
            prodqk = misc8[0:4, :]
            V("tensor_tensor", [Bqs, Bks, B_misc8], [B_misc8], out=prodqk, in0=qf[0:4, :], in1=kf[0][0:4, :], op=ALU.mult)
            V("tensor_reduce", [B_misc8], S6, out=sself, in_=prodqk.rearrange("p (h d) -> p h d", h=8), axis=AX.X, op=ALU.add)
            act(eself, sself, AF.Exp, S6, S6)
            for b in range(4):
                V("tensor_scalar", S6 + [B_["cf"]], S6, out=D2[:, b * 8:(b + 1) * 8], in0=eself, scalar1=cf[0:4, 936 + b:937 + b], scalar2=None, op0=ALU.mult)
            mm(bank(6)[0:64, 0:32], cf[0:4, 940:1004], D2, True, True, S6 + [B_["cf"]], [bkB[6]])
            for h in range(8):
                tr(bank(6)[0:64, 64 + h * 4:64 + h * 4 + 4], vf[0][0:4, h * 64:(h + 1) * 64], identf[0:4, 0:4], [Bvs, B_["identf"]], [bkB[6]])
            act(ebvt, bank(6)[0:64, 0:96], AF.Copy, [bkB[6]], S6)
            ebc3 = ebvt[:, 0:32].rearrange("p (b h) -> p b h", b=4)
            vT3 = ebvt[:, 64:96].rearrange("p (h b) -> p b h", b=4)
            V("tensor_tensor", S6, S6, out=numt.rearrange("p (b h) -> p b h", b=4), in0=vT3, in1=ebc3, op=ALU.mult)
            V("tensor_tensor", S6 + [bkB[7]], S6, out=numt, in0=numt, in1=bank(7)[0:64, 480:512], op=ALU.add)
            V("tensor_tensor", S6, S6, out=dent, in0=denp, in1=ebvt[:, 0:32], op=ALU.add)
            V("reciprocal", S6, S6, out=dent, in_=dent)
            V("tensor_tensor", S6, [B_att[16]], out=ATT[0:64, 16, :, 0:4], in0=numt.rearrange("p (b h) -> p h b", b=4),
              in1=dent.rearrange("p (b h) -> p h b", b=4), op=ALU.mult)
            yield
        s2gen = s2_steps()

        def s2_advance(n):
            for _ in range(n):
                try:
                    next(s2gen)
                except StopIteration:
                    return
        steps = []
        for h in range(8):
            for c in range(4):
                for kt in range(4 * c + 4):
                    steps.append((h, c, kt))
        LOOK = 2
        hc_ob = {}
        for si in range(len(steps) + LOOK):
            if si < len(steps):
                h, c, kt = steps[si]
                if kt == 0:
                    it_ = h * 4 + c
                    s1_advance(4)
                    hc_ob[(h, c)] = 3 + (len(hc_ob) % 2)
                i = max(0, kt - 4 * c)
                sb = si % 3
                pi = si % 4
                qr = [B_qt[4 * c + i_] for i_ in range(4)] + [B_qtb[4 * c + i_] for i_ in range(4)]
                mm(bank(sb)[:, i * 128:512], KT[0:72, h, kt * 128:(kt + 1) * 128], QT[0:72, h, c * 512 + i * 128:(c + 1) * 512],
                   True, True, [B_kt[kt]] + qr, [bkB[sb]])
                act(Pb[pi][:, i * 128:512], bank(sb)[:, i * 128:512], AF.Exp, [bkB[sb]], [B_P[pi]])
                if kt >= 4 * c:
                    V("tensor_tensor", [B_P[pi], B_["tri"]], [B_P[pi]], out=Pb[pi][:, i * 128:(i + 1) * 128],
                      in0=Pb[pi][:, i * 128:(i + 1) * 128], in1=tri[:], op=ALU.mult)
            sj = si - LOOK
            if sj >= 0:
                h, c, kt = steps[sj]
                nkt = 4 * c + 4
                i = max(0, kt - 4 * c)
                pi = sj % 4
                ob = hc_ob[(h, c)]
                mm(bank(ob)[0:65, i * 128:512], Vb[:, kt, h, :], Pb[pi][:, i * 128:512], kt == 0, kt == nkt - 1,
                   [B_vb[kt], B_P[pi]], [bkB[ob]])
                if kt == nkt - 1:
                    oi = ob - 3
                    V("tensor_copy", [bkB[ob]], [B_osb[oi]], out=osb[oi], in_=bank(ob)[0:65, :])
                    act(osb[oi][64:65, :], osb[oi][64:65, :], AF.Ln, [B_osb[oi]], [B_osb[oi]])
                    act(osb[oi][64:65, :], osb[oi][64:65, :], AF.Exp, [B_osb[oi]], [B_osb[oi]], scale=-1.0)
                    mm(bank(5)[0:64, :], cf[64:65, 940:1004], osb[oi][64:65, :], True, True, [B_osb[oi], B_["cf"]], [bkB[5]])
                    V("tensor_tensor", [B_osb[oi], bkB[5]], [B_att[4 * c + i_] for i_ in range(4)],
                      out=ATT[0:64, 4 * c:4 * c + 4, h, :], in0=osb[oi][0:64, :].rearrange("p (a b) -> p a b", a=4),
                      in1=bank(5)[0:64, :].rearrange("p (a b) -> p a b", a=4), op=ALU.mult)
        s1_advance(1000)
        s2_advance(1000)
        att_done = B_qt + B_kt + B_vb + B_qtb
        ACC = AV(48 * KiB, [128, NT, 1024], F32); B_acc = [Buf("acc%d" % t) for t in range(NT)]
        for b_ in B_acc:
            P.alias(b_, att_done)
        WoC = AV(116 * KiB, [128, 4, 1024], BF16); B_woc = Buf("woc"); P.alias(B_woc, att_done)
        gates = AV(124 * KiB, [128, NT, 32], F32); B_gates = [Buf("gates%d" % t) for t in range(NT)]
        for b_ in B_gates:
            P.alias(b_, att_done)
        WoA = AV(146 * KiB, [64, 8, 1024], BF16); B_woa = Buf("woa")
        xr2 = [AV((162 + 4 * i) * KiB, [128, 1024], F32) for i in range(2)]; B_xr2 = [Buf("xr0"), Buf("xr1")]
        hn2 = [AV((170 + 4 * i) * KiB, [128, 1024], F32) for i in range(2)]; B_hn2 = [Buf("hn0"), Buf("hn1")]
        hnT32_2 = [AV((178 + 4 * i) * KiB, [128, 8, 128], F32) for i in range(2)]; B_hnT2 = [Buf("hnT0"), Buf("hnT1")]
        B_rs2 = []
        wstg = AV(34 * KiB, [128, 3, 1024], F32); B_wstg = Buf("wstg")
        p2_bufs = B_P + B_osb + B_bp + B_gct + [B_gm, B_top8, B_sel]
        p3_w = [B_woa] + B_xr2 + B_hn2 + B_hnT2 + B_rs2
        for b_ in p3_w + [B_wstg]:
            P.alias(b_, all_p1 + SAMPLE_BUFS + p2_bufs)
        for hg in ((0, 1, 2), (3, 4, 5), (6, 7)):
            n = len(hg)
            dma("sync", wstg[0:64, 0:n, :], w_out[hg[0] * 64:(hg[-1] + 1) * 64, :].rearrange("(h r) n -> r h n", r=64), w=[B_wstg])
            V("tensor_copy", [B_wstg], [B_woa], out=WoA[:, hg[0]:hg[0] + n, :], in_=wstg[0:64, 0:n, :])
        for cg in ((0, 1, 2), (3,)):
            n = len(cg)
            dma("sync", wstg[:, 0:n, :], w_out[512 + cg[0] * 128:512 + (cg[-1] + 1) * 128, :].rearrange("(c p) n -> p c n", p=128), w=[B_wstg])
            V("tensor_copy", [B_wstg], [B_woc], out=WoC[:, cg[0]:cg[0] + n, :], in_=wstg[:, 0:n, :])
        dma("sync", gbc[:], g_ffn.partition_broadcast(128), w=[B_["gbc"]])
        pT32 = PS[2].rearrange("p (a b) -> p a b", a=8)
        lgall = AV(186 * KiB, [128, NT, 36], F32); B_lgall = Buf("lgall")
        P.alias(B_lgall, all_p1 + SAMPLE_BUFS + p2_bufs)

        def p3_W(t):
            hps = PS[t % 2]
            Bh = [bkB[2 * (t % 2)], bkB[2 * (t % 2) + 1]]
            for half in range(2):
                for h in range(8):
                    mm(hps[:, half * 512:(half + 1) * 512], ATT[0:64, t, h, :], WoA[:, h, half * 512:(half + 1) * 512], h == 0, False,
                       [B_att[t], B_woa], Bh)
                for ct in range(4):
                    mm(hps[:, half * 512:(half + 1) * 512], convT[:, t, ct, :], WoC[:, ct, half * 512:(half + 1) * 512], False, ct == 3,
                       [B_ct[t], B_woc], Bh)
            i = t % 2
            if t < 16:
                dma("sync", xr2[i], xp[t * 128:(t + 1) * 128, :], w=[B_xr2[i]])
            else:
                V("memset", [], [B_xr2[i]], eng="gpsimd", ap=xr2[i], constant=0.0)
                dma("sync", xr2[i][0:4, :], xs, w=[B_xr2[i]])

        def p3_rest(t):
            i = t % 2
            hps = PS[i]; Bh = [bkB[2 * i], bkB[2 * i + 1]]
            hn = hn2[i]; B_hn = B_hn2[i]; hnT32 = hnT32_2[i]; B_hnT32 = B_hnT2[i]
            V("tensor_tensor", Bh + [B_xr2[i]], [B_acc[t]], out=ACC[:, t, :], in0=hps[:, :], in1=xr2[i], op=ALU.add)
            col = NT + t
            act(hn, ACC[:, t, :], AF.Square, [B_acc[t]], [B_hn, B_ssq[col]], accum_out=ssq[:, col:col + 1])
            act(rt[:, col:col + 1], ssq[:, col:col + 1], AF.Sqrt, [B_ssq[col], B_["epsb"]], [B_ssq[col]], scale=1.0 / 1024.0, bias=epsb[:, 0:1])
            V("reciprocal", [B_ssq[col]], [B_ssq[col]], out=rstd[:, col:col + 1], in_=rt[:, col:col + 1])
            V("scalar_tensor_tensor", [B_acc[t], B_ssq[col], B_["gbc"]], [B_hn], out=hn, in0=ACC[:, t, :], scalar=rstd[:, col:col + 1],
              in1=gbc[:], op0=ALU.mult, op1=ALU.mult)
            for c in range(8):
                tr(pT32[:, c, :], hn[:, c * 128:(c + 1) * 128], identf[:], [B_hn, B_["identf"]], [bkB[4], bkB[5]])
            act(hnT32, pT32, AF.Copy, [bkB[4], bkB[5]], [B_hnT32])
            V("tensor_copy", [B_hnT32], [B_att[t]], eng="gpsimd", out=ATT[:, t, :, :], in_=hnT32)
            for c in range(8):
                mm(bank(6)[:, 0:36], hnT32[:, c, :], wr[:, c, :], c == 0, c == 7, [B_hnT32, B_["wr"]], [bkB[6]])
            V("tensor_tensor", [bkB[6], B_["rb"]], [B_lgall], out=lgall[:, t, :], in0=bank(6)[:, 0:36], in1=rb[:], op=ALU.add)

        p3_W(0)
        for t in range(NT):
            if t + 1 < NT:
                p3_W(t + 1)
            p3_rest(t)

        ob_ = [162 * KiB]

        def rtile(shape):
            n = 1
            for x_ in shape[1:]:
                n *= x_
            ap = AV(ob_[0], shape, F32)
            ob_[0] += (n * 4 + 31) // 32 * 32
            assert ob_[0] <= 170 * KiB
            return ap
        B_rt = Buf("rtmp"); P.alias(B_rt, B_xr2 + B_hn2)
        Rr = [B_rt]
        m4 = rtile([128, NT]); ohg = rtile([128, NT, 4]); eg = rtile([128, NT, 4]); sg4 = rtile([128, NT]); pg = rtile([128, NT])
        leg = rtile([128, NT, 8]); tmp8 = rtile([128, NT, 8]); t8a = rtile([128, NT, 8]); dd = rtile([128, NT]); w1 = rtile([128, NT]); w2 = rtile([128, NT])
        e1 = rtile([128, NT, 8]); e2 = rtile([128, NT, 8]); gi = rtile([128, NT, 8])
        lgg = lgall[:, :, 0:4]
        lge = lgall[:, :, 4:36].rearrange("p t (g i) -> p t g i", g=4)

        def bc(ap2, n):
            return ap2.unsqueeze(2).to_broadcast([128, NT, n])
        V("tensor_reduce", [B_lgall], Rr, out=m4, in_=lgg, axis=AX.X, op=ALU.max)
        V("tensor_tensor", [B_lgall] + Rr, Rr, out=ohg, in0=lgg, in1=bc(m4, 4), op=ALU.is_ge)
        V("tensor_tensor", [B_lgall] + Rr, Rr, out=eg, in0=lgg, in1=bc(m4, 4), op=ALU.subtract)
        act(eg, eg, AF.Exp, Rr, Rr)
        V("tensor_reduce", Rr, Rr, out=sg4, in_=eg, axis=AX.X, op=ALU.add)
        V("reciprocal", Rr, Rr, out=pg, in_=sg4)
        for g_ in range(4):
            if g_ == 0:
                V("tensor_tensor", [B_lgall] + Rr, Rr, out=leg, in0=lge[:, :, 0, :], in1=bc(ohg[:, :, 0], 8), op=ALU.mult)
            else:
                V("tensor_tensor", [B_lgall] + Rr, Rr, out=tmp8, in0=lge[:, :, g_, :], in1=bc(ohg[:, :, g_], 8), op=ALU.mult)
                V("tensor_tensor", Rr, Rr, out=leg, in0=leg, in1=tmp8, op=ALU.add)
        for t in range(NT):
            V("max", Rr, Rr, out=t8a[:, t, :], in_=leg[:, t, :])
        V("tensor_tensor", Rr, Rr, out=dd, in0=t8a[:, :, 1], in1=t8a[:, :, 0], op=ALU.subtract)
        act(w2, dd, AF.Exp, Rr, Rr)
        V("tensor_scalar", Rr, Rr, out=w1, in0=w2, scalar1=1.0, scalar2=None, op0=ALU.add)
        V("reciprocal", Rr, Rr, out=w1, in_=w1)
        V("tensor_tensor", Rr, Rr, out=w2, in0=w2, in1=w1, op=ALU.mult)
        V("tensor_tensor", Rr, Rr, out=w1, in0=w1, in1=pg, op=ALU.mult)
        V("tensor_tensor", Rr, Rr, out=w2, in0=w2, in1=pg, op=ALU.mult)
        V("tensor_tensor", Rr, Rr, out=e1, in0=leg, in1=bc(t8a[:, :, 0], 8), op=ALU.is_equal)
        V("tensor_tensor", Rr, Rr, out=e1, in0=e1, in1=bc(w1, 8), op=ALU.mult)
        V("tensor_tensor", Rr, Rr, out=e2, in0=leg, in1=bc(t8a[:, :, 1], 8), op=ALU.is_equal)
        V("tensor_tensor", Rr, Rr, out=e2, in0=e2, in1=bc(w2, 8), op=ALU.mult)
        V("tensor_tensor", Rr, Rr, out=gi, in0=e1, in1=e2, op=ALU.add)
        gates4 = gates.rearrange("p t (g i) -> p t g i", g=4)
        for g_ in range(4):
            V("tensor_tensor", Rr, B_gates, out=gates4[:, :, g_, :], in0=gi, in1=bc(ohg[:, :, g_], 8), op=ALU.mult)

        p3_done = p3_w + [B_wstg, B_woc, B_lgall, B_rt] + B_ct
        WB0 = 129 * KiB
        NSTG = 3
        estg = [AV(34 * KiB + i * 4 * KiB, [128, 1024], F32) for i in range(NSTG)]; B_estg = [Buf("estg%d" % i) for i in range(NSTG)]
        Wgu = [AV(WB0 + i * 12 * KiB, [128, 8, 512], BF16) for i in range(4)]
        Wd = [AV(WB0 + i * 12 * KiB + 8 * KiB, [128, 2, 1024], BF16) for i in range(4)]
        B_wgu = [Buf("wgu%d" % i) for i in range(4)]; B_wd = [Buf("wd%d" % i) for i in range(4)]
        o = WB0 + 48 * KiB
        sgb = [AV(o + i * KiB, [128, 256], F32) for i in range(2)]; o += 2 * KiB
        hbb = [AV(o + i * 512, [128, 256], BF16) for i in range(2)]; o += KiB
        hTb = [AV(o + i * 512, [128, 2, 128], BF16) for i in range(2)]; o += KiB
        assert o <= ARENA
        B_sg = [Buf("sg0"), Buf("sg1")]; B_hb = [Buf("hb0"), Buf("hb1")]; B_hT = [Buf("hT0"), Buf("hT1")]
        for b_ in B_estg + B_wgu + B_wd + B_sg + B_hb + B_hT:
            P.alias(b_, p3_done + p2_bufs + all_p1 + SAMPLE_BUFS)
        sti = [0]

        def load_expert(e, slot):
            for (src, coff) in ((w_gate, 0), (w_up, 256)):
                for hf in range(2):
                    i = sti[0] % NSTG; sti[0] += 1
                    dma("sync", estg[i].rearrange("p (c f) -> p c f", c=4),
                        src[e, hf * 512:(hf + 1) * 512, :].rearrange("(c p) f -> p c f", p=128), w=[B_estg[i]])
                    V("tensor_copy", [B_estg[i]], [B_wgu[slot]], eng="gpsimd", out=Wgu[slot][:, hf * 4:(hf + 1) * 4, coff:coff + 256],
                      in_=estg[i].rearrange("p (c f) -> p c f", c=4))
            for f_ in range(2):
                i = sti[0] % NSTG; sti[0] += 1
                dma("sync", estg[i], w_down[e, f_ * 128:(f_ + 1) * 128, :], w=[B_estg[i]])
                V("tensor_copy", [B_estg[i]], [B_wd[slot]], eng="gpsimd", out=Wd[slot][:, f_, :], in_=estg[i])

        pTh4 = bank(3).bitcast(BF16).rearrange("p (a b) -> p a b", a=8)
        B_pth = [Buf("pth%d" % i) for i in range(4)]
        for b_ in B_pth:
            b_.last_w = bkB[3].last_w; b_.readers = list(bkB[3].readers)
        sg3 = [AV(o_, [128, 256], F32) for o_ in (WB0 + 48 * KiB, WB0 + 49 * KiB, WB0 + 52 * KiB)]
        hb3 = [AV(WB0 + 50 * KiB + i * 512, [128, 256], BF16) for i in range(3)]
        hT4 = [AV(WB0 + 53 * KiB + i * 512, [128, 2, 128], BF16) for i in range(4)]
        assert WB0 + 55 * KiB <= ARENA
        B_sg3 = [Buf("sg3_%d" % i) for i in range(3)]; B_hb3 = [Buf("hb3_%d" % i) for i in range(3)]; B_hT4 = [Buf("hT4_%d" % i) for i in range(4)]
        for b_ in B_sg3 + B_hb3 + B_hT4:
            P.alias(b_, p3_done + p2_bufs + all_p1 + SAMPLE_BUFS + B_sg + B_hb + B_hT)
        load_expert(0, 0); load_expert(1, 1)
        msteps = []
        for ep in range(NE // 2):
            for t in range(NT):
                for k in range(2):
                    msteps.append((ep, t, k))

        def emit_G(i):
            ep, t, k = msteps[i]
            e = 2 * ep + k; slot = e % 4; gb = i % 3
            for c in range(8):
                mm(bank(gb), ATT[:, t, c, :], Wgu[slot][:, c, :], c == 0, c == 7, [B_att[t], B_wgu[slot]], [bkB[gb]])

        def emit_A(i):
            ep, t, k = msteps[i]
            e = 2 * ep + k; gb = i % 3
            act(sg3[gb], bank(gb)[:, 0:256], AF.Silu, [bkB[gb]], [B_sg3[gb]])
            V("scalar_tensor_tensor", [bkB[gb], B_sg3[gb], B_gates[t]], [B_hb3[gb]], out=hb3[gb], in0=bank(gb)[:, 256:512],
              scalar=gates[:, t, e:e + 1], in1=sg3[gb], op0=ALU.mult, op1=ALU.mult)

        def emit_T(i):
            gb = i % 3; ts = i % 4
            for f_ in range(2):
                tr(pTh4[:, 2 * ts + f_, :], hb3[gb][:, f_ * 128:(f_ + 1) * 128], identb[:], [B_hb3[gb], B_["identb"]], [B_pth[ts]])
            act(hT4[ts], pTh4[:, 2 * ts:2 * ts + 2, :], AF.Copy, [B_pth[ts]], [B_hT4[ts]])

        def emit_D(i):
            ep, t, k = msteps[i]
            if t == 0 and k == 0 and ep + 1 < NE // 2:
                load_expert(2 * ep + 2, (2 * ep + 2) % 4); load_expert(2 * ep + 3, (2 * ep + 3) % 4)
            e = 2 * ep + k; slot = e % 4; ts = i % 4
            ob = 2 + ((i // 2) % 2)
            outp = PS[ob]; Bout = [bkB[2 * ob], bkB[2 * ob + 1]]
            for half in range(2):
                for f_ in range(2):
                    mm(outp[:, half * 512:(half + 1) * 512], hT4[ts][:, f_, :], Wd[slot][:, f_, half * 512:(half + 1) * 512],
                       k == 0 and f_ == 0, k == 1 and f_ == 1, [B_hT4[ts], B_wd[slot]], Bout)
            if k == 1:
                V("tensor_tensor", Bout + [B_acc[t]], [B_acc[t]], out=ACC[:, t, :], in0=outp[:, :], in1=ACC[:, t, :], op=ALU.add)

        NS = len(msteps)
        emit_G(0)
        for i in range(NS + 1):
            if i + 1 < NS:
                emit_G(i + 1)
            if i < NS:
                emit_A(i)
                emit_T(i)
            if i - 1 >= 0:
                emit_D(i - 1)

        dma("sync", gbc[:], g_final.partition_broadcast(128), w=[B_["gbc"]])
        yb = [estg[0], estg[1]]; B_yb = [B_estg[0], B_estg[1]]
        jk5 = estg[2]; B_jk5 = B_estg[2]
        for t in range(NT):
            col = 2 * NT + t
            i = t % 2
            act(jk5, ACC[:, t, :], AF.Square, [B_acc[t]], [B_jk5, B_ssq[col]], accum_out=ssq[:, col:col + 1])
            act(rt[:, col:col + 1], ssq[:, col:col + 1], AF.Sqrt, [B_ssq[col], B_["epsb"]], [B_ssq[col]], scale=1.0 / 1024.0, bias=epsb[:, 0:1])
            V("reciprocal", [B_ssq[col]], [B_ssq[col]], out=rstd[:, col:col + 1], in_=rt[:, col:col + 1])
            V("scalar_tensor_tensor", [B_acc[t], B_ssq[col], B_["gbc"]], [B_yb[i]], out=yb[i], in0=ACC[:, t, :], scalar=rstd[:, col:col + 1],
              in1=gbc[:], op0=ALU.mult, op1=ALU.mult)
            if t < 16:
                dma("sync", yp[t * 128:(t + 1) * 128, :], yb[i], r=[B_yb[i]])
            else:
                dma("sync", ys, yb[i][0:4, :], r=[B_yb[i]])
        P.run()
    return nc


_NC = None


def kernel(**inputs):
    global _NC
    if _NC is None:
        _NC = build_program()
    nc = _NC
    f = lambda a: np.ascontiguousarray(np.asarray(a))
    consts = host_consts()
    ckf = f(inputs["cache_k"]).reshape(2621440, 64)
    cvf = f(inputs["cache_v"]).reshape(2621440, 64)
    shared = {
        "ck": ckf, "cv": cvf,
        "g_mix": f(inputs["g_mix"]).reshape(1024), "w_in": f(inputs["w_in"]).reshape(1024, 3072),
        "conv_w": f(inputs["conv_w"]).reshape(3, 512), "w_out": f(inputs["w_out"]).reshape(1024, 1024),
        "g_ffn": f(inputs["g_ffn"]).reshape(1024), "w_rg": f(inputs["w_router_group"]).reshape(1024, 4),
        "b_rg": f(inputs["b_router_group"]).reshape(4), "w_re": f(inputs["w_router_expert"]).reshape(1024, 32),
        "b_re": f(inputs["b_router_expert"]).reshape(32), "w_gate": f(inputs["w_gate"]).reshape(32, 1024, 256),
        "w_up": f(inputs["w_up"]).reshape(32, 1024, 256), "w_down": f(inputs["w_down"]).reshape(32, 256, 1024),
        "g_final": f(inputs["g_final"]).reshape(1024),
    }
    shared.update(consts)
    xpr = f(inputs["x_prompt"]); xsm = f(inputs["x_sample"]).reshape(32, 1024)
    scv = f(inputs["state_conv"]).reshape(32, 2, 512); ptb = f(inputs["page_table"]).astype(np.int32)
    in_maps = []
    for c in range(NCORES):
        m = dict(shared)
        m["xp"] = xpr[c]
        m["xs"] = xsm[4 * c:4 * c + 4]
        m["sc"] = scv[4 * c:4 * c + 4].reshape(8, 512)
        m["pt"] = ptb[4 * c:4 * c + 4]
        in_maps.append(m)
    res = run_bass_kernel_spmd(nc, in_maps, core_ids=list(range(NCORES)))
    R = res.results
    y_prompt = np.stack([R[c]["yp"] for c in range(NCORES)]).reshape(8, 2048, 1024)
    y_sample = np.concatenate([R[c]["ys"] for c in range(NCORES)]).reshape(32, 1, 1024)
    k_prompt = np.stack([R[c]["kp"] for c in range(NCORES)]).reshape(1, 8, 2048, 8, 64)
    v_prompt = np.stack([R[c]["vp"] for c in range(NCORES)]).reshape(1, 8, 2048, 8, 64)
    conv_prompt = np.stack([R[c]["cp"] for c in range(NCORES)]).reshape(1, 8, 2, 512)
    k_sample = np.concatenate([R[c]["ks"] for c in range(NCORES)]).reshape(1, 32, 1, 8, 64)
    v_sample = np.concatenate([R[c]["vs"] for c in range(NCORES)]).reshape(1, 32, 1, 8, 64)
    conv_sample = np.concatenate([R[c]["cso"] for c in range(NCORES)]).reshape(1, 32, 2, 512)
    return tuple(np.asarray(a, dtype=np.float32) for a in
                 (y_prompt, y_sample, k_prompt, v_prompt, conv_prompt, k_sample, v_sample, conv_sample))
```

```python
import contextlib
import numpy as np
import ml_dtypes
import concourse.bass as bass
import concourse.mybir as mybir
from concourse.bass_utils import run_bass_kernel_spmd

F32 = mybir.dt.float32
BF16 = mybir.dt.bfloat16
I32 = mybir.dt.int32
U8 = mybir.dt.uint8
AF = mybir.ActivationFunctionType
ALU = mybir.AluOpType
AX = mybir.AxisListType

ENGS = ("sync", "scalar", "vector", "gpsimd", "tensor")
NCORES = 8
NT = 17
NE = 32
BIG = 30000.0
KiB = 1024


class Buf:
    __slots__ = ("name", "last_w", "readers")

    def __init__(self, name):
        self.name = name
        self.last_w = None
        self.readers = []


class Ins:
    __slots__ = ("eng", "fn", "deps", "signal", "sigval", "is_dma", "dsem", "dval")

    def __init__(self, eng, fn, is_dma):
        self.eng = eng
        self.fn = fn
        self.deps = []
        self.signal = False
        self.sigval = None
        self.is_dma = is_dma
        self.dsem = None
        self.dval = None


class Prog:
    def __init__(self, nc, ring=8):
        self.nc = nc
        self.q = {e: [] for e in ENGS}
        self.ringd = {e: ring for e in ENGS}
        self.ringd["gpsimd"] = 8
        self.dma_count = {e: 0 for e in ENGS}
        self.dma_hist = {e: [] for e in ENGS}

    def _add(self, ins, r, w):
        deps = []
        for b in r:
            if b.last_w is not None:
                deps.append(b.last_w)
        for b in w:
            if b.last_w is not None:
                deps.append(b.last_w)
            deps.extend(b.readers)
        for b in w:
            b.last_w = ins
            b.readers = []
        for b in r:
            if b.last_w is not ins:
                b.readers.append(ins)
        seen = set()
        for d in deps:
            if d is ins or id(d) in seen:
                continue
            seen.add(id(d))
            if (not d.is_dma) and d.eng == ins.eng and d.eng == "tensor" and not ins.is_dma:
                continue
            ins.deps.append(d)
            if not d.is_dma:
                d.signal = True
        self.q[ins.eng].append(ins)
        return ins

    def op(self, eng, fn, r=(), w=()):
        return self._add(Ins(eng, fn, False), list(r), list(w))

    def dma(self, eng, fn, r=(), w=()):
        ins = Ins(eng, fn, True)
        k = self.dma_count[eng]
        self.dma_count[eng] += 1
        hist = self.dma_hist[eng]
        ring = self.ringd[eng]
        if k >= ring:
            ins.deps.append(hist[k - ring])
        hist.append(ins)
        ins.dsem = (eng, k % ring)
        ins.dval = 16 * (k // ring + 1)
        return self._add(ins, list(r), list(w))

    def alias(self, new, olds):
        for o in olds:
            if o.last_w is not None:
                new.readers.append(o.last_w)
            new.readers.extend(o.readers)

    def run(self):
        nc = self.nc
        with contextlib.ExitStack() as st:
            esem = {e: st.enter_context(nc.semaphore("es_" + e)) for e in ENGS}
            dsem = {}
            for e in ENGS:
                for i in range(min(self.ringd[e], self.dma_count[e])):
                    dsem[(e, i)] = st.enter_context(nc.semaphore("ds_%s%d" % (e, i)))
            for e in ENGS:
                c = 0
                for ins in self.q[e]:
                    if (not ins.is_dma) and ins.signal:
                        c += 1
                        ins.sigval = c
            allsems = list(esem.values()) + list(dsem.values())
            for s_ in allsems:
                nc.gpsimd.sem_clear(s_)
            nc.all_engine_barrier()
            block = nc.Block()
            block.__enter__()

            def make(ename):
                def body(eng):
                    known = {}
                    for ins in self.q[ename]:
                        need = {}
                        for d in ins.deps:
                            if d.is_dma:
                                s, v = dsem[d.dsem], d.dval
                            else:
                                s, v = esem[d.eng], d.sigval
                            key = id(s)
                            if known.get(key, 0) >= v:
                                continue
                            if key not in need or need[key][1] < v:
                                need[key] = (s, v)
                        for key, (s, v) in need.items():
                            eng.wait_ge(s, v)
                            known[key] = v
                        r = ins.fn(eng)
                        if ins.is_dma:
                            r.then_inc(dsem[ins.dsem], 16)
                        elif ins.signal:
                            r.then_inc(esem[ename], 1)
                    for ins in self.dma_hist[ename][-self.ringd[ename]:]:
                        s, v = dsem[ins.dsem], ins.dval
                        if known.get(id(s), 0) < v:
                            eng.wait_ge(s, v)
                            known[id(s)] = v
                return body

            for e in ENGS:
                if self.q[e]:
                    getattr(block, e)(make(e))
            block.__exit__(None, None, None)
            st.pop_all()
            nc.all_engine_barrier()
            for s_ in allsems:
                nc.gpsimd.sem_clear(s_)
            nc.all_engine_barrier()


NFC = 1068


def host_consts():
    c = {}
    c["c_identb"] = np.eye(128, dtype=np.float32).astype(ml_dtypes.bfloat16)
    c["c_identf"] = np.eye(128, dtype=np.float32)
    kk = np.arange(128)[:, None]
    qq = np.arange(128)[None, :]
    c["c_tri"] = (qq >= kk).astype(np.float32).astype(ml_dtypes.bfloat16)
    inv = (500000.0 ** (-np.arange(0, 16, 2, dtype=np.float32) / np.float32(16))).astype(np.float32)
    cs = np.zeros((NT, 128, 128), np.float32)
    for t in range(NT):
        pos = (t * 128 + np.arange(128)) if t < 16 else np.full(128, 8192)
        ang = pos.astype(np.float32)[:, None] * inv[None, :]
        ang = ang.astype(np.float32).astype(np.float64)
        cs[t, :, 0:64] = np.tile(np.cos(ang), (1, 8))
        cs[t, :, 64:128] = np.tile(np.sin(ang), (1, 8))
    c["c_cs"] = cs
    g = np.zeros((16, 128, 192), np.float32)
    for t in range(16):
        own = (t * 128 + np.arange(128)) // 256
        j = np.arange(8)[None, :]
        past = (j < own[:, None]).astype(np.float32)
        ownm = (j == own[:, None]).astype(np.float32)
        g[t, :, 0:64] = np.tile((past - 1.0) * 1e30, (1, 8))
        g[t, :, 64:128] = np.tile(past, (1, 8))
        g[t, :, 128:192] = np.tile(ownm, (1, 8))
    c["c_g"] = g
    blk = np.zeros((8, 2048), np.float32)
    for j in range(8):
        blk[j, j * 256:(j + 1) * 256] = 1.0
    c["c_blk"] = blk.astype(ml_dtypes.bfloat16)
    f = np.zeros((128, NFC), np.float32)
    p = np.arange(128)
    f[p, p // 2] = 1.0
    for h in range(8):
        for b in range(4):
            for r in range(3):
                for hf in range(2):
                    col = 64 + ((h * 4 + b) * 3 + r) * 2 + hf
                    f[:, col] = p * 8 + h
    f[:, 256:288] = np.arange(32)[None, :]
    for pr in range(2):
        for m in range(64):
            f[2 * pr + m // 32, 288 + pr * 64 + m] = 1.0
    for b in range(4):
        f[b, 416 + b * 128: 416 + (b + 1) * 128] = 1.0
    f[0:8, 928:936] = np.eye(8)
    f[0:4, 936:940] = np.eye(4)
    f[:, 940:1068] = 1.0
    c["c_f"] = f
    return c


def build_program(debug=False):
    nc = bass.Bass("TRN2", target_bir_lowering=False)
    P = Prog(nc)

    def din(name, shape, dt=F32):
        return nc.dram_tensor(name, list(shape), dt, kind="ExternalInput").ap()

    def dout(name, shape, dt=F32):
        return nc.dram_tensor(name, list(shape), dt, kind="ExternalOutput").ap()

    xp = din("xp", [2048, 1024]); xs = din("xs", [4, 1024])
    ck = din("ck", [2621440, 64]); cv = din("cv", [2621440, 64])
    sc = din("sc", [8, 512]); pt = din("pt", [4, 64], I32)
    g_mix = din("g_mix", [1024]); w_in = din("w_in", [1024, 3072]); conv_w = din("conv_w", [3, 512])
    w_out = din("w_out", [1024, 1024]); g_ffn = din("g_ffn", [1024])
    w_rg = din("w_rg", [1024, 4]); b_rg = din("b_rg", [4]); w_re = din("w_re", [1024, 32]); b_re = din("b_re", [32])
    w_gate = din("w_gate", [32, 1024, 256]); w_up = din("w_up", [32, 1024, 256]); w_down = din("w_down", [32, 256, 1024])
    g_final = din("g_final", [1024])
    c_identb = din("c_identb", [128, 128], BF16); c_identf = din("c_identf", [128, 128])
    c_tri = din("c_tri", [128, 128], BF16); c_cs = din("c_cs", [NT, 128, 128]); c_g = din("c_g", [16, 128, 192])
    c_blk = din("c_blk", [8, 2048], BF16); c_f = din("c_f", [128, NFC])
    yp = dout("yp", [2048, 1024]); ys = dout("ys", [4, 1024])
    kp = dout("kp", [2048, 512]); vp = dout("vp", [2048, 512]); cp = dout("cp", [2, 512])
    ks = dout("ks", [4, 512]); vs = dout("vs", [4, 512]); cso = dout("cso", [8, 512])
    ckc = ck.rearrange("(a b) d -> a (b d)", b=32)

    st = contextlib.ExitStack()
    with st:
        ARENA = 190 * KiB
        arena = st.enter_context(nc.sbuf_tensor("arena", [128, ARENA], U8))

        def AV(off, shape, dt, p0=0):
            isz = 2 if dt == BF16 else 4
            n = 1
            for s in shape[1:]:
                n *= s
            nb = n * isz
            assert off + nb <= ARENA, (off, nb)
            ap = arena[p0:p0 + shape[0], off:off + nb].bitcast(dt)
            if len(shape) == 3:
                ap = ap.rearrange("p (a b) -> p a b", a=shape[1])
            elif len(shape) == 4:
                ap = ap.rearrange("p (a b c) -> p a b c", a=shape[1], b=shape[2])
            return ap

        def ST(name, shape, dt):
            return st.enter_context(nc.sbuf_tensor(name, list(shape), dt))

        PS = [st.enter_context(nc.psum_tensor("ps%d" % i, [128, 1024], F32)) for i in range(4)]

        def bank(i):
            return PS[i // 2][:, (i % 2) * 512:(i % 2 + 1) * 512]

        bkB = [P_buf for P_buf in (Buf("bank%d" % i) for i in range(8))]

        identb = ST("identb", [128, 128], BF16); identf = ST("identf", [128, 128], F32); tri = ST("tri", [128, 128], BF16)
        cf = ST("cf", [128, NFC], F32)
        gbc = ST("gbc", [128, 1024], F32)
        ssq = ST("ssq", [128, 3 * NT], F32); rt = ST("rt", [128, 3 * NT], F32); rstd = ST("rstd", [128, 3 * NT], F32)
        convw = ST("convw", [128, 12], F32)
        ksum = ST("ksum", [64, 64], F32); ksumhi = ST("ksumhi", [64, 64], BF16); ksumlo = ST("ksumlo", [64, 64], BF16)
        ksumr = ST("ksumr", [64, 64], F32)
        epsb = ST("epsb", [128, 1], F32)
        rb = ST("rb", [128, 36], F32)
        wr = ST("wr", [128, 8, 36], F32)
        B_ = {n: Buf(n) for n in ("identb identf tri cf gbc convw ksum ksumhi ksumlo ksumr epsb rb wr").split()}
        B_ssq = [Buf("ssq%d" % i) for i in range(3 * NT)]

        def dma(q, out, in_, r=(), w=()):
            return P.dma(q, lambda e: e.dma_start(out=out, in_=in_), r, w)

        def act(out, in_, func, r, w, **kw):
            return P.op("scalar", lambda e: e.activation(out=out, in_=in_, func=func, **kw), r, w)

        def mm(out, lhsT, rhs, start, stop, r, w):
            return P.op("tensor", lambda e: e.matmul(out, lhsT=lhsT, rhs=rhs, start=start, stop=stop), r, w)

        def tr(out, in_, ident, r, w):
            return P.op("tensor", lambda e: e.transpose(out=out, in_=in_, identity=ident), r, w)

        def V(name, r, w, eng="vector", **kw):
            return P.op(eng, lambda e: getattr(e, name)(**kw), r, w)

        dma("sync", identb[:], c_identb, w=[B_["identb"]])
        dma("sync", identf[:], c_identf, w=[B_["identf"]])
        dma("sync", tri[:], c_tri, w=[B_["tri"]])
        dma("sync", cf[:], c_f, w=[B_["cf"]])
        dma("sync", gbc[:], g_mix.partition_broadcast(128), w=[B_["gbc"]])
        V("memset", [], [B_["epsb"]], ap=epsb[:], constant=1e-6)
        PairM = cf[:, 0:64]; poshc = cf[:, 64:256]; chunkid = cf[:, 256:288]
        ones_f = cf[:, 940:1068]

        cw12 = ST("cw12", [12, 128], F32); Bcw12 = Buf("cw12")
        dma("sync", cw12[:], conv_w.rearrange("k (t p) -> (k t) p", p=128), w=[Bcw12])
        tr(bank(0)[:, 0:12], cw12[:], identf[0:12, 0:12], [Bcw12, B_["identf"]], [bkB[0]])
        act(convw[:], bank(0)[:, 0:12], AF.Copy, [bkB[0]], [B_["convw"]])
        dma("sync", wr[:, :, 0:4], w_rg.rearrange("(c p) n -> p c n", p=128), w=[B_["wr"]])
        dma("sync", wr[:, :, 4:36], w_re.rearrange("(c p) n -> p c n", p=128), w=[B_["wr"]])
        dma("sync", rb[:, 0:4], b_rg.partition_broadcast(128), w=[B_["rb"]])
        dma("sync", rb[:, 4:36], b_re.partition_broadcast(128), w=[B_["rb"]])

        WinB = AV(0, [128, 8, 3072], BF16); B_win = [Buf("win%d" % c) for c in range(8)]
        W0 = 146 * KiB
        stg = [AV(W0 + i * 12 * KiB, [128, 3072], F32) for i in range(2)]
        B_stg = [Buf("stg%d" % i) for i in range(2)]
        for c in range(8):
            s = c % 2
            dma("sync", stg[s], w_in[c * 128:(c + 1) * 128, :], w=[B_stg[s]])
            V("tensor_copy", [B_stg[s]], [B_win[c]], eng="vector", out=WinB[:, c, :], in_=stg[s])

        QT = AV(48 * KiB, [72, 8, 2048], BF16); KT = AV(80 * KiB, [72, 8, 2048], BF16)
        Vb = AV(112 * KiB, [128, 16, 8, 65], BF16)
        convT = AV(129 * KiB, [128, NT, 4, 128], BF16)
        B_qt = [Buf("qt%d" % t) for t in range(16)]; B_kt = [Buf("kt%d" % t) for t in range(16)]
        B_vb = [Buf("vb%d" % t) for t in range(16)]; B_ct = [Buf("convT%d" % t) for t in range(NT)]
        B_qtb = [Buf("qtb%d" % t) for t in range(16)]
        V("memset", [], B_vb, eng="gpsimd", ap=Vb[:, :, :, 64:65], constant=1.0)
        for h in range(8):
            dma("sync", KT[64:72, h, :], c_blk, w=B_kt)
        V("memset", [], [B_ct[16]], eng="gpsimd", ap=convT[:, 16, :, :], constant=0.0)

        o = W0
        qf = AV(o, [128, 512], F32); o += 2 * KiB
        kf = [AV(o + i * 2 * KiB, [128, 512], F32) for i in range(2)]; o += 4 * KiB
        vf = [AV(o, [128, 512], F32)] * 2; o += 2 * KiB
        xt = [AV(o + i * 4 * KiB, [128, 1024], F32) for i in range(2)]; o += 8 * KiB
        xnb = AV(o, [128, 1024], BF16); o += 2 * KiB
        xnTg = AV(o, [128, 8, 512], BF16); o += 8 * KiB
        qb = AV(o, [128, 512], BF16); o += 1 * KiB
        kb = AV(o, [128, 512], BF16); o += 1 * KiB
        rtq = [AV(o + i * 256, [128, 8, 8], F32) for i in range(4)]; o += 1 * KiB
        rtk = [AV(o + i * 256, [128, 8, 8], F32) for i in range(4)]; o += 1 * KiB
        Hs = AV(o, [128, 512], F32); o += 2 * KiB
        ctmp = AV(o, [128, 512], F32); o += 2 * KiB
        ug = AV(o, [128, 4, 514], F32); o += 8224 + 32
        cst = [AV(o + i * 512, [128, 128], F32) for i in range(2)]; o += 1 * KiB
        assert o <= ARENA, o
        Bw = {n: Buf(n) for n in "xnb xnTg qf qb kb rtq rtk Hs ctmp ug".split()}
        B_xt = [Buf("xt0"), Buf("xt1")]; B_kf = [Buf("kf0"), Buf("kf1")]; B_vf = [Buf("vf0")] * 2
        B_cst = [Buf("cst0"), Buf("cst1")]
        for b_ in list(Bw.values()) + B_xt + B_kf + B_vf[:1] + B_cst:
            P.alias(b_, B_stg)
        V("memset", [], [Bw["ug"]], eng="gpsimd", ap=ug[:, :, 0:2], constant=0.0)

        def load_x(t):
            s = t % 2
            if t < 16:
                dma("sync", xt[s], xp[t * 128:(t + 1) * 128, :], w=[B_xt[s]])
            else:
                V("memset", [], [B_xt[s]], eng="gpsimd", ap=xt[s], constant=0.0)
                dma("sync", xt[s][0:4, :], xs, w=[B_xt[s]])
            dma("sync", cst[s], c_cs[t], w=[B_cst[s]])

        def rmsnorm_stats(src, col, rbufs):
            act(junk, src, AF.Square, rbufs, B_junkl + [B_ssq[col]], accum_out=ssq[:, col:col + 1])
            act(rt[:, col:col + 1], ssq[:, col:col + 1], AF.Sqrt, [B_ssq[col], B_["epsb"]], [B_ssq[col]],
                scale=1.0 / 1024.0, bias=epsb[:, 0:1])
            V("reciprocal", [B_ssq[col]], [B_ssq[col]], out=rstd[:, col:col + 1], in_=rt[:, col:col + 1])

        junk = AV(W0 + 30 * KiB, [128, 1024], F32); B_junkl = [Bw["Hs"], Bw["ctmp"]]
        us = ST("us", [128, 4, 4], F32); B_us = Buf("us")
        cs4 = ST("cs4", [128, 4, 4], F32); B_cs4 = Buf("cs4")
        prevT = ST("prevT", [128, 4, 4, 2], F32); B_prevT = Buf("prevT")
        csT = ST("csT", [128, 4, 4, 2], F32); B_csT = Buf("csT")
        misc8 = ST("misc8", [8, 512], F32); B_misc8 = Buf("misc8")
        cpo = misc8[0:2, :]; sc8 = misc8; cso8 = misc8
        B_cpo = B_sc8 = B_cso8 = B_misc8

        def rope(tf, tmps, cs_t, Bt, Btmp, Bcs, eng):
            v = tf.rearrange("p (h d) -> p h d", h=8)
            x1 = v[:, :, 0:8]; x2 = v[:, :, 8:16]
            cosv = cs_t[:, 0:64].rearrange("p (h d) -> p h d", h=8); sinv = cs_t[:, 64:128].rearrange("p (h d) -> p h d", h=8)
            t1, t2, t3, t4 = tmps
            V("tensor_tensor", [Bt, Bcs], [Btmp], eng=eng, out=t1, in0=x1, in1=cosv, op=ALU.mult)
            V("tensor_tensor", [Bt, Bcs], [Btmp], eng=eng, out=t2, in0=x2, in1=sinv, op=ALU.mult)
            V("tensor_tensor", [Bt, Bcs], [Btmp], eng=eng, out=t3, in0=x2, in1=cosv, op=ALU.mult)
            V("tensor_tensor", [Bt, Bcs], [Btmp], eng=eng, out=t4, in0=x1, in1=sinv, op=ALU.mult)
            V("tensor_tensor", [Btmp], [Bt], eng=eng, out=x1, in0=t1, in1=t2, op=ALU.subtract)
            V("tensor_tensor", [Btmp], [Bt], eng=eng, out=x2, in0=t3, in1=t4, op=ALU.add)

        pTx = bank(0).bitcast(BF16).rearrange("p (a b) -> p a b", a=8)
        pTqk = bank(4).bitcast(BF16).rearrange("p (a b) -> p a b", a=8)

        load_x(0)
        load_x(1)
        groups = [[0, 1, 2, 3], [4, 5, 6, 7], [8, 9, 10, 11], [12, 13, 14, 15], [16]]

        def FE(t, tt):
            s = t % 2
            rmsnorm_stats(xt[s], t, [B_xt[s]])
            V("scalar_tensor_tensor", [B_xt[s], B_ssq[t], B_["gbc"]], [Bw["xnb"]], out=xnb, in0=xt[s],
              scalar=rstd[:, t:t + 1], in1=gbc[:], op0=ALU.mult, op1=ALU.mult)
            for c in range(8):
                tr(pTx[:, c, :], xnb[:, c * 128:(c + 1) * 128], identb[:], [Bw["xnb"], B_["identb"]], [bkB[0]])
            act(xnTg[:, :, tt * 128:(tt + 1) * 128], pTx, AF.Copy, [bkB[0]], [B_xs[tt]])

        B_xs = [Buf("xnTg%d" % i) for i in range(4)]
        for b_ in B_xs:
            P.alias(b_, B_stg)
        for g, tiles in enumerate(groups):
            ncol = 128 * len(tiles)
            for tt, t in enumerate(tiles):
                s = t % 2
                if tt == 0:
                    FE(t, tt)
                if tt + 1 < len(tiles):
                    FE(tiles[tt + 1], tt + 1)
                if False:
                    rmsnorm_stats(xt[s], t, [B_xt[s]])
                for j in range(3):
                    for c in range(8):
                        mm(bank(1 + j), xnTg[:, c, tt * 128:(tt + 1) * 128], WinB[:, c, j * 512:(j + 1) * 512],
                           c == 0, c == 7, [B_xs[tt], B_win[c]], [bkB[1 + j]])
                tq, tk, tv = qf, kf[s], vf[s]
                Bq, Bk, Bv = Bw["qf"], B_kf[s], B_vf[s]
                act(tq, bank(1), AF.Copy, [bkB[1]], [Bq], scale=0.125)
                act(tk, bank(2), AF.Copy, [bkB[2]], [Bk])
                act(tv, bank(3), AF.Copy, [bkB[3]], [Bv])
                rope(tq, rtq, cst[s], Bq, Bw["rtq"], B_cst[s], "vector")
                rope(tk, rtk, cst[s], Bk, Bw["rtk"], B_cst[s], "gpsimd")
                if t < 16:
                    dma("sync", kp[t * 128:(t + 1) * 128, :], tk, r=[Bk])
                    dma("sync", vp[t * 128:(t + 1) * 128, :], tv, r=[Bv])
                    V("tensor_copy", [Bv], [B_vb[t]], eng="gpsimd", out=Vb[:, t, :, 0:64],
                      in_=tv.rearrange("p (h d) -> p h d", h=8))
                    V("tensor_copy", [Bq], [Bw["qb"]], out=qb, in_=tq)
                    V("tensor_copy", [Bk], [Bw["kb"]], eng="gpsimd", out=kb, in_=tk)
                    for h in range(8):
                        tr(pTqk[0:64, h, :], qb[:, h * 64:(h + 1) * 64], identb[:], [Bw["qb"], B_["identb"]], [bkB[4]])
                    act(QT[0:64, :, t * 128:(t + 1) * 128], pTqk[0:64, :, :], AF.Copy, [bkB[4]], [B_qt[t]])
                    for h in range(8):
                        tr(pTqk[0:64, h, :], kb[:, h * 64:(h + 1) * 64], identb[:], [Bw["kb"], B_["identb"]], [bkB[4]])
                    act(KT[0:64, :, t * 128:(t + 1) * 128], pTqk[0:64, :, :], AF.Copy, [bkB[4]], [B_kt[t]])
                else:
                    dma("sync", ks, tk[0:4, :], r=[Bk])
                    dma("sync", vs, tv[0:4, :], r=[Bv])
                if t + 2 < NT:
                    load_x(t + 2)
            for ct in range(4):
                def colsW(j):
                    return slice(1536 + j * 512 + ct * 128, 1536 + j * 512 + (ct + 1) * 128)
                for c in range(8):
                    mm(bank(5)[:, 0:ncol], WinB[:, c, colsW(2)], xnTg[:, c, 0:ncol], c == 0, c == 7, B_xs + [B_win[c]], [bkB[5]])
                for c in range(8):
                    mm(bank(6)[:, 0:ncol], WinB[:, c, colsW(1)], xnTg[:, c, 0:ncol], c == 0, c == 7, B_xs + [B_win[c]], [bkB[6]])
                act(Hs[:, 0:ncol], bank(5)[:, 0:ncol], AF.Copy, [bkB[5]], [Bw["Hs"]])
                for c in range(8):
                    mm(bank(5)[:, 0:ncol], WinB[:, c, colsW(0)], xnTg[:, c, 0:ncol], c == 0, c == 7, B_xs + [B_win[c]], [bkB[5]])
                w0 = convw[:, 0 * 4 + ct:0 * 4 + ct + 1]; w1 = convw[:, 4 + ct:4 + ct + 1]; w2 = convw[:, 8 + ct:8 + ct + 1]
                if g < 4:
                    V("tensor_tensor", [bkB[6], Bw["Hs"]], [Bw["ug"]], out=ug[:, ct, 2:514], in0=bank(6), in1=Hs, op=ALU.mult)
                    V("tensor_scalar", [Bw["ug"], B_["convw"]], [Bw["ctmp"]], out=ctmp, in0=ug[:, ct, 2:514], scalar1=w2, scalar2=None, op0=ALU.mult)
                    V("scalar_tensor_tensor", [Bw["ug"], Bw["ctmp"], B_["convw"]], [Bw["ctmp"]], out=ctmp, in0=ug[:, ct, 1:513], scalar=w1, in1=ctmp, op0=ALU.mult, op1=ALU.add)
                    V("scalar_tensor_tensor", [Bw["ug"], Bw["ctmp"], B_["convw"]], [Bw["ctmp"]], out=ctmp, in0=ug[:, ct, 0:512], scalar=w0, in1=ctmp, op0=ALU.mult, op1=ALU.add)
                    V("tensor_tensor", [bkB[5], Bw["ctmp"]], [B_ct[t_] for t_ in tiles],
                      out=convT[:, tiles[0]:tiles[0] + 4, ct, :], in0=bank(5).rearrange("p (a b) -> p a b", a=4),
                      in1=ctmp.rearrange("p (a b) -> p a b", a=4), op=ALU.mult)
                    V("tensor_copy", [Bw["ug"]], [Bw["ug"]], out=ug[:, ct, 0:2], in_=ug[:, ct, 512:514])
                else:
                    V("tensor_tensor", [bkB[6], Bw["Hs"]], [B_us], out=us[:, ct, :], in0=bank(6)[:, 0:4], in1=Hs[:, 0:4], op=ALU.mult)
                    V("tensor_scalar", [B_us, B_["convw"]], [B_cs4], out=cs4[:, ct, :], in0=us[:, ct, :], scalar1=w2, scalar2=None, op0=ALU.mult)
                    V("scalar_tensor_tensor", [B_prevT, B_cs4, B_["convw"]], [B_cs4], out=cs4[:, ct, :], in0=prevT[:, ct, :, 1], scalar=w1, in1=cs4[:, ct, :], op0=ALU.mult, op1=ALU.add)
                    V("scalar_tensor_tensor", [B_prevT, B_cs4, B_["convw"]], [B_cs4], out=cs4[:, ct, :], in0=prevT[:, ct, :, 0], scalar=w0, in1=cs4[:, ct, :], op0=ALU.mult, op1=ALU.add)
                    V("tensor_tensor", [bkB[5], B_cs4], [B_ct[16]], out=convT[:, 16, ct, 0:4], in0=bank(5)[:, 0:4], in1=cs4[:, ct, :], op=ALU.mult)
            if g == 3:
                for ct in range(4):
                    tr(bank(0)[0:2, ct * 128:(ct + 1) * 128], ug[:, ct, 0:2], identf[:], [Bw["ug"], B_["identf"]], [bkB[0]])
                act(cpo, bank(0)[0:2, :], AF.Copy, [bkB[0]], [B_cpo])
                dma("sync", cp, cpo, r=[B_cpo])
                dma("sync", sc8[:], sc, w=[B_sc8])
                for ct in range(4):
                    tr(bank(0)[:, 512 - 32 + ct * 8: 512 - 32 + (ct + 1) * 8], sc8[:, ct * 128:(ct + 1) * 128], identf[0:8, 0:8], [B_sc8, B_["identf"]], [bkB[0]])
                act(prevT[:].rearrange("p a b c -> p (a b c)"), bank(0)[:, 480:512], AF.Copy, [bkB[0]], [B_prevT])
            if g == 4:
                V("tensor_copy", [B_prevT], [B_csT], out=csT[:, :, :, 0], in_=prevT[:, :, :, 1])
                V("tensor_copy", [B_us], [B_csT], out=csT[:, :, :, 1], in_=us[:, :, :])
                for ct in range(4):
                    tr(bank(0)[0:8, ct * 128:(ct + 1) * 128], csT[:, ct, :, :].rearrange("p a b -> p (a b)"), identf[:], [B_csT, B_["identf"]], [bkB[0]])
                act(cso8[:], bank(0)[0:8, :], AF.Copy, [bkB[0]], [B_cso8])
                dma("sync", cso, cso8[:], r=[B_cso8])

        all_p1 = list(Bw.values()) + B_xt + B_kf + B_vf[:1] + B_cst + B_win + B_xs
        ATT = AV(0, [128, NT, 8, 128], BF16)
        B_att = [Buf("att%d" % t) for t in range(NT)]
        for b_ in B_att:
            P.alias(b_, B_win)
        Pb = [AV(34 * KiB + i * KiB, [128, 512], BF16) for i in range(4)]; B_P = [Buf("P%d" % i) for i in range(4)]
        osb = [AV(38 * KiB + i * 2 * KiB, [65, 512], F32) for i in range(2)]; B_osb = [Buf("osb%d" % i) for i in range(2)]
        biasp = [AV(42 * KiB + i * 1152, [128, 8, 72], BF16) for i in range(2)]; B_bp = [Buf("bp%d" % i) for i in range(2)]
        gct = [AV(45 * KiB + i * 768, [128, 192], F32) for i in range(2)]; B_gct = [Buf("gct%d" % i) for i in range(2)]
        gm = AV(46 * KiB + 512, [128, 64], F32); top8 = AV(46 * KiB + 768, [128, 8, 8], F32); sel = AV(47 * KiB, [128, 64], F32)
        B_gm = Buf("gm"); B_top8 = Buf("top8"); B_sel = Buf("sel")
        for b_ in B_P + B_osb + B_bp + B_gct + [B_gm, B_top8, B_sel]:
            P.alias(b_, B_win)
        V("memset", [], [B_att[16]], eng="gpsimd", ap=ATT[0:64, 16, :, :], constant=0.0)
        for i in range(2):
            V("memset", [], [B_bp[i]], eng="gpsimd", ap=biasp[i], constant=0.0)
        SAMPLE_BUFS = []

        def sbuf_(name, olds=None):
            b_ = Buf(name)
            P.alias(b_, (all_p1 if olds is None else olds) + SAMPLE_BUFS)
            SAMPLE_BUFS.append(b_)
            return b_
        SB = 154 * KiB
        G = [AV(SB + i * 8 * KiB, [128, 2048], F32) for i in range(2)]
        accs = [AV(SB + 16 * KiB + i * 8 * KiB, [128, 2048], F32) for i in range(2)]
        B_G = [sbuf_("G0"), sbuf_("G1")]; B_accs = [sbuf_("accs0"), sbuf_("accs1")]
        oA = [186 * KiB]

        def smallA(shape, dt):
            isz = 2 if dt == BF16 else 4
            n = 1
            for x_ in shape[1:]:
                n *= x_
            ap = AV(oA[0], shape, dt)
            oA[0] += (n * isz + 31) // 32 * 32
            assert oA[0] <= ARENA
            return ap
        ptI = smallA([128, 2], I32); ptF = smallA([128, 2], F32); idxf = smallA([128, 64], F32); idxI = smallA([128, 64], I32)
        ptb8 = smallA([8, 256], I32); PTf = smallA([8, 256], F32)
        B_s0 = sbuf_("s0")
        for pr in range(2):
            dma("sync", ptI[:, pr:pr + 1], pt[2 * pr:2 * pr + 2, :].rearrange("b (g o) -> (b g) o", o=1), w=[B_s0])
        dma("sync", ptb8, pt.rearrange("b g -> (b g)").partition_broadcast(8), w=[B_s0])
        V("tensor_copy", [B_s0], [B_s0], out=ptF, in_=ptI)
        V("tensor_copy", [B_s0], [B_s0], out=PTf, in_=ptb8)
        V("tensor_scalar", [B_s0], [B_s0], out=ptF, in0=ptF, scalar1=32.0, scalar2=None, op0=ALU.mult)
        for pr in range(2):
            V("tensor_scalar", [B_s0, B_["cf"]], [B_s0], out=idxf[:, pr * 32:(pr + 1) * 32], in0=chunkid, scalar1=ptF[:, pr:pr + 1], scalar2=None, op0=ALU.add)
        V("tensor_scalar", [B_s0], [B_s0], out=idxf, in0=idxf, scalar1=0.0, scalar2=81919.0, op0=ALU.max, op1=ALU.min)
        V("tensor_copy", [B_s0], [B_s0], out=idxI, in_=idxf)

        def s1_steps():
            chunks = [(pr, c) for pr in range(2) for c in range(32)]

            def gather(k_):
                pr, c = chunks[k_]
                gi_ = k_ % 2
                col = pr * 32 + c
                P.dma("gpsimd", lambda e, gi_=gi_, col=col: e.indirect_dma_start(
                    out=G[gi_], out_offset=None, in_=ckc[:, :], in_offset=bass.IndirectOffsetOnAxis(ap=idxI[:, col:col + 1], axis=0)),
                    [B_s0], [B_G[gi_]])

            def accum(k_):
                pr, c = chunks[k_]
                gi_ = k_ % 2
                if c == 0:
                    V("tensor_copy", [B_G[gi_]], [B_accs[pr]], out=accs[pr], in_=G[gi_])
                else:
                    V("tensor_tensor", [B_G[gi_], B_accs[pr]], [B_accs[pr]], out=accs[pr], in0=accs[pr], in1=G[gi_], op=ALU.add)
            gather(0)
            for k_ in range(len(chunks)):
                if k_ + 1 < len(chunks):
                    gather(k_ + 1)
                accum(k_)
                yield
        s1gen = s1_steps()

        def s1_advance(n):
            for _ in range(n):
                try:
                    next(s1gen)
                except StopIteration:
                    return
        for h in range(8):
            V("tensor_reduce", B_kt, [B_["ksum"]], out=ksum[:, h * 8:(h + 1) * 8],
              in_=KT[0:64, h, :].rearrange("p (b k) -> p b k", b=8), axis=AX.X, op=ALU.add)
        V("tensor_copy", [B_["ksum"]], [B_["ksumhi"]], out=ksumhi[:], in_=ksum[:])
        V("tensor_tensor", [B_["ksum"], B_["ksumhi"]], [B_["ksumlo"]], out=ksumlo[:], in0=ksum[:], in1=ksumhi[:], op=ALU.subtract)
        pB = bank(7).bitcast(BF16).rearrange("p (a b) -> p a b", a=8)
        for t in range(16):
            s = t % 2
            s1_advance(4)
            dma("sync", gct[s], c_g[t], w=[B_gct[s]])
            for h in range(8):
                mm(bank(6)[:, h * 8:(h + 1) * 8], QT[0:64, h, t * 128:(t + 1) * 128], ksumhi[:, h * 8:(h + 1) * 8], True, False,
                   [B_qt[t], B_["ksumhi"]], [bkB[6]])
                mm(bank(6)[:, h * 8:(h + 1) * 8], QT[0:64, h, t * 128:(t + 1) * 128], ksumlo[:, h * 8:(h + 1) * 8], False, True,
                   [B_qt[t], B_["ksumlo"]], [bkB[6]])
            V("tensor_tensor", [bkB[6], B_gct[s]], [B_gm], out=gm, in0=bank(6)[:, 0:64], in1=gct[s][:, 0:64], op=ALU.add)
            for h in range(8):
                V("max", [B_gm], [B_top8], out=top8[:, h, :], in_=gm[:, h * 8:(h + 1) * 8])
            for h in range(8):
                V("tensor_scalar", [B_gm, B_top8], [B_sel], out=sel[:, h * 8:(h + 1) * 8], in0=gm[:, h * 8:(h + 1) * 8],
                  scalar1=top8[:, h, 2:3], scalar2=None, op0=ALU.is_ge)
            V("tensor_tensor", [B_sel, B_gct[s]], [B_sel], out=sel, in0=sel, in1=gct[s][:, 64:128], op=ALU.mult)
            V("tensor_tensor", [B_sel, B_gct[s]], [B_sel], out=sel, in0=sel, in1=gct[s][:, 128:192], op=ALU.add)
            V("tensor_scalar", [B_sel], [B_bp[s]], out=biasp[s][:, :, 64:72], in0=sel.rearrange("p (h j) -> p h j", h=8),
              scalar1=-1.0, scalar2=BIG, op0=ALU.add, op1=ALU.mult)
            for h in range(8):
                tr(pB[0:72, h, :], biasp[s][:, h, :], identb[:], [B_bp[s], B_["identb"]], [bkB[7]])
            act(QT[64:72, :, t * 128:(t + 1) * 128], pB[64:72, :, :], AF.Copy, [bkB[7]], [B_qtb[t]])


        def s2_steps():
            def sb_view(off_kib, shape, dt):
                return AV(int(off_kib * KiB), shape, dt)
            pagesum = [sb_view(154 + 2 * i, [128, 512], F32) for i in range(2)]
            B_ps_ = sbuf_("pagesum", [])
            for pr in range(2):
                V("tensor_reduce", [B_accs[pr]], [B_ps_], out=pagesum[pr], in_=accs[pr].rearrange("p (pos f) -> p f pos", pos=4), axis=AX.X, op=ALU.add)
            qbcs = sb_view(158, [64, 512], F32); prod = sb_view(160, [64, 512], F32)
            oB = [162 * KiB]

            def smallB(shape, dt):
                n = 1
                for x_ in shape[1:]:
                    n *= x_
                ap = AV(oB[0], shape, dt)
                oB[0] += (n * 4 + 31) // 32 * 32
                assert oB[0] <= 166 * KiB
                return ap
            gate2 = smallB([64, 16], F32); gateT = smallB([8, 128], F32); top8s = smallB([8, 4, 8], F32); oh = smallB([8, 32], F32)
            junk8 = smallB([8, 32], F32); physf = smallB([8, 24], F32); Dm = smallB([8, 8, 24], F32)
            idxf2 = smallB([128, 192], F32); idxI2 = smallB([128, 192], I32)
            B_s2 = sbuf_("s2", [])
            Bqs = Bw["qf"]; Bks = B_kf[0]; Bvs = B_vf[0]
            for pr in range(2):
                mm(bank(7)[0:64, :], cf[:, 0:64], pagesum[pr], True, True, [B_ps_, B_["cf"]], [bkB[7]])
                mm(bank(6)[0:64, :], cf[0:4, 288 + 64 * pr:288 + 64 * (pr + 1)], qf[0:4, :], True, True, [Bqs, B_["cf"]], [bkB[6]])
                act(qbcs, bank(6)[0:64, :], AF.Copy, [bkB[6]], [B_s2])
                V("tensor_tensor", [bkB[7], B_s2], [B_s2], out=prod, in0=bank(7)[0:64, :], in1=qbcs, op=ALU.mult)
                V("tensor_reduce", [B_s2], [B_s2], out=gate2[:, pr * 8:(pr + 1) * 8], in_=prod.rearrange("p (h d) -> p h d", h=8), axis=AX.X, op=ALU.add)
                tr(bank(7)[0:8, pr * 64:(pr + 1) * 64], gate2[:, pr * 8:(pr + 1) * 8], identf[0:64, 0:64], [B_s2, B_["identf"]], [bkB[7]])
                act(gateT[:, pr * 64:(pr + 1) * 64], bank(7)[0:8, pr * 64:(pr + 1) * 64], AF.Copy, [bkB[7]], [B_s2])
            S2 = [B_s2]
            for b in range(4):
                V("max", S2, S2, out=top8s[:, b, :], in_=gateT[:, b * 32:(b + 1) * 32])
                for r_ in range(3):
                    V("tensor_scalar", S2, S2, out=oh, in0=gateT[:, b * 32:(b + 1) * 32], scalar1=top8s[:, b, r_:r_ + 1], scalar2=None, op0=ALU.is_equal)
                    for hf in range(2):
                        colp = (b * 3 + r_) * 2 + hf
                        V("tensor_tensor", S2 + [B_s0], S2, out=junk8, in0=oh,
                          in1=PTf[:, b * 64:(b + 1) * 64].rearrange("p (j t) -> p j t", t=2)[:, :, hf], op=ALU.mult)
                        V("tensor_reduce", S2, S2, out=physf[:, colp:colp + 1], in_=junk8, axis=AX.X, op=ALU.add)
            for h in range(8):
                V("tensor_scalar", S2 + [B_["cf"]], S2, out=Dm[:, h, :], in0=physf, scalar1=cf[0:8, 928 + h:929 + h], scalar2=None, op0=ALU.mult)
            mm(bank(7)[:, 0:192], cf[0:8, 940:1068], Dm.rearrange("p a b -> p (a b)"), True, True, S2 + [B_["cf"]], [bkB[7]])
            V("scalar_tensor_tensor", [bkB[7], B_["cf"]], S2, out=idxf2, in0=bank(7)[:, 0:192], scalar=1024.0, in1=poshc, op0=ALU.mult, op1=ALU.add)
            V("tensor_scalar", S2, S2, out=idxf2, in0=idxf2, scalar1=0.0, scalar2=2621439.0, op0=ALU.max, op1=ALU.min)
            V("tensor_copy", S2, S2, out=idxI2, in_=idxf2)
            yield
            NSET = 4
            Ks = [sb_view(166 + 6 * i, [128, 12, 64], F32) for i in range(NSET)]
            Vs = [sb_view(169 + 6 * i, [128, 12, 64], F32) for i in range(NSET)]
            B_ks = [sbuf_("ks%d" % i, []) for i in range(NSET)]; B_vs = [sbuf_("vs%d" % i, []) for i in range(NSET)]
            qbu = [sb_view(154 + 0.5 * i, [128, 128], F32) for i in range(2)]; B_qbu = [sbuf_("qbu0", []), sbuf_("qbu1", [])]
            oC = [158 * KiB]

            def smallC(shape):
                n = 1
                for x_ in shape[1:]:
                    n *= x_
                ap = AV(oC[0], shape, F32)
                oC[0] += (n * 4 + 31) // 32 * 32
                assert oC[0] <= 162 * KiB
                return ap
            STs = smallC([128, 192]); Es = smallC([128, 192]); denp = smallC([64, 32]); sself = smallC([4, 8])
            eself = smallC([4, 8]); D2 = smallC([4, 32]); ebvt = smallC([64, 96]); numt = smallC([64, 32]); dent = smallC([64, 32])
            B_s6 = sbuf_("s6", [])
            S6 = [B_s6]
            units = [(b, hp) for b in range(4) for hp in range(4)]

            def u_gather(u):
                b, hp = units[u]
                st_ = u % NSET
                for jj in range(12):
                    h = 2 * hp + jj // 6
                    col = h * 24 + b * 6 + (jj % 6)
                    P.dma("gpsimd", lambda e, jj=jj, col=col, st_=st_: e.indirect_dma_start(
                        out=Ks[st_][:, jj, :], out_offset=None, in_=ck[:, :], in_offset=bass.IndirectOffsetOnAxis(ap=idxI2[:, col:col + 1], axis=0)),
                        S2, [B_ks[st_]])
                    P.dma("gpsimd", lambda e, jj=jj, col=col, st_=st_: e.indirect_dma_start(
                        out=Vs[st_][:, jj, :], out_offset=None, in_=cv[:, :], in_offset=bass.IndirectOffsetOnAxis(ap=idxI2[:, col:col + 1], axis=0)),
                        S2, [B_vs[st_]])

            def u_compute(u):
                b, hp = units[u]
                st_ = u % NSET
                qs_ = u % 2
                mm(bank(6)[:, 0:128], cf[0:4, 416 + 128 * b:416 + 128 * (b + 1)], qf[0:4, hp * 128:(hp + 1) * 128], True, True, [Bqs, B_["cf"]], [bkB[6]])
                act(qbu[qs_], bank(6)[:, 0:128], AF.Copy, [bkB[6]], [B_qbu[qs_]])
                for jj in range(12):
                    hh = jj // 6
                    V("tensor_tensor", [B_ks[st_], B_qbu[qs_]], [B_ks[st_]], out=Ks[st_][:, jj, :], in0=Ks[st_][:, jj, :],
                      in1=qbu[qs_][:, hh * 64:(hh + 1) * 64], op=ALU.mult)
                c0 = b * 48 + hp * 12
                V("tensor_reduce", [B_ks[st_]], S6, out=STs[:, c0:c0 + 12], in_=Ks[st_], axis=AX.X, op=ALU.add)
                act(Es[:, c0:c0 + 12], STs[:, c0:c0 + 12], AF.Exp, S6, S6)
                for hh in range(2):
                    h = 2 * hp + hh
                    for s6 in range(6):
                        cc = c0 + hh * 6 + s6
                        mm(bank(7)[0:64, 480 + b * 8 + h:480 + b * 8 + h + 1], Vs[st_][:, hh * 6 + s6, :], Es[:, cc:cc + 1], s6 == 0, s6 == 5, S6 + [B_vs[st_]], [bkB[7]])
            LAG = NSET - 1
            for u in range(16 + LAG):
                if u < 16:
                    u_gather(u)
                if u - LAG >= 0:
                    u_compute(u - LAG)
                yield
            mm(bank(7)[0:64, 0:192], cf[:, 940:1004], Es, True, True, S6 + [B_["cf"]], [bkB[7]])
            V("tensor_reduce", [bkB[7]], S6, out=denp, in_=bank(7)[0:64, 0:192].rearrange("p (g s) -> p g s", s=6), axis=AX.X, op=ALU.add)
            prodqk = misc8[0:4, :]
            V("tensor_tensor", [Bqs, Bks, B_misc8], [B_misc8], out=prodqk, in0=qf[0:4, :], in1=kf[0][0:4, :], op=ALU.mult)
            V("tensor_reduce", [B_misc8], S6, out=sself, in_=prodqk.rearrange("p (h d) -> p h d", h=8), axis=AX.X, op=ALU.add)
            act(eself, sself, AF.Exp, S6, S6)
            for b in range(4):
                V("tensor_scalar", S6 + [B_["cf"]], S6, out=D2[:, b * 8:(b + 1) * 8], in0=eself, scalar1=cf[0:4, 936 + b:937 + b], scalar2=None, op0=ALU.mult)
            mm(bank(6)[0:64, 0:32], cf[0:4, 940:1004], D2, True, True, S6 + [B_["cf"]], [bkB[6]])
            for h in range(8):
                tr(bank(6)[0:64, 64 + h * 4:64 + h * 4 + 4], vf[0][0:4, h * 64:(h + 1) * 64], identf[0:4, 0:4], [Bvs, B_["identf"]], [bkB[6]])
            act(ebvt, bank(6)[0:64, 0:96], AF.Copy, [bkB[6]], S6)
            ebc3 = ebvt[:, 0:32].rearrange("p (b h) -> p b h", b=4)
            vT3 = ebvt[:, 64:96].rearrange("p (h b) -> p b h", b=4)
            V("tensor_tensor", S6, S6, out=numt.rearrange("p (b h) -> p b h", b=4), in0=vT3, in1=ebc3, op=ALU.mult)
            V("tensor_tensor", S6 + [bkB[7]], S6, out=numt, in0=numt, in1=bank(7)[0:64, 480:512], op=ALU.add)
            V("tensor_tensor", S6, S6, out=dent, in0=denp, in1=ebvt[:, 0:32], op=ALU.add)
            V("reciprocal", S6, S6, out=dent, in_=dent)
            V("tensor_tensor", S6, [B_att[16]], out=ATT[0:64, 16, :, 0:4], in0=numt.rearrange("p (b h) -> p h b", b=4),
              in1=dent.rearrange("p (b h) -> p h b", b=4), op=ALU.mult)
            yield
        s2gen = s2_steps()

        def s2_advance(n):
            for _ in range(n):
                try:
                    next(s2gen)
                except StopIteration:
                    return
        steps = []
        for h in range(8):
            for c in range(4):
                for kt in range(4 * c + 4):
                    steps.append((h, c, kt))
        LOOK = 2
        hc_ob = {}
        for si in range(len(steps) + LOOK):
            if si < len(steps):
                h, c, kt = steps[si]
                if kt == 0:
                    it_ = h * 4 + c
                    s1_advance(4)
                    hc_ob[(h, c)] = 3 + (len(hc_ob) % 2)
                i = max(0, kt - 4 * c)
                sb = si % 3
                pi = si % 4
                qr = [B_qt[4 * c + i_] for i_ in range(4)] + [B_qtb[4 * c + i_] for i_ in range(4)]
                mm(bank(sb)[:, i * 128:512], KT[0:72, h, kt * 128:(kt + 1) * 128], QT[0:72, h, c * 512 + i * 128:(c + 1) * 512],
                   True, True, [B_kt[kt]] + qr, [bkB[sb]])
                act(Pb[pi][:, i * 128:512], bank(sb)[:, i * 128:512], AF.Exp, [bkB[sb]], [B_P[pi]])
                if kt >= 4 * c:
                    V("tensor_tensor", [B_P[pi], B_["tri"]], [B_P[pi]], out=Pb[pi][:, i * 128:(i + 1) * 128],
                      in0=Pb[pi][:, i * 128:(i + 1) * 128], in1=tri[:], op=ALU.mult)
            sj = si - LOOK
            if sj >= 0:
                h, c, kt = steps[sj]
                nkt = 4 * c + 4
                i = max(0, kt - 4 * c)
                pi = sj % 4
                ob = hc_ob[(h, c)]
                mm(bank(ob)[0:65, i * 128:512], Vb[:, kt, h, :], Pb[pi][:, i * 128:512], kt == 0, kt == nkt - 1,
                   [B_vb[kt], B_P[pi]], [bkB[ob]])
                if kt == nkt - 1:
                    oi = ob - 3
                    V("tensor_copy", [bkB[ob]], [B_osb[oi]], out=osb[oi], in_=bank(ob)[0:65, :])
                    act(osb[oi][64:65, :], osb[oi][64:65, :], AF.Ln, [B_osb[oi]], [B_osb[oi]])
                    act(osb[oi][64:65, :], osb[oi][64:65, :], AF.Exp, [B_osb[oi]], [B_osb[oi]], scale=-1.0)
                    mm(bank(5)[0:64, :], cf[64:65, 940:1004], osb[oi][64:65, :], True, True, [B_osb[oi], B_["cf"]], [bkB[5]])
                    V("tensor_tensor", [B_osb[oi], bkB[5]], [B_att[4 * c + i_] for i_ in range(4)],
                      out=ATT[0:64, 4 * c:4 * c + 4, h, :], in0=osb[oi][0:64, :].rearrange("p (a b) -> p a b", a=4),
                      in1=bank(5)[0:64, :].rearrange("p (a b) -> p a b", a=4), op=ALU.mult)
        s1_advance(1000)
        s2_advance(1000)
        att_done = B_qt + B_kt + B_vb + B_qtb
        ACC = AV(48 * KiB, [128, NT, 1024], F32); B_acc = [Buf("acc%d" % t) for t in range(NT)]
        for b_ in B_acc:
            P.alias(b_, att_done)
        WoC = AV(116 * KiB, [128, 4, 1024], BF16); B_woc = Buf("woc"); P.alias(B_woc, att_done)
        gates = AV(124 * KiB, [128, NT, 32], F32); B_gates = [Buf("gates%d" % t) for t in range(NT)]
        for b_ in B_gates:
            P.alias(b_, att_done)
        WoA = AV(146 * KiB, [64, 8, 1024], BF16); B_woa = Buf("woa")
        xr2 = [AV((162 + 4 * i) * KiB, [128, 1024], F32) for i in range(2)]; B_xr2 = [Buf("xr0"), Buf("xr1")]
        hn2 = [AV((170 + 4 * i) * KiB, [128, 1024], F32) for i in range(2)]; B_hn2 = [Buf("hn0"), Buf("hn1")]
        hnT32_2 = [AV((178 + 4 * i) * KiB, [128, 8, 128], F32) for i in range(2)]; B_hnT2 = [Buf("hnT0"), Buf("hnT1")]
        B_rs2 = []
        wstg = AV(34 * KiB, [128, 3, 1024], F32); B_wstg = Buf("wstg")
        p2_bufs = B_P + B_osb + B_bp + B_gct + [B_gm, B_top8, B_sel]
        p3_w = [B_woa] + B_xr2 + B_hn2 + B_hnT2 + B_rs2
        for b_ in p3_w + [B_wstg]:
            P.alias(b_, all_p1 + SAMPLE_BUFS + p2_bufs)
        for hg in ((0, 1, 2), (3, 4, 5), (6, 7)):
            n = len(hg)
            dma("sync", wstg[0:64, 0:n, :], w_out[hg[0] * 64:(hg[-1] + 1) * 64, :].rearrange("(h r) n -> r h n", r=64), w=[B_wstg])
            V("tensor_copy", [B_wstg], [B_woa], out=WoA[:, hg[0]:hg[0] + n, :], in_=wstg[0:64, 0:n, :])
        for cg in ((0, 1, 2), (3,)):
            n = len(cg)
            dma("sync", wstg[:, 0:n, :], w_out[512 + cg[0] * 128:512 + (cg[-1] + 1) * 128, :].rearrange("(c p) n -> p c n", p=128), w=[B_wstg])
            V("tensor_copy", [B_wstg], [B_woc], out=WoC[:, cg[0]:cg[0] + n, :], in_=wstg[:, 0:n, :])
        dma("sync", gbc[:], g_ffn.partition_broadcast(128), w=[B_["gbc"]])
        pT32 = PS[2].rearrange("p (a b) -> p a b", a=8)
        lgall = AV(186 * KiB, [128, NT, 36], F32); B_lgall = Buf("lgall")
        P.alias(B_lgall, all_p1 + SAMPLE_BUFS + p2_bufs)

        def p3_W(t):
            hps = PS[t % 2]
            Bh = [bkB[2 * (t % 2)], bkB[2 * (t % 2) + 1]]
            for half in range(2):
                for h in range(8):
                    mm(hps[:, half * 512:(half + 1) * 512], ATT[0:64, t, h, :], WoA[:, h, half * 512:(half + 1) * 512], h == 0, False,
                       [B_att[t], B_woa], Bh)
                for ct in range(4):
                    mm(hps[:, half * 512:(half + 1) * 512], convT[:, t, ct, :], WoC[:, ct, half * 512:(half + 1) * 512], False, ct == 3,
                       [B_ct[t], B_woc], Bh)
            i = t % 2
            if t < 16:
                dma("sync", xr2[i], xp[t * 128:(t + 1) * 128, :], w=[B_xr2[i]])
            else:
                V("memset", [], [B_xr2[i]], eng="gpsimd", ap=xr2[i], constant=0.0)
                dma("sync", xr2[i][0:4, :], xs, w=[B_xr2[i]])

        def p3_rest(t):
            i = t % 2
            hps = PS[i]; Bh = [bkB[2 * i], bkB[2 * i + 1]]
            hn = hn2[i]; B_hn = B_hn2[i]; hnT32 = hnT32_2[i]; B_hnT32 = B_hnT2[i]
            V("tensor_tensor", Bh + [B_xr2[i]], [B_acc[t]], out=ACC[:, t, :], in0=hps[:, :], in1=xr2[i], op=ALU.add)
            col = NT + t
            act(hn, ACC[:, t, :], AF.Square, [B_acc[t]], [B_hn, B_ssq[col]], accum_out=ssq[:, col:col + 1])
            act(rt[:, col:col + 1], ssq[:, col:col + 1], AF.Sqrt, [B_ssq[col], B_["epsb"]], [B_ssq[col]], scale=1.0 / 1024.0, bias=epsb[:, 0:1])
            V("reciprocal", [B_ssq[col]], [B_ssq[col]], out=rstd[:, col:col + 1], in_=rt[:, col:col + 1])
            V("scalar_tensor_tensor", [B_acc[t], B_ssq[col], B_["gbc"]], [B_hn], out=hn, in0=ACC[:, t, :], scalar=rstd[:, col:col + 1],
              in1=gbc[:], op0=ALU.mult, op1=ALU.mult)
            for c in range(8):
                tr(pT32[:, c, :], hn[:, c * 128:(c + 1) * 128], identf[:], [B_hn, B_["identf"]], [bkB[4], bkB[5]])
            act(hnT32, pT32, AF.Copy, [bkB[4], bkB[5]], [B_hnT32])
            V("tensor_copy", [B_hnT32], [B_att[t]], eng="gpsimd", out=ATT[:, t, :, :], in_=hnT32)
            for c in range(8):
                mm(bank(6)[:, 0:36], hnT32[:, c, :], wr[:, c, :], c == 0, c == 7, [B_hnT32, B_["wr"]], [bkB[6]])
            V("tensor_tensor", [bkB[6], B_["rb"]], [B_lgall], out=lgall[:, t, :], in0=bank(6)[:, 0:36], in1=rb[:], op=ALU.add)

        p3_W(0)
        for t in range(NT):
            if t + 1 < NT:
                p3_W(t + 1)
            p3_rest(t)

        ob_ = [162 * KiB]

        def rtile(shape):
            n = 1
            for x_ in shape[1:]:
                n *= x_
            ap = AV(ob_[0], shape, F32)
            ob_[0] += (n * 4 + 31) // 32 * 32
            assert ob_[0] <= 170 * KiB
            return ap
        B_rt = Buf("rtmp"); P.alias(B_rt, B_xr2 + B_hn2)
        Rr = [B_rt]
        m4 = rtile([128, NT]); ohg = rtile([128, NT, 4]); eg = rtile([128, NT, 4]); sg4 = rtile([128, NT]); pg = rtile([128, NT])
        leg = rtile([128, NT, 8]); tmp8 = rtile([128, NT, 8]); t8a = rtile([128, NT, 8]); dd = rtile([128, NT]); w1 = rtile([128, NT]); w2 = rtile([128, NT])
        e1 = rtile([128, NT, 8]); e2 = rtile([128, NT, 8]); gi = rtile([128, NT, 8])
        lgg = lgall[:, :, 0:4]
        lge = lgall[:, :, 4:36].rearrange("p t (g i) -> p t g i", g=4)

        def bc(ap2, n):
            return ap2.unsqueeze(2).to_broadcast([128, NT, n])
        V("tensor_reduce", [B_lgall], Rr, out=m4, in_=lgg, axis=AX.X, op=ALU.max)
        V("tensor_tensor", [B_lgall] + Rr, Rr, out=ohg, in0=lgg, in1=bc(m4, 4), op=ALU.is_ge)
        V("tensor_tensor", [B_lgall] + Rr, Rr, out=eg, in0=lgg, in1=bc(m4, 4), op=ALU.subtract)
        act(eg, eg, AF.Exp, Rr, Rr)
        V("tensor_reduce", Rr, Rr, out=sg4, in_=eg, axis=AX.X, op=ALU.add)
        V("reciprocal", Rr, Rr, out=pg, in_=sg4)
        for g_ in range(4):
            if g_ == 0:
                V("tensor_tensor", [B_lgall] + Rr, Rr, out=leg, in0=lge[:, :, 0, :], in1=bc(ohg[:, :, 0], 8), op=ALU.mult)
            else:
                V("tensor_tensor", [B_lgall] + Rr, Rr, out=tmp8, in0=lge[:, :, g_, :], in1=bc(ohg[:, :, g_], 8), op=ALU.mult)
                V("tensor_tensor", Rr, Rr, out=leg, in0=leg, in1=tmp8, op=ALU.add)
        for t in range(NT):
            V("max", Rr, Rr, out=t8a[:, t, :], in_=leg[:, t, :])
        V("tensor_tensor", Rr, Rr, out=dd, in0=t8a[:, :, 1], in1=t8a[:, :, 0], op=ALU.subtract)
        act(w2, dd, AF.Exp, Rr, Rr)
        V("tensor_scalar", Rr, Rr, out=w1, in0=w2, scalar1=1.0, scalar2=None, op0=ALU.add)
        V("reciprocal", Rr, Rr, out=w1, in_=w1)
        V("tensor_tensor", Rr, Rr, out=w2, in0=w2, in1=w1, op=ALU.mult)
        V("tensor_tensor", Rr, Rr, out=w1, in0=w1, in1=pg, op=ALU.mult)
        V("tensor_tensor", Rr, Rr, out=w2, in0=w2, in1=pg, op=ALU.mult)
        V("tensor_tensor", Rr, Rr, out=e1, in0=leg, in1=bc(t8a[:, :, 0], 8), op=ALU.is_equal)
        V("tensor_tensor", Rr, Rr, out=e1, in0=e1, in1=bc(w1, 8), op=ALU.mult)
        V("tensor_tensor", Rr, Rr, out=e2, in0=leg, in1=bc(t8a[:, :, 1], 8), op=ALU.is_equal)
        V("tensor_tensor", Rr, Rr, out=e2, in0=e2, in1=bc(w2, 8), op=ALU.mult)
        V("tensor_tensor", Rr, Rr, out=gi, in0=e1, in1=e2, op=ALU.add)
        gates4 = gates.rearrange("p t (g i) -> p t g i", g=4)
        for g_ in range(4):
            V("tensor_tensor", Rr, B_gates, out=gates4[:, :, g_, :], in0=gi, in1=bc(ohg[:, :, g_], 8), op=ALU.mult)

        p3_done = p3_w + [B_wstg, B_woc, B_lgall, B_rt] + B_ct
        WB0 = 129 * KiB
        NSTG = 3
        estg = [AV(34 * KiB + i * 4 * KiB, [128, 1024], F32) for i in range(NSTG)]; B_estg = [Buf("estg%d" % i) for i in range(NSTG)]
        Wgu = [AV(WB0 + i * 12 * KiB, [128, 8, 512], BF16) for i in range(4)]
        Wd = [AV(WB0 + i * 12 * KiB + 8 * KiB, [128, 2, 1024], BF16) for i in range(4)]
        B_wgu = [Buf("wgu%d" % i) for i in range(4)]; B_wd = [Buf("wd%d" % i) for i in range(4)]
        o = WB0 + 48 * KiB
        sgb = [AV(o + i * KiB, [128, 256], F32) for i in range(2)]; o += 2 * KiB
        hbb = [AV(o + i * 512, [128, 256], BF16) for i in range(2)]; o += KiB
        hTb = [AV(o + i * 512, [128, 2, 128], BF16) for i in range(2)]; o += KiB
        assert o <= ARENA
        B_sg = [Buf("sg0"), Buf("sg1")]; B_hb = [Buf("hb0"), Buf("hb1")]; B_hT = [Buf("hT0"), Buf("hT1")]
        for b_ in B_estg + B_wgu + B_wd + B_sg + B_hb + B_hT:
            P.alias(b_, p3_done + p2_bufs + all_p1 + SAMPLE_BUFS)
        sti = [0]

        def load_expert(e, slot):
            for (src, coff) in ((w_gate, 0), (w_up, 256)):
                for hf in range(2):
                    i = sti[0] % NSTG; sti[0] += 1
                    dma("sync", estg[i].rearrange("p (c f) -> p c f", c=4),
                        src[e, hf * 512:(hf + 1) * 512, :].rearrange("(c p) f -> p c f", p=128), w=[B_estg[i]])
                    V("tensor_copy", [B_estg[i]], [B_wgu[slot]], eng="gpsimd", out=Wgu[slot][:, hf * 4:(hf + 1) * 4, coff:coff + 256],
                      in_=estg[i].rearrange("p (c f) -> p c f", c=4))
            for f_ in range(2):
                i = sti[0] % NSTG; sti[0] += 1
                dma("sync", estg[i], w_down[e, f_ * 128:(f_ + 1) * 128, :], w=[B_estg[i]])
                V("tensor_copy", [B_estg[i]], [B_wd[slot]], eng="gpsimd", out=Wd[slot][:, f_, :], in_=estg[i])

        pTh4 = bank(3).bitcast(BF16).rearrange("p (a b) -> p a b", a=8)
        B_pth = [Buf("pth%d" % i) for i in range(4)]
        for b_ in B_pth:
            b_.last_w = bkB[3].last_w; b_.readers = list(bkB[3].readers)
        sg3 = [AV(o_, [128, 256], F32) for o_ in (WB0 + 48 * KiB, WB0 + 49 * KiB, WB0 + 52 * KiB)]
        hb3 = [AV(WB0 + 50 * KiB + i * 512, [128, 256], BF16) for i in range(3)]
        hT4 = [AV(WB0 + 53 * KiB + i * 512, [128, 2, 128], BF16) for i in range(4)]
        assert WB0 + 55 * KiB <= ARENA
        B_sg3 = [Buf("sg3_%d" % i) for i in range(3)]; B_hb3 = [Buf("hb3_%d" % i) for i in range(3)]; B_hT4 = [Buf("hT4_%d" % i) for i in range(4)]
        for b_ in B_sg3 + B_hb3 + B_hT4:
            P.alias(b_, p3_done + p2_bufs + all_p1 + SAMPLE_BUFS + B_sg + B_hb + B_hT)
        load_expert(0, 0); load_expert(1, 1)
        msteps = []
        for ep in range(NE // 2):
            for t in range(NT):
                for k in range(2):
                    msteps.append((ep, t, k))

        def emit_G(i):
            ep, t, k = msteps[i]
            e = 2 * ep + k; slot = e % 4; gb = i % 3
            for c in range(8):
                mm(bank(gb), ATT[:, t, c, :], Wgu[slot][:, c, :], c == 0, c == 7, [B_att[t], B_wgu[slot]], [bkB[gb]])

        def emit_A(i):
            ep, t, k = msteps[i]
            e = 2 * ep + k; gb = i % 3
            act(sg3[gb], bank(gb)[:, 0:256], AF.Silu, [bkB[gb]], [B_sg3[gb]])
            V("scalar_tensor_tensor", [bkB[gb], B_sg3[gb], B_gates[t]], [B_hb3[gb]], out=hb3[gb], in0=bank(gb)[:, 256:512],
              scalar=gates[:, t, e:e + 1], in1=sg3[gb], op0=ALU.mult, op1=ALU.mult)

        def emit_T(i):
            gb = i % 3; ts = i % 4
            for f_ in range(2):
                tr(pTh4[:, 2 * ts + f_, :], hb3[gb][:, f_ * 128:(f_ + 1) * 128], identb[:], [B_hb3[gb], B_["identb"]], [B_pth[ts]])
            act(hT4[ts], pTh4[:, 2 * ts:2 * ts + 2, :], AF.Copy, [B_pth[ts]], [B_hT4[ts]])

        def emit_D(i):
            ep, t, k = msteps[i]
            if t == 0 and k == 0 and ep + 1 < NE // 2:
                load_expert(2 * ep + 2, (2 * ep + 2) % 4); load_expert(2 * ep + 3, (2 * ep + 3) % 4)
            e = 2 * ep + k; slot = e % 4; ts = i % 4
            ob = 2 + ((i // 2) % 2)
            outp = PS[ob]; Bout = [bkB[2 * ob], bkB[2 * ob + 1]]
            for half in range(2):
                for f_ in range(2):
                    mm(outp[:, half * 512:(half + 1) * 512], hT4[ts][:, f_, :], Wd[slot][:, f_, half * 512:(half + 1) * 512],
                       k == 0 and f_ == 0, k == 1 and f_ == 1, [B_hT4[ts], B_wd[slot]], Bout)
            if k == 1:
                V("tensor_tensor", Bout + [B_acc[t]], [B_acc[t]], out=ACC[:, t, :], in0=outp[:, :], in1=ACC[:, t, :], op=ALU.add)

        NS = len(msteps)
        emit_G(0)
        for i in range(NS + 1):
            if i + 1 < NS:
                emit_G(i + 1)
            if i < NS:
                emit_A(i)
                emit_T(i)
            if i - 1 >= 0:
                emit_D(i - 1)

        dma("sync", gbc[:], g_final.partition_broadcast(128), w=[B_["gbc"]])
        yb = [estg[0], estg[1]]; B_yb = [B_estg[0], B_estg[1]]
        jk5 = estg[2]; B_jk5 = B_estg[2]
        for t in range(NT):
            col = 2 * NT + t
            i = t % 2
            act(jk5, ACC[:, t, :], AF.Square, [B_acc[t]], [B_jk5, B_ssq[col]], accum_out=ssq[:, col:col + 1])
            act(rt[:, col:col + 1], ssq[:, col:col + 1], AF.Sqrt, [B_ssq[col], B_["epsb"]], [B_ssq[col]], scale=1.0 / 1024.0, bias=epsb[:, 0:1])
            V("reciprocal", [B_ssq[col]], [B_ssq[col]], out=rstd[:, col:col + 1], in_=rt[:, col:col + 1])
            V("scalar_tensor_tensor", [B_acc[t], B_ssq[col], B_["gbc"]], [B_yb[i]], out=yb[i], in0=ACC[:, t, :], scalar=rstd[:, col:col + 1],
              in1=gbc[:], op0=ALU.mult, op1=ALU.mult)
            if t < 16:
                dma("sync", yp[t * 128:(t + 1) * 128, :], yb[i], r=[B_yb[i]])
            else:
                dma("sync", ys, yb[i][0:4, :], r=[B_yb[i]])
        P.run()
    return nc


_NC = None


def kernel(**inputs):
    global _NC
    if _NC is None:
        _NC = build_program()
    nc = _NC
    f = lambda a: np.ascontiguousarray(np.asarray(a))
    consts = host_consts()
    ckf = f(inputs["cache_k"]).reshape(2621440, 64)
    cvf = f(inputs["cache_v"]).reshape(2621440, 64)
    shared = {
        "ck": ckf, "cv": cvf,
        "g_mix": f(inputs["g_mix"]).reshape(1024), "w_in": f(inputs["w_in"]).reshape(1024, 3072),
        "conv_w": f(inputs["conv_w"]).reshape(3, 512), "w_out": f(inputs["w_out"]).reshape(1024, 1024),
        "g_ffn": f(inputs["g_ffn"]).reshape(1024), "w_rg": f(inputs["w_router_group"]).reshape(1024, 4),
        "b_rg": f(inputs["b_router_group"]).reshape(4), "w_re": f(inputs["w_router_expert"]).reshape(1024, 32),
        "b_re": f(inputs["b_router_expert"]).reshape(32), "w_gate": f(inputs["w_gate"]).reshape(32, 1024, 256),
        "w_up": f(inputs["w_up"]).reshape(32, 1024, 256), "w_down": f(inputs["w_down"]).reshape(32, 256, 1024),
        "g_final": f(inputs["g_final"]).reshape(1024),
    }
    shared.update(consts)
    xpr = f(inputs["x_prompt"]); xsm = f(inputs["x_sample"]).reshape(32, 1024)
    scv = f(inputs["state_conv"]).reshape(32, 2, 512); ptb = f(inputs["page_table"]).astype(np.int32)
    in_maps = []
    for c in range(NCORES):
        m = dict(shared)
        m["xp"] = xpr[c]
        m["xs"] = xsm[4 * c:4 * c + 4]
        m["sc"] = scv[4 * c:4 * c + 4].reshape(8, 512)
        m["pt"] = ptb[4 * c:4 * c + 4]
        in_maps.append(m)
    res = run_bass_kernel_spmd(nc, in_maps, core_ids=list(range(NCORES)))
    R = res.results
    y_prompt = np.stack([R[c]["yp"] for c in range(NCORES)]).reshape(8, 2048, 1024)
    y_sample = np.concatenate([R[c]["ys"] for c in range(NCORES)]).reshape(32, 1, 1024)
    k_prompt = np.stack([R[c]["kp"] for c in range(NCORES)]).reshape(1, 8, 2048, 8, 64)
    v_prompt = np.stack([R[c]["vp"] for c in range(NCORES)]).reshape(1, 8, 2048, 8, 64)
    conv_prompt = np.stack([R[c]["cp"] for c in range(NCORES)]).reshape(1, 8, 2, 512)
    k_sample = np.concatenate([R[c]["ks"] for c in range(NCORES)]).reshape(1, 32, 1, 8, 64)
    v_sample = np.concatenate([R[c]["vs"] for c in range(NCORES)]).reshape(1, 32, 1, 8, 64)
    conv_sample = np.concatenate([R[c]["cso"] for c in range(NCORES)]).reshape(1, 32, 2, 512)
    return tuple(np.asarray(a, dtype=np.float32) for a in
                 (y_prompt, y_sample, k_prompt, v_prompt, conv_prompt, k_sample, v_sample, conv_sample))
```

```python
import contextlib
import numpy as np
import ml_dtypes
import concourse.bass as bass
import concourse.mybir as mybir
from concourse.bass_utils import run_bass_kernel_spmd

F32 = mybir.dt.float32
BF16 = mybir.dt.bfloat16
I32 = mybir.dt.int32
U8 = mybir.dt.uint8
AF = mybir.ActivationFunctionType
ALU = mybir.AluOpType
AX = mybir.AxisListType

ENGS = ("sync", "scalar", "vector", "gpsimd", "tensor")
NCORES = 8
NT = 17
NE = 32
BIG = 30000.0
KiB = 1024


class Buf:
    __slots__ = ("name", "last_w", "readers")

    def __init__(self, name):
        self.name = name
        self.last_w = None
        self.readers = []


class Ins:
    __slots__ = ("eng", "fn", "deps", "signal", "sigval", "is_dma", "dsem", "dval")

    def __init__(self, eng, fn, is_dma):
        self.eng = eng
        self.fn = fn
        self.deps = []
        self.signal = False
        self.sigval = None
        self.is_dma = is_dma
        self.dsem = None
        self.dval = None


class Prog:
    def __init__(self, nc, ring=8):
        self.nc = nc
        self.q = {e: [] for e in ENGS}
        self.ringd = {e: ring for e in ENGS}
        self.ringd["gpsimd"] = 8
        self.dma_count = {e: 0 for e in ENGS}
        self.dma_hist = {e: [] for e in ENGS}

    def _add(self, ins, r, w):
        deps = []
        for b in r:
            if b.last_w is not None:
                deps.append(b.last_w)
        for b in w:
            if b.last_w is not None:
                deps.append(b.last_w)
            deps.extend(b.readers)
        for b in w:
            b.last_w = ins
            b.readers = []
        for b in r:
            if b.last_w is not ins:
                b.readers.append(ins)
        seen = set()
        for d in deps:
            if d is ins or id(d) in seen:
                continue
            seen.add(id(d))
            if (not d.is_dma) and d.eng == ins.eng and d.eng == "tensor" and not ins.is_dma:
                continue
            ins.deps.append(d)
            if not d.is_dma:
                d.signal = True
        self.q[ins.eng].append(ins)
        return ins

    def op(self, eng, fn, r=(), w=()):
        return self._add(Ins(eng, fn, False), list(r), list(w))

    def dma(self, eng, fn, r=(), w=()):
        ins = Ins(eng, fn, True)
        k = self.dma_count[eng]
        self.dma_count[eng] += 1
        hist = self.dma_hist[eng]
        ring = self.ringd[eng]
        if k >= ring:
            ins.deps.append(hist[k - ring])
        hist.append(ins)
        ins.dsem = (eng, k % ring)
        ins.dval = 16 * (k // ring + 1)
        return self._add(ins, list(r), list(w))

    def alias(self, new, olds):
        for o in olds:
            if o.last_w is not None:
                new.readers.append(o.last_w)
            new.readers.extend(o.readers)

    def run(self):
        nc = self.nc
        with contextlib.ExitStack() as st:
            esem = {e: st.enter_context(nc.semaphore("es_" + e)) for e in ENGS}
            dsem = {}
            for e in ENGS:
                for i in range(min(self.ringd[e], self.dma_count[e])):
                    dsem[(e, i)] = st.enter_context(nc.semaphore("ds_%s%d" % (e, i)))
            for e in ENGS:
                c = 0
                for ins in self.q[e]:
                    if (not ins.is_dma) and ins.signal:
                        c += 1
                        ins.sigval = c
            allsems = list(esem.values()) + list(dsem.values())
            for s_ in allsems:
                nc.gpsimd.sem_clear(s_)
            nc.all_engine_barrier()
            block = nc.Block()
            block.__enter__()

            def make(ename):
                def body(eng):
                    known = {}
                    for ins in self.q[ename]:
                        need = {}
                        for d in ins.deps:
                            if d.is_dma:
                                s, v = dsem[d.dsem], d.dval
                            else:
                                s, v = esem[d.eng], d.sigval
                            key = id(s)
                            if known.get(key, 0) >= v:
                                continue
                            if key not in need or need[key][1] < v:
                                need[key] = (s, v)
                        for key, (s, v) in need.items():
                            eng.wait_ge(s, v)
                            known[key] = v
                        r = ins.fn(eng)
                        if ins.is_dma:
                            r.then_inc(dsem[ins.dsem], 16)
                        elif ins.signal:
                            r.then_inc(esem[ename], 1)
                    for ins in self.dma_hist[ename][-self.ringd[ename]:]:
                        s, v = dsem[ins.dsem], ins.dval
                        if known.get(id(s), 0) < v:
                            eng.wait_ge(s, v)
                            known[id(s)] = v
                return body

            for e in ENGS:
                if self.q[e]:
                    getattr(block, e)(make(e))
            block.__exit__(None, None, None)
            st.pop_all()
            nc.all_engine_barrier()
            for s_ in allsems:
                nc.gpsimd.sem_clear(s_)
            nc.all_engine_barrier()


NFC = 1068


def host_consts():
    c = {}
    c["c_identb"] = np.eye(128, dtype=np.float32).astype(ml_dtypes.bfloat16)
    c["c_identf"] = np.eye(128, dtype=np.float32)
    kk = np.arange(128)[:, None]
    qq = np.arange(128)[None, :]
    c["c_tri"] = (qq >= kk).astype(np.float32).astype(ml_dtypes.bfloat16)
    inv = (500000.0 ** (-np.arange(0, 16, 2, dtype=np.float32) / np.float32(16))).astype(np.float32)
    cs = np.zeros((NT, 128, 128), np.float32)
    for t in range(NT):
        pos = (t * 128 + np.arange(128)) if t < 16 else np.full(128, 8192)
        ang = pos.astype(np.float32)[:, None] * inv[None, :]
        ang = ang.astype(np.float32).astype(np.float64)
        cs[t, :, 0:64] = np.tile(np.cos(ang), (1, 8))
        cs[t, :, 64:128] = np.tile(np.sin(ang), (1, 8))
    c["c_cs"] = cs
    g = np.zeros((16, 128, 192), np.float32)
    for t in range(16):
        own = (t * 128 + np.arange(128)) // 256
        j = np.arange(8)[None, :]
        past = (j < own[:, None]).astype(np.float32)
        ownm = (j == own[:, None]).astype(np.float32)
        g[t, :, 0:64] = np.tile((past - 1.0) * 1e30, (1, 8))
        g[t, :, 64:128] = np.tile(past, (1, 8))
        g[t, :, 128:192] = np.tile(ownm, (1, 8))
    c["c_g"] = g
    blk = np.zeros((8, 2048), np.float32)
    for j in range(8):
        blk[j, j * 256:(j + 1) * 256] = 1.0
    c["c_blk"] = blk.astype(ml_dtypes.bfloat16)
    f = np.zeros((128, NFC), np.float32)
    p = np.arange(128)
    f[p, p // 2] = 1.0
    for h in range(8):
        for b in range(4):
            for r in range(3):
                for hf in range(2):
                    col = 64 + ((h * 4 + b) * 3 + r) * 2 + hf
                    f[:, col] = p * 8 + h
    f[:, 256:288] = np.arange(32)[None, :]
    for pr in range(2):
        for m in range(64):
            f[2 * pr + m // 32, 288 + pr * 64 + m] = 1.0
    for b in range(4):
        f[b, 416 + b * 128: 416 + (b + 1) * 128] = 1.0
    f[0:8, 928:936] = np.eye(8)
    f[0:4, 936:940] = np.eye(4)
    f[:, 940:1068] = 1.0
    c["c_f"] = f
    return c


def build_program(debug=False):
    nc = bass.Bass("TRN2", target_bir_lowering=False)
    P = Prog(nc)

    def din(name, shape, dt=F32):
        return nc.dram_tensor(name, list(shape), dt, kind="ExternalInput").ap()

    def dout(name, shape, dt=F32):
        return nc.dram_tensor(name, list(shape), dt, kind="ExternalOutput").ap()

    xp = din("xp", [2048, 1024]); xs = din("xs", [4, 1024])
    ck = din("ck", [2621440, 64]); cv = din("cv", [2621440, 64])
    sc = din("sc", [8, 512]); pt = din("pt", [4, 64], I32)
    g_mix = din("g_mix", [1024]); w_in = din("w_in", [1024, 3072]); conv_w = din("conv_w", [3, 512])
    w_out = din("w_out", [1024, 1024]); g_ffn = din("g_ffn", [1024])
    w_rg = din("w_rg", [1024, 4]); b_rg = din("b_rg", [4]); w_re = din("w_re", [1024, 32]); b_re = din("b_re", [32])
    w_gate = din("w_gate", [32, 1024, 256]); w_up = din("w_up", [32, 1024, 256]); w_down = din("w_down", [32, 256, 1024])
    g_final = din("g_final", [1024])
    c_identb = din("c_identb", [128, 128], BF16); c_identf = din("c_identf", [128, 128])
    c_tri = din("c_tri", [128, 128], BF16); c_cs = din("c_cs", [NT, 128, 128]); c_g = din("c_g", [16, 128, 192])
    c_blk = din("c_blk", [8, 2048], BF16); c_f = din("c_f", [128, NFC])
    yp = dout("yp", [2048, 1024]); ys = dout("ys", [4, 1024])
    kp = dout("kp", [2048, 512]); vp = dout("vp", [2048, 512]); cp = dout("cp", [2, 512])
    ks = dout("ks", [4, 512]); vs = dout("vs", [4, 512]); cso = dout("cso", [8, 512])
    ckc = ck.rearrange("(a b) d -> a (b d)", b=16)

    st = contextlib.ExitStack()
    with st:
        ARENA = 190 * KiB
        arena = st.enter_context(nc.sbuf_tensor("arena", [128, ARENA], U8))

        def AV(off, shape, dt, p0=0):
            isz = 2 if dt == BF16 else 4
            n = 1
            for s in shape[1:]:
                n *= s
            nb = n * isz
            assert off + nb <= ARENA, (off, nb)
            ap = arena[p0:p0 + shape[0], off:off + nb].bitcast(dt)
            if len(shape) == 3:
                ap = ap.rearrange("p (a b) -> p a b", a=shape[1])
            elif len(shape) == 4:
                ap = ap.rearrange("p (a b c) -> p a b c", a=shape[1], b=shape[2])
            return ap

        def ST(name, shape, dt):
            return st.enter_context(nc.sbuf_tensor(name, list(shape), dt))

        PS = [st.enter_context(nc.psum_tensor("ps%d" % i, [128, 1024], F32)) for i in range(4)]

        def bank(i):
            return PS[i // 2][:, (i % 2) * 512:(i % 2 + 1) * 512]

        bkB = [P_buf for P_buf in (Buf("bank%d" % i) for i in range(8))]

        identb = ST("identb", [128, 128], BF16); identf = ST("identf", [128, 128], F32); tri = ST("tri", [128, 128], BF16)
        cf = ST("cf", [128, NFC], F32)
        gbc = ST("gbc", [128, 1024], F32)
        ssq = ST("ssq", [128, 3 * NT], F32); rt = ST("rt", [128, 3 * NT], F32); rstd = ST("rstd", [128, 3 * NT], F32)
        convw = ST("convw", [128, 12], F32)
        ksum = ST("ksum", [64, 64], F32); ksumhi = ST("ksumhi", [64, 64], BF16); ksumlo = ST("ksumlo", [64, 64], BF16)
        ksumr = ST("ksumr", [64, 64], F32)
        epsb = ST("epsb", [128, 1], F32)
        rb = ST("rb", [128, 36], F32)
        wr = ST("wr", [128, 8, 36], F32)
        B_ = {n: Buf(n) for n in ("identb identf tri cf gbc convw ksum ksumhi ksumlo ksumr epsb rb wr").split()}
        B_ssq = [Buf("ssq%d" % i) for i in range(3 * NT)]

        def dma(q, out, in_, r=(), w=()):
            return P.dma(q, lambda e: e.dma_start(out=out, in_=in_), r, w)

        def act(out, in_, func, r, w, **kw):
            return P.op("scalar", lambda e: e.activation(out=out, in_=in_, func=func, **kw), r, w)

        def mm(out, lhsT, rhs, start, stop, r, w):
            return P.op("tensor", lambda e: e.matmul(out, lhsT=lhsT, rhs=rhs, start=start, stop=stop), r, w)

        def tr(out, in_, ident, r, w):
            return P.op("tensor", lambda e: e.transpose(out=out, in_=in_, identity=ident), r, w)

        def V(name, r, w, eng="vector", **kw):
            return P.op(eng, lambda e: getattr(e, name)(**kw), r, w)

        dma("sync", identb[:], c_identb, w=[B_["identb"]])
        dma("sync", identf[:], c_identf, w=[B_["identf"]])
        dma("sync", tri[:], c_tri, w=[B_["tri"]])
        dma("sync", cf[:], c_f, w=[B_["cf"]])
        dma("sync", gbc[:], g_mix.partition_broadcast(128), w=[B_["gbc"]])
        V("memset", [], [B_["epsb"]], ap=epsb[:], constant=1e-6)
        PairM = cf[:, 0:64]; poshc = cf[:, 64:256]; chunkid = cf[:, 256:288]
        ones_f = cf[:, 940:1068]

        cw12 = ST("cw12", [12, 128], F32); Bcw12 = Buf("cw12")
        dma("sync", cw12[:], conv_w.rearrange("k (t p) -> (k t) p", p=128), w=[Bcw12])
        tr(bank(0)[:, 0:12], cw12[:], identf[0:12, 0:12], [Bcw12, B_["identf"]], [bkB[0]])
        act(convw[:], bank(0)[:, 0:12], AF.Copy, [bkB[0]], [B_["convw"]])
        dma("sync", wr[:, :, 0:4], w_rg.rearrange("(c p) n -> p c n", p=128), w=[B_["wr"]])
        dma("sync", wr[:, :, 4:36], w_re.rearrange("(c p) n -> p c n", p=128), w=[B_["wr"]])
        dma("sync", rb[:, 0:4], b_rg.partition_broadcast(128), w=[B_["rb"]])
        dma("sync", rb[:, 4:36], b_re.partition_broadcast(128), w=[B_["rb"]])

        WinB = AV(0, [128, 8, 3072], BF16); B_win = [Buf("win%d" % c) for c in range(8)]
        W0 = 146 * KiB
        stg = [AV(W0 + i * 12 * KiB, [128, 3072], F32) for i in range(2)]
        B_stg = [Buf("stg%d" % i) for i in range(2)]
        for c in range(8):
            s = c % 2
            dma("sync", stg[s], w_in[c * 128:(c + 1) * 128, :], w=[B_stg[s]])
            V("tensor_copy", [B_stg[s]], [B_win[c]], eng="vector", out=WinB[:, c, :], in_=stg[s])

        QT = AV(48 * KiB, [72, 8, 2048], BF16); KT = AV(80 * KiB, [72, 8, 2048], BF16)
        Vb = AV(112 * KiB, [128, 16, 8, 65], BF16)
        convT = AV(129 * KiB, [128, NT, 4, 128], BF16)
        B_qt = [Buf("qt%d" % t) for t in range(16)]; B_kt = [Buf("kt%d" % t) for t in range(16)]
        B_vb = [Buf("vb%d" % t) for t in range(16)]; B_ct = [Buf("convT%d" % t) for t in range(NT)]
        B_qtb = [Buf("qtb%d" % t) for t in range(16)]
        V("memset", [], B_vb, eng="gpsimd", ap=Vb[:, :, :, 64:65], constant=1.0)
        for h in range(8):
            dma("sync", KT[64:72, h, :], c_blk, w=B_kt)
        V("memset", [], [B_ct[16]], eng="gpsimd", ap=convT[:, 16, :, :], constant=0.0)

        o = W0
        qf = AV(o, [128, 512], F32); o += 2 * KiB
        kf = [AV(o + i * 2 * KiB, [128, 512], F32) for i in range(2)]; o += 4 * KiB
        vf = [AV(o, [128, 512], F32)] * 2; o += 2 * KiB
        xt = [AV(o + i * 4 * KiB, [128, 1024], F32) for i in range(2)]; o += 8 * KiB
        xnb = AV(o, [128, 1024], BF16); o += 2 * KiB
        xnTg = AV(o, [128, 8, 512], BF16); o += 8 * KiB
        qb = AV(o, [128, 512], BF16); o += 1 * KiB
        kb = AV(o, [128, 512], BF16); o += 1 * KiB
        rtq = [AV(o + i * 256, [128, 8, 8], F32) for i in range(4)]; o += 1 * KiB
        rtk = [AV(o + i * 256, [128, 8, 8], F32) for i in range(4)]; o += 1 * KiB
        Hs = AV(o, [128, 512], F32); o += 2 * KiB
        ctmp = AV(o, [128, 512], F32); o += 2 * KiB
        ug = AV(o, [128, 4, 514], F32); o += 8224 + 32
        cst = [AV(o + i * 512, [128, 128], F32) for i in range(2)]; o += 1 * KiB
        assert o <= ARENA, o
        Bw = {n: Buf(n) for n in "xnb xnTg qf qb kb rtq rtk Hs ctmp ug".split()}
        B_xt = [Buf("xt0"), Buf("xt1")]; B_kf = [Buf("kf0"), Buf("kf1")]; B_vf = [Buf("vf0")] * 2
        B_cst = [Buf("cst0"), Buf("cst1")]
        for b_ in list(Bw.values()) + B_xt + B_kf + B_vf[:1] + B_cst:
            P.alias(b_, B_stg)
        V("memset", [], [Bw["ug"]], eng="gpsimd", ap=ug[:, :, 0:2], constant=0.0)

        def load_x(t):
            s = t % 2
            if t < 16:
                dma("sync", xt[s], xp[t * 128:(t + 1) * 128, :], w=[B_xt[s]])
            else:
                V("memset", [], [B_xt[s]], eng="gpsimd", ap=xt[s], constant=0.0)
                dma("sync", xt[s][0:4, :], xs, w=[B_xt[s]])
            dma("sync", cst[s], c_cs[t], w=[B_cst[s]])

        def rmsnorm_stats(src, col, rbufs):
            act(junk, src, AF.Square, rbufs, B_junkl + [B_ssq[col]], accum_out=ssq[:, col:col + 1])
            act(rt[:, col:col + 1], ssq[:, col:col + 1], AF.Sqrt, [B_ssq[col], B_["epsb"]], [B_ssq[col]],
                scale=1.0 / 1024.0, bias=epsb[:, 0:1])
            V("reciprocal", [B_ssq[col]], [B_ssq[col]], out=rstd[:, col:col + 1], in_=rt[:, col:col + 1])

        junk = AV(W0 + 30 * KiB, [128, 1024], F32); B_junkl = [Bw["Hs"], Bw["ctmp"]]
        us = ST("us", [128, 4, 4], F32); B_us = Buf("us")
        cs4 = ST("cs4", [128, 4, 4], F32); B_cs4 = Buf("cs4")
        prevT = ST("prevT", [128, 4, 4, 2], F32); B_prevT = Buf("prevT")
        csT = ST("csT", [128, 4, 4, 2], F32); B_csT = Buf("csT")
        misc8 = ST("misc8", [8, 512], F32); B_misc8 = Buf("misc8")
        cpo = misc8[0:2, :]; sc8 = misc8; cso8 = misc8
        B_cpo = B_sc8 = B_cso8 = B_misc8

        def rope(tf, tmps, cs_t, Bt, Btmp, Bcs, eng):
            v = tf.rearrange("p (h d) -> p h d", h=8)
            x1 = v[:, :, 0:8]; x2 = v[:, :, 8:16]
            cosv = cs_t[:, 0:64].rearrange("p (h d) -> p h d", h=8); sinv = cs_t[:, 64:128].rearrange("p (h d) -> p h d", h=8)
            t1, t2, t3, t4 = tmps
            V("tensor_tensor", [Bt, Bcs], [Btmp], eng=eng, out=t1, in0=x1, in1=cosv, op=ALU.mult)
            V("tensor_tensor", [Bt, Bcs], [Btmp], eng=eng, out=t2, in0=x2, in1=sinv, op=ALU.mult)
            V("tensor_tensor", [Bt, Bcs], [Btmp], eng=eng, out=t3, in0=x2, in1=cosv, op=ALU.mult)
            V("tensor_tensor", [Bt, Bcs], [Btmp], eng=eng, out=t4, in0=x1, in1=sinv, op=ALU.mult)
            V("tensor_tensor", [Btmp], [Bt], eng=eng, out=x1, in0=t1, in1=t2, op=ALU.subtract)
            V("tensor_tensor", [Btmp], [Bt], eng=eng, out=x2, in0=t3, in1=t4, op=ALU.add)

        pTx = bank(0).bitcast(BF16).rearrange("p (a b) -> p a b", a=8)
        pTqk = bank(4).bitcast(BF16).rearrange("p (a b) -> p a b", a=8)

        load_x(0)
        load_x(1)
        groups = [[0, 1, 2, 3], [4, 5, 6, 7], [8, 9, 10, 11], [12, 13, 14, 15], [16]]

        def FE(t, tt):
            s = t % 2
            rmsnorm_stats(xt[s], t, [B_xt[s]])
            V("scalar_tensor_tensor", [B_xt[s], B_ssq[t], B_["gbc"]], [Bw["xnb"]], out=xnb, in0=xt[s],
              scalar=rstd[:, t:t + 1], in1=gbc[:], op0=ALU.mult, op1=ALU.mult)
            for c in range(8):
                tr(pTx[:, c, :], xnb[:, c * 128:(c + 1) * 128], identb[:], [Bw["xnb"], B_["identb"]], [bkB[0]])
            act(xnTg[:, :, tt * 128:(tt + 1) * 128], pTx, AF.Copy, [bkB[0]], [B_xs[tt]])

        B_xs = [Buf("xnTg%d" % i) for i in range(4)]
        for b_ in B_xs:
            P.alias(b_, B_stg)
        for g, tiles in enumerate(groups):
            ncol = 128 * len(tiles)
            for tt, t in enumerate(tiles):
                s = t % 2
                if tt == 0:
                    FE(t, tt)
                if tt + 1 < len(tiles):
                    FE(tiles[tt + 1], tt + 1)
                if False:
                    rmsnorm_stats(xt[s], t, [B_xt[s]])
                for j in range(3):
                    for c in range(8):
                        mm(bank(1 + j), xnTg[:, c, tt * 128:(tt + 1) * 128], WinB[:, c, j * 512:(j + 1) * 512],
                           c == 0, c == 7, [B_xs[tt], B_win[c]], [bkB[1 + j]])
                tq, tk, tv = qf, kf[s], vf[s]
                Bq, Bk, Bv = Bw["qf"], B_kf[s], B_vf[s]
                act(tq, bank(1), AF.Copy, [bkB[1]], [Bq], scale=0.125)
                act(tk, bank(2), AF.Copy, [bkB[2]], [Bk])
                act(tv, bank(3), AF.Copy, [bkB[3]], [Bv])
                rope(tq, rtq, cst[s], Bq, Bw["rtq"], B_cst[s], "vector")
                rope(tk, rtk, cst[s], Bk, Bw["rtk"], B_cst[s], "gpsimd")
                if t < 16:
                    dma("sync", kp[t * 128:(t + 1) * 128, :], tk, r=[Bk])
                    dma("sync", vp[t * 128:(t + 1) * 128, :], tv, r=[Bv])
                    V("tensor_copy", [Bv], [B_vb[t]], eng="gpsimd", out=Vb[:, t, :, 0:64],
                      in_=tv.rearrange("p (h d) -> p h d", h=8))
                    V("tensor_copy", [Bq], [Bw["qb"]], out=qb, in_=tq)
                    V("tensor_copy", [Bk], [Bw["kb"]], eng="gpsimd", out=kb, in_=tk)
                    for h in range(8):
                        tr(pTqk[0:64, h, :], qb[:, h * 64:(h + 1) * 64], identb[:], [Bw["qb"], B_["identb"]], [bkB[4]])
                    act(QT[0:64, :, t * 128:(t + 1) * 128], pTqk[0:64, :, :], AF.Copy, [bkB[4]], [B_qt[t]])
                    for h in range(8):
                        tr(pTqk[0:64, h, :], kb[:, h * 64:(h + 1) * 64], identb[:], [Bw["kb"], B_["identb"]], [bkB[4]])
                    act(KT[0:64, :, t * 128:(t + 1) * 128], pTqk[0:64, :, :], AF.Copy, [bkB[4]], [B_kt[t]])
                else:
                    dma("sync", ks, tk[0:4, :], r=[Bk])
                    dma("sync", vs, tv[0:4, :], r=[Bv])
                if t + 2 < NT:
                    load_x(t + 2)
            for ct in range(4):
                def colsW(j):
                    return slice(1536 + j * 512 + ct * 128, 1536 + j * 512 + (ct + 1) * 128)
                for c in range(8):
                    mm(bank(5)[:, 0:ncol], WinB[:, c, colsW(2)], xnTg[:, c, 0:ncol], c == 0, c == 7, B_xs + [B_win[c]], [bkB[5]])
                for c in range(8):
                    mm(bank(6)[:, 0:ncol], WinB[:, c, colsW(1)], xnTg[:, c, 0:ncol], c == 0, c == 7, B_xs + [B_win[c]], [bkB[6]])
                act(Hs[:, 0:ncol], bank(5)[:, 0:ncol], AF.Copy, [bkB[5]], [Bw["Hs"]])
                for c in range(8):
                    mm(bank(5)[:, 0:ncol], WinB[:, c, colsW(0)], xnTg[:, c, 0:ncol], c == 0, c == 7, B_xs + [B_win[c]], [bkB[5]])
                w0 = convw[:, 0 * 4 + ct:0 * 4 + ct + 1]; w1 = convw[:, 4 + ct:4 + ct + 1]; w2 = convw[:, 8 + ct:8 + ct + 1]
                if g < 4:
                    V("tensor_tensor", [bkB[6], Bw["Hs"]], [Bw["ug"]], out=ug[:, ct, 2:514], in0=bank(6), in1=Hs, op=ALU.mult)
                    V("tensor_scalar", [Bw["ug"], B_["convw"]], [Bw["ctmp"]], out=ctmp, in0=ug[:, ct, 2:514], scalar1=w2, scalar2=None, op0=ALU.mult)
                    V("scalar_tensor_tensor", [Bw["ug"], Bw["ctmp"], B_["convw"]], [Bw["ctmp"]], out=ctmp, in0=ug[:, ct, 1:513], scalar=w1, in1=ctmp, op0=ALU.mult, op1=ALU.add)
                    V("scalar_tensor_tensor", [Bw["ug"], Bw["ctmp"], B_["convw"]], [Bw["ctmp"]], out=ctmp, in0=ug[:, ct, 0:512], scalar=w0, in1=ctmp, op0=ALU.mult, op1=ALU.add)
                    V("tensor_tensor", [bkB[5], Bw["ctmp"]], [B_ct[t_] for t_ in tiles],
                      out=convT[:, tiles[0]:tiles[0] + 4, ct, :], in0=bank(5).rearrange("p (a b) -> p a b", a=4),
                      in1=ctmp.rearrange("p (a b) -> p a b", a=4), op=ALU.mult)
                    V("tensor_copy", [Bw["ug"]], [Bw["ug"]], out=ug[:, ct, 0:2], in_=ug[:, ct, 512:514])
                else:
                    V("tensor_tensor", [bkB[6], Bw["Hs"]], [B_us], out=us[:, ct, :], in0=bank(6)[:, 0:4], in1=Hs[:, 0:4], op=ALU.mult)
                    V("tensor_scalar", [B_us, B_["convw"]], [B_cs4], out=cs4[:, ct, :], in0=us[:, ct, :], scalar1=w2, scalar2=None, op0=ALU.mult)
                    V("scalar_tensor_tensor", [B_prevT, B_cs4, B_["convw"]], [B_cs4], out=cs4[:, ct, :], in0=prevT[:, ct, :, 1], scalar=w1, in1=cs4[:, ct, :], op0=ALU.mult, op1=ALU.add)
                    V("scalar_tensor_tensor", [B_prevT, B_cs4, B_["convw"]], [B_cs4], out=cs4[:, ct, :], in0=prevT[:, ct, :, 0], scalar=w0, in1=cs4[:, ct, :], op0=ALU.mult, op1=ALU.add)
                    V("tensor_tensor", [bkB[5], B_cs4], [B_ct[16]], out=convT[:, 16, ct, 0:4], in0=bank(5)[:, 0:4], in1=cs4[:, ct, :], op=ALU.mult)
            if g == 3:
                for ct in range(4):
                    tr(bank(0)[0:2, ct * 128:(ct + 1) * 128], ug[:, ct, 0:2], identf[:], [Bw["ug"], B_["identf"]], [bkB[0]])
                act(cpo, bank(0)[0:2, :], AF.Copy, [bkB[0]], [B_cpo])
                dma("sync", cp, cpo, r=[B_cpo])
                dma("sync", sc8[:], sc, w=[B_sc8])
                for ct in range(4):
                    tr(bank(0)[:, 512 - 32 + ct * 8: 512 - 32 + (ct + 1) * 8], sc8[:, ct * 128:(ct + 1) * 128], identf[0:8, 0:8], [B_sc8, B_["identf"]], [bkB[0]])
                act(prevT[:].rearrange("p a b c -> p (a b c)"), bank(0)[:, 480:512], AF.Copy, [bkB[0]], [B_prevT])
            if g == 4:
                V("tensor_copy", [B_prevT], [B_csT], out=csT[:, :, :, 0], in_=prevT[:, :, :, 1])
                V("tensor_copy", [B_us], [B_csT], out=csT[:, :, :, 1], in_=us[:, :, :])
                for ct in range(4):
                    tr(bank(0)[0:8, ct * 128:(ct + 1) * 128], csT[:, ct, :, :].rearrange("p a b -> p (a b)"), identf[:], [B_csT, B_["identf"]], [bkB[0]])
                act(cso8[:], bank(0)[0:8, :], AF.Copy, [bkB[0]], [B_cso8])
                dma("sync", cso, cso8[:], r=[B_cso8])

        all_p1 = list(Bw.values()) + B_xt + B_kf + B_vf[:1] + B_cst + B_win + B_xs
        ATT = AV(0, [128, NT, 8, 128], BF16)
        B_att = [Buf("att%d" % t) for t in range(NT)]
        for b_ in B_att:
            P.alias(b_, B_win)
        Pb = [AV(34 * KiB + i * KiB, [128, 512], BF16) for i in range(4)]; B_P = [Buf("P%d" % i) for i in range(4)]
        osb = [AV(38 * KiB + i * 2 * KiB, [65, 512], F32) for i in range(2)]; B_osb = [Buf("osb%d" % i) for i in range(2)]
        biasp = [AV(42 * KiB + i * 1152, [128, 8, 72], BF16) for i in range(2)]; B_bp = [Buf("bp%d" % i) for i in range(2)]
        gct = [AV(45 * KiB + i * 768, [128, 192], F32) for i in range(2)]; B_gct = [Buf("gct%d" % i) for i in range(2)]
        gm = AV(46 * KiB + 512, [128, 64], F32); top8 = AV(46 * KiB + 768, [128, 8, 8], F32); sel = AV(47 * KiB, [128, 64], F32)
        B_gm = Buf("gm"); B_top8 = Buf("top8"); B_sel = Buf("sel")
        for b_ in B_P + B_osb + B_bp + B_gct + [B_gm, B_top8, B_sel]:
            P.alias(b_, B_win)
        V("memset", [], [B_att[16]], eng="gpsimd", ap=ATT[0:64, 16, :, :], constant=0.0)
        for i in range(2):
            V("memset", [], [B_bp[i]], eng="gpsimd", ap=biasp[i], constant=0.0)
        SAMPLE_BUFS = []

        def sbuf_(name, olds=None):
            b_ = Buf(name)
            P.alias(b_, (all_p1 if olds is None else olds) + SAMPLE_BUFS)
            SAMPLE_BUFS.append(b_)
            return b_
        SB = 154 * KiB
        NG = 5
        G = [AV(SB + i * 4 * KiB, [128, 1024], F32) for i in range(NG)]
        accs = [AV(SB + 20 * KiB + i * 4 * KiB, [128, 1024], F32) for i in range(2)]
        B_G = [sbuf_("G%d" % i) for i in range(NG)]; B_accs = [sbuf_("accs0"), sbuf_("accs1")]
        oA = [186 * KiB]

        def smallA(shape, dt):
            isz = 2 if dt == BF16 else 4
            n = 1
            for x_ in shape[1:]:
                n *= x_
            ap = AV(oA[0], shape, dt)
            oA[0] += (n * isz + 31) // 32 * 32
            assert oA[0] <= ARENA
            return ap
        ptI = smallA([128, 2], I32); ptF = smallA([128, 2], F32); idxf = smallA([128, 128], F32); idxI = smallA([128, 128], I32)
        ptb8 = smallA([8, 256], I32); PTf = smallA([8, 256], F32)
        B_s0 = sbuf_("s0")
        for pr in range(2):
            dma("sync", ptI[:, pr:pr + 1], pt[2 * pr:2 * pr + 2, :].rearrange("b (g o) -> (b g) o", o=1), w=[B_s0])
        dma("sync", ptb8, pt.rearrange("b g -> (b g)").partition_broadcast(8), w=[B_s0])
        V("tensor_copy", [B_s0], [B_s0], out=ptF, in_=ptI)
        V("tensor_copy", [B_s0], [B_s0], out=PTf, in_=ptb8)
        V("tensor_scalar", [B_s0], [B_s0], out=ptF, in0=ptF, scalar1=64.0, scalar2=None, op0=ALU.mult)
        for pr in range(2):
            V("tensor_scalar", [B_s0, B_["cf"]], [B_s0], out=idxf[:, pr * 64:pr * 64 + 32], in0=chunkid, scalar1=ptF[:, pr:pr + 1], scalar2=None, op0=ALU.add)
            V("tensor_scalar", [B_s0], [B_s0], out=idxf[:, pr * 64 + 32:pr * 64 + 64], in0=idxf[:, pr * 64:pr * 64 + 32], scalar1=32.0, scalar2=None, op0=ALU.add)
        V("tensor_scalar", [B_s0], [B_s0], out=idxf, in0=idxf, scalar1=0.0, scalar2=163839.0, op0=ALU.max, op1=ALU.min)
        V("tensor_copy", [B_s0], [B_s0], out=idxI, in_=idxf)

        def s1_steps():
            chunks = [(pr, c) for pr in range(2) for c in range(64)]
            AHEAD = NG - 1

            def gather(k_):
                pr, c = chunks[k_]
                gi_ = k_ % NG
                col = pr * 64 + c
                P.dma("gpsimd", lambda e, gi_=gi_, col=col: e.indirect_dma_start(
                    out=G[gi_], out_offset=None, in_=ckc[:, :], in_offset=bass.IndirectOffsetOnAxis(ap=idxI[:, col:col + 1], axis=0)),
                    [B_s0], [B_G[gi_]])

            def accum(k_):
                pr, c = chunks[k_]
                gi_ = k_ % NG
                if c == 0:
                    V("tensor_copy", [B_G[gi_]], [B_accs[pr]], out=accs[pr], in_=G[gi_])
                else:
                    V("tensor_tensor", [B_G[gi_], B_accs[pr]], [B_accs[pr]], out=accs[pr], in0=accs[pr], in1=G[gi_], op=ALU.add)
            for k_ in range(min(AHEAD, len(chunks))):
                gather(k_)
            for k_ in range(len(chunks)):
                if k_ + AHEAD < len(chunks):
                    gather(k_ + AHEAD)
                accum(k_)
                yield
        s1gen = s1_steps()

        def s1_advance(n):
            for _ in range(n):
                try:
                    next(s1gen)
                except StopIteration:
                    return
        for h in range(8):
            V("tensor_reduce", B_kt, [B_["ksum"]], out=ksum[:, h * 8:(h + 1) * 8],
              in_=KT[0:64, h, :].rearrange("p (b k) -> p b k", b=8), axis=AX.X, op=ALU.add)
        V("tensor_copy", [B_["ksum"]], [B_["ksumhi"]], out=ksumhi[:], in_=ksum[:])
        V("tensor_tensor", [B_["ksum"], B_["ksumhi"]], [B_["ksumlo"]], out=ksumlo[:], in0=ksum[:], in1=ksumhi[:], op=ALU.subtract)
        pB = bank(7).bitcast(BF16).rearrange("p (a b) -> p a b", a=8)
        for t in range(16):
            s = t % 2
            s1_advance(8)
            dma("sync", gct[s], c_g[t], w=[B_gct[s]])
            for h in range(8):
                mm(bank(6)[:, h * 8:(h + 1) * 8], QT[0:64, h, t * 128:(t + 1) * 128], ksumhi[:, h * 8:(h + 1) * 8], True, False,
                   [B_qt[t], B_["ksumhi"]], [bkB[6]])
                mm(bank(6)[:, h * 8:(h + 1) * 8], QT[0:64, h, t * 128:(t + 1) * 128], ksumlo[:, h * 8:(h + 1) * 8], False, True,
                   [B_qt[t], B_["ksumlo"]], [bkB[6]])
            V("tensor_tensor", [bkB[6], B_gct[s]], [B_gm], out=gm, in0=bank(6)[:, 0:64], in1=gct[s][:, 0:64], op=ALU.add)
            for h in range(8):
                V("max", [B_gm], [B_top8], out=top8[:, h, :], in_=gm[:, h * 8:(h + 1) * 8])
            for h in range(8):
                V("tensor_scalar", [B_gm, B_top8], [B_sel], out=sel[:, h * 8:(h + 1) * 8], in0=gm[:, h * 8:(h + 1) * 8],
                  scalar1=top8[:, h, 2:3], scalar2=None, op0=ALU.is_ge)
            V("tensor_tensor", [B_sel, B_gct[s]], [B_sel], out=sel, in0=sel, in1=gct[s][:, 64:128], op=ALU.mult)
            V("tensor_tensor", [B_sel, B_gct[s]], [B_sel], out=sel, in0=sel, in1=gct[s][:, 128:192], op=ALU.add)
            V("tensor_scalar", [B_sel], [B_bp[s]], out=biasp[s][:, :, 64:72], in0=sel.rearrange("p (h j) -> p h j", h=8),
              scalar1=-1.0, scalar2=BIG, op0=ALU.add, op1=ALU.mult)
            for h in range(8):
                tr(pB[0:72, h, :], biasp[s][:, h, :], identb[:], [B_bp[s], B_["identb"]], [bkB[7]])
            act(QT[64:72, :, t * 128:(t + 1) * 128], pB[64:72, :, :], AF.Copy, [bkB[7]], [B_qtb[t]])


        def s2_steps():
            def sb_view(off_kib, shape, dt):
                return AV(int(off_kib * KiB), shape, dt)
            pagesum = [sb_view(154 + 2 * i, [128, 512], F32) for i in range(2)]
            B_ps_ = sbuf_("pagesum", [])
            for pr in range(2):
                V("tensor_reduce", [B_accs[pr]], [B_ps_], out=pagesum[pr], in_=accs[pr].rearrange("p (pos f) -> p f pos", pos=2), axis=AX.X, op=ALU.add)
            qbcs = sb_view(158, [64, 512], F32); prod = sb_view(160, [64, 512], F32)
            oB = [162 * KiB]

            def smallB(shape, dt):
                n = 1
                for x_ in shape[1:]:
                    n *= x_
                ap = AV(oB[0], shape, dt)
                oB[0] += (n * 4 + 31) // 32 * 32
                assert oB[0] <= 166 * KiB
                return ap
            gate2 = smallB([64, 16], F32); gateT = smallB([8, 128], F32); top8s = smallB([8, 4, 8], F32); oh = smallB([8, 32], F32)
            junk8 = smallB([8, 32], F32); physf = smallB([8, 24], F32); Dm = smallB([8, 8, 24], F32)
            idxf2 = smallB([128, 192], F32); idxI2 = smallB([128, 192], I32)
            B_s2 = sbuf_("s2", [])
            Bqs = Bw["qf"]; Bks = B_kf[0]; Bvs = B_vf[0]
            for pr in range(2):
                mm(bank(7)[0:64, :], cf[:, 0:64], pagesum[pr], True, True, [B_ps_, B_["cf"]], [bkB[7]])
                mm(bank(6)[0:64, :], cf[0:4, 288 + 64 * pr:288 + 64 * (pr + 1)], qf[0:4, :], True, True, [Bqs, B_["cf"]], [bkB[6]])
                act(qbcs, bank(6)[0:64, :], AF.Copy, [bkB[6]], [B_s2])
                V("tensor_tensor", [bkB[7], B_s2], [B_s2], out=prod, in0=bank(7)[0:64, :], in1=qbcs, op=ALU.mult)
                V("tensor_reduce", [B_s2], [B_s2], out=gate2[:, pr * 8:(pr + 1) * 8], in_=prod.rearrange("p (h d) -> p h d", h=8), axis=AX.X, op=ALU.add)
                tr(bank(7)[0:8, pr * 64:(pr + 1) * 64], gate2[:, pr * 8:(pr + 1) * 8], identf[0:64, 0:64], [B_s2, B_["identf"]], [bkB[7]])
                act(gateT[:, pr * 64:(pr + 1) * 64], bank(7)[0:8, pr * 64:(pr + 1) * 64], AF.Copy, [bkB[7]], [B_s2])
            S2 = [B_s2]
            for b in range(4):
                V("max", S2, S2, out=top8s[:, b, :], in_=gateT[:, b * 32:(b + 1) * 32])
                for r_ in range(3):
                    V("tensor_scalar", S2, S2, out=oh, in0=gateT[:, b * 32:(b + 1) * 32], scalar1=top8s[:, b, r_:r_ + 1], scalar2=None, op0=ALU.is_equal)
                    for hf in range(2):
                        colp = (b * 3 + r_) * 2 + hf
                        V("tensor_tensor", S2 + [B_s0], S2, out=junk8, in0=oh,
                          in1=PTf[:, b * 64:(b + 1) * 64].rearrange("p (j t) -> p j t", t=2)[:, :, hf], op=ALU.mult)
                        V("tensor_reduce", S2, S2, out=physf[:, colp:colp + 1], in_=junk8, axis=AX.X, op=ALU.add)
            for h in range(8):
                V("tensor_scalar", S2 + [B_["cf"]], S2, out=Dm[:, h, :], in0=physf, scalar1=cf[0:8, 928 + h:929 + h], scalar2=None, op0=ALU.mult)
            mm(bank(7)[:, 0:192], cf[0:8, 940:1068], Dm.rearrange("p a b -> p (a b)"), True, True, S2 + [B_["cf"]], [bkB[7]])
            V("scalar_tensor_tensor", [bkB[7], B_["cf"]], S2, out=idxf2, in0=bank(7)[:, 0:192], scalar=1024.0, in1=poshc, op0=ALU.mult, op1=ALU.add)
            V("tensor_scalar", S2, S2, out=idxf2, in0=idxf2, scalar1=0.0, scalar2=2621439.0, op0=ALU.max, op1=ALU.min)
            V("tensor_copy", S2, S2, out=idxI2, in_=idxf2)
            yield
            NSET = 4
            Ks = [sb_view(166 + 6 * i, [128, 12, 64], F32) for i in range(NSET)]
            Vs = [sb_view(169 + 6 * i, [128, 12, 64], F32) for i in range(NSET)]
            B_ks = [sbuf_("ks%d" % i, []) for i in range(NSET)]; B_vs = [sbuf_("vs%d" % i, []) for i in range(NSET)]
            qbu = [sb_view(154 + 0.5 * i, [128, 128], F32) for i in range(2)]; B_qbu = [sbuf_("qbu0", []), sbuf_("qbu1", [])]
            oC = [158 * KiB]

            def smallC(shape):
                n = 1
                for x_ in shape[1:]:
                    n *= x_
                ap = AV(oC[0], shape, F32)
                oC[0] += (n * 4 + 31) // 32 * 32
                assert oC[0] <= 162 * KiB
                return ap
            STs = smallC([128, 192]); Es = smallC([128, 192]); denp = smallC([64, 32]); sself = smallC([4, 8])
            eself = smallC([4, 8]); D2 = smallC([4, 32]); ebvt = smallC([64, 96]); numt = smallC([64, 32]); dent = smallC([64, 32])
            B_s6 = sbuf_("s6", [])
            S6 = [B_s6]
            units = [(b, hp) for b in range(4) for hp in range(4)]

            def u_gather(u):
                b, hp = units[u]
                st_ = u % NSET
                for jj in range(12):
                    h = 2 * hp + jj // 6
                    col = h * 24 + b * 6 + (jj % 6)
                    P.dma("gpsimd", lambda e, jj=jj, col=col, st_=st_: e.indirect_dma_start(
                        out=Ks[st_][:, jj, :], out_offset=None, in_=ck[:, :], in_offset=bass.IndirectOffsetOnAxis(ap=idxI2[:, col:col + 1], axis=0)),
                        S2, [B_ks[st_]])
                    P.dma("gpsimd", lambda e, jj=jj, col=col, st_=st_: e.indirect_dma_start(
                        out=Vs[st_][:, jj, :], out_offset=None, in_=cv[:, :], in_offset=bass.IndirectOffsetOnAxis(ap=idxI2[:, col:col + 1], axis=0)),
                        S2, [B_vs[st_]])

            def u_compute(u):
                b, hp = units[u]
                st_ = u % NSET
                qs_ = u % 2
                mm(bank(6)[:, 0:128], cf[0:4, 416 + 128 * b:416 + 128 * (b + 1)], qf[0:4, hp * 128:(hp + 1) * 128], True, True, [Bqs, B_["cf"]], [bkB[6]])
                act(qbu[qs_], bank(6)[:, 0:128], AF.Copy, [bkB[6]], [B_qbu[qs_]])
                for jj in range(12):
                    hh = jj // 6
                    V("tensor_tensor", [B_ks[st_], B_qbu[qs_]], [B_ks[st_]], out=Ks[st_][:, jj, :], in0=Ks[st_][:, jj, :],
                      in1=qbu[qs_][:, hh * 64:(hh + 1) * 64], op=ALU.mult)
                c0 = b * 48 + hp * 12
                V("tensor_reduce", [B_ks[st_]], S6, out=STs[:, c0:c0 + 12], in_=Ks[st_], axis=AX.X, op=ALU.add)
                act(Es[:, c0:c0 + 12], STs[:, c0:c0 + 12], AF.Exp, S6, S6)
                for hh in range(2):
                    h = 2 * hp + hh
                    for s6 in range(6):
                        cc = c0 + hh * 6 + s6
                        mm(bank(7)[0:64, 480 + b * 8 + h:480 + b * 8 + h + 1], Vs[st_][:, hh * 6 + s6, :], Es[:, cc:cc + 1], s6 == 0, s6 == 5, S6 + [B_vs[st_]], [bkB[7]])
            LAG = NSET - 1
            for u in range(16 + LAG):
                if u < 16:
                    u_gather(u)
                if u - LAG >= 0:
                    u_compute(u - LAG)
                yield
            mm(bank(7)[0:64, 0:192], cf[:, 940:1004], Es, True, True, S6 + [B_["cf"]], [bkB[7]])
            V("tensor_reduce", [bkB[7]], S6, out=denp, in_=bank(7)[0:64, 0:192].rearrange("p (g s) -> p g s", s=6), axis=AX.X, op=ALU.add)
            prodqk = misc8[0:4, :]
            V("tensor_tensor", [Bqs, Bks, B_misc8], [B_misc8], out=prodqk, in0=qf[0:4, :], in1=kf[0][0:4, :], op=ALU.mult)
            V("tensor_reduce", [B_misc8], S6, out=sself, in_=prodqk.rearrange("p (h d) -> p h d", h=8), axis=AX.X, op=ALU.add)
            act(eself, sself, AF.Exp, S6, S6)
            for b in range(4):
                V("tensor_scalar", S6 + [B_["cf"]], S6, out=D2[:, b * 8:(b + 1) * 8], in0=eself, scalar1=cf[0:4, 936 + b:937 + b], scalar2=None, op0=ALU.mult)
            mm(bank(6)[0:64, 0:32], cf[0:4, 940:1004], D2, True, True, S6 + [B_["cf"]], [bkB[6]])
            for h in range(8):
                tr(bank(6)[0:64, 64 + h * 4:64 + h * 4 + 4], vf[0][0:4, h * 64:(h + 1) * 64], identf[0:4, 0:4], [Bvs, B_["identf"]], [bkB[6]])
            act(ebvt, bank(6)[0:64, 0:96], AF.Copy, [bkB[6]], S6)
            ebc3 = ebvt[:, 0:32].rearrange("p (b h) -> p b h", b=4)
            vT3 = ebvt[:, 64:96].rearrange("p (h b) -> p b h", b=4)
            V("tensor_tensor", S6, S6, out=numt.rearrange("p (b h) -> p b h", b=4), in0=vT3, in1=ebc3, op=ALU.mult)
            V("tensor_tensor", S6 + [bkB[7]], S6, out=numt, in0=numt, in1=bank(7)[0:64, 480:512], op=ALU.add)
            V("tensor_tensor", S6, S6, out=dent, in0=denp, in1=ebvt[:, 0:32], op=ALU.add)
            V("reciprocal", S6, S6, out=dent, in_=dent)
            V("tensor_tensor", S6, [B_att[16]], out=ATT[0:64, 16, :, 0:4], in0=numt.rearrange("p (b h) -> p h b", b=4),
              in1=dent.rearrange("p (b h) -> p h b", b=4), op=ALU.mult)
            yield
        s2gen = s2_steps()

        def s2_advance(n):
            for _ in range(n):
                try:
                    next(s2gen)
                except StopIteration:
                    return
        steps = []
        for h in range(8):
            for c in range(4):
                for kt in range(4 * c + 4):
                    steps.append((h, c, kt))
        LOOK = 2
        hc_ob = {}
        for si in range(len(steps) + LOOK):
            if si < len(steps):
                h, c, kt = steps[si]
                if kt == 0:
                    it_ = h * 4 + c
                    s1_advance(4)
                    hc_ob[(h, c)] = 3 + (len(hc_ob) % 2)
                i = max(0, kt - 4 * c)
                sb = si % 3
                pi = si % 4
                qr = [B_qt[4 * c + i_] for i_ in range(4)] + [B_qtb[4 * c + i_] for i_ in range(4)]
                mm(bank(sb)[:, i * 128:512], KT[0:72, h, kt * 128:(kt + 1) * 128], QT[0:72, h, c * 512 + i * 128:(c + 1) * 512],
                   True, True, [B_kt[kt]] + qr, [bkB[sb]])
                act(Pb[pi][:, i * 128:512], bank(sb)[:, i * 128:512], AF.Exp, [bkB[sb]], [B_P[pi]])
                if kt >= 4 * c:
                    V("tensor_tensor", [B_P[pi], B_["tri"]], [B_P[pi]], out=Pb[pi][:, i * 128:(i + 1) * 128],
                      in0=Pb[pi][:, i * 128:(i + 1) * 128], in1=tri[:], op=ALU.mult)
            sj = si - LOOK
            if sj >= 0:
                h, c, kt = steps[sj]
                nkt = 4 * c + 4
                i = max(0, kt - 4 * c)
                pi = sj % 4
                ob = hc_ob[(h, c)]
                mm(bank(ob)[0:65, i * 128:512], Vb[:, kt, h, :], Pb[pi][:, i * 128:512], kt == 0, kt == nkt - 1,
                   [B_vb[kt], B_P[pi]], [bkB[ob]])
                if kt == nkt - 1:
                    oi = ob - 3
                    V("tensor_copy", [bkB[ob]], [B_osb[oi]], out=osb[oi], in_=bank(ob)[0:65, :])
                    act(osb[oi][64:65, :], osb[oi][64:65, :], AF.Ln, [B_osb[oi]], [B_osb[oi]])
                    act(osb[oi][64:65, :], osb[oi][64:65, :], AF.Exp, [B_osb[oi]], [B_osb[oi]], scale=-1.0)
                    mm(bank(5)[0:64, :], cf[64:65, 940:1004], osb[oi][64:65, :], True, True, [B_osb[oi], B_["cf"]], [bkB[5]])
                    V("tensor_tensor", [B_osb[oi], bkB[5]], [B_att[4 * c + i_] for i_ in range(4)],
                      out=ATT[0:64, 4 * c:4 * c + 4, h, :], in0=osb[oi][0:64, :].rearrange("p (a b) -> p a b", a=4),
                      in1=bank(5)[0:64, :].rearrange("p (a b) -> p a b", a=4), op=ALU.mult)
        s1_advance(1000)
        s2_advance(1000)
        att_done = B_qt + B_kt + B_vb + B_qtb
        ACC = AV(48 * KiB, [128, NT, 1024], F32); B_acc = [Buf("acc%d" % t) for t in range(NT)]
        for b_ in B_acc:
            P.alias(b_, att_done)
        WoC = AV(116 * KiB, [128, 4, 1024], BF16); B_woc = Buf("woc"); P.alias(B_woc, att_done)
        gates = AV(124 * KiB, [128, NT, 32], F32); B_gates = [Buf("gates%d" % t) for t in range(NT)]
        for b_ in B_gates:
            P.alias(b_, att_done)
        WoA = AV(146 * KiB, [64, 8, 1024], BF16); B_woa = Buf("woa")
        xr2 = [AV((162 + 4 * i) * KiB, [128, 1024], F32) for i in range(2)]; B_xr2 = [Buf("xr0"), Buf("xr1")]
        hn2 = [AV((170 + 4 * i) * KiB, [128, 1024], F32) for i in range(2)]; B_hn2 = [Buf("hn0"), Buf("hn1")]
        hnT32_2 = [AV((178 + 4 * i) * KiB, [128, 8, 128], F32) for i in range(2)]; B_hnT2 = [Buf("hnT0"), Buf("hnT1")]
        B_rs2 = []
        wstg = AV(34 * KiB, [128, 3, 1024], F32); B_wstg = Buf("wstg")
        p2_bufs = B_P + B_osb + B_bp + B_gct + [B_gm, B_top8, B_sel]
        p3_w = [B_woa] + B_xr2 + B_hn2 + B_hnT2 + B_rs2
        for b_ in p3_w + [B_wstg]:
            P.alias(b_, all_p1 + SAMPLE_BUFS + p2_bufs)
        for hg in ((0, 1, 2), (3, 4, 5), (6, 7)):
            n = len(hg)
            dma("sync", wstg[0:64, 0:n, :], w_out[hg[0] * 64:(hg[-1] + 1) * 64, :].rearrange("(h r) n -> r h n", r=64), w=[B_wstg])
            V("tensor_copy", [B_wstg], [B_woa], out=WoA[:, hg[0]:hg[0] + n, :], in_=wstg[0:64, 0:n, :])
        for cg in ((0, 1, 2), (3,)):
            n = len(cg)
            dma("sync", wstg[:, 0:n, :], w_out[512 + cg[0] * 128:512 + (cg[-1] + 1) * 128, :].rearrange("(c p) n -> p c n", p=128), w=[B_wstg])
            V("tensor_copy", [B_wstg], [B_woc], out=WoC[:, cg[0]:cg[0] + n, :], in_=wstg[:, 0:n, :])
        dma("sync", gbc[:], g_ffn.partition_broadcast(128), w=[B_["gbc"]])
        pT32 = PS[2].rearrange("p (a b) -> p a b", a=8)
        lgall = AV(186 * KiB, [128, NT, 36], F32); B_lgall = Buf("lgall")
        P.alias(B_lgall, all_p1 + SAMPLE_BUFS + p2_bufs)

        def p3_W(t):
            hps = PS[t % 2]
            Bh = [bkB[2 * (t % 2)], bkB[2 * (t % 2) + 1]]
            for half in range(2):
                for h in range(8):
                    mm(hps[:, half * 512:(half + 1) * 512], ATT[0:64, t, h, :], WoA[:, h, half * 512:(half + 1) * 512], h == 0, False,
                       [B_att[t], B_woa], Bh)
                for ct in range(4):
                    mm(hps[:, half * 512:(half + 1) * 512], convT[:, t, ct, :], WoC[:, ct, half * 512:(half + 1) * 512], False, ct == 3,
                       [B_ct[t], B_woc], Bh)
            i = t % 2
            if t < 16:
                dma("sync", xr2[i], xp[t * 128:(t + 1) * 128, :], w=[B_xr2[i]])
            else:
                V("memset", [], [B_xr2[i]], eng="gpsimd", ap=xr2[i], constant=0.0)
                dma("sync", xr2[i][0:4, :], xs, w=[B_xr2[i]])

        def p3_rest(t):
            i = t % 2
            hps = PS[i]; Bh = [bkB[2 * i], bkB[2 * i + 1]]
            hn = hn2[i]; B_hn = B_hn2[i]; hnT32 = hnT32_2[i]; B_hnT32 = B_hnT2[i]
            V("tensor_tensor", Bh + [B_xr2[i]], [B_acc[t]], out=ACC[:, t, :], in0=hps[:, :], in1=xr2[i], op=ALU.add)
            col = NT + t
            act(hn, ACC[:, t, :], AF.Square, [B_acc[t]], [B_hn, B_ssq[col]], accum_out=ssq[:, col:col + 1])
            act(rt[:, col:col + 1], ssq[:, col:col + 1], AF.Sqrt, [B_ssq[col], B_["epsb"]], [B_ssq[col]], scale=1.0 / 1024.0, bias=epsb[:, 0:1])
            V("reciprocal", [B_ssq[col]], [B_ssq[col]], out=rstd[:, col:col + 1], in_=rt[:, col:col + 1])
            V("scalar_tensor_tensor", [B_acc[t], B_ssq[col], B_["gbc"]], [B_hn], out=hn, in0=ACC[:, t, :], scalar=rstd[:, col:col + 1],
              in1=gbc[:], op0=ALU.mult, op1=ALU.mult)
            for c in range(8):
                tr(pT32[:, c, :], hn[:, c * 128:(c + 1) * 128], identf[:], [B_hn, B_["identf"]], [bkB[4], bkB[5]])
            act(hnT32, pT32, AF.Copy, [bkB[4], bkB[5]], [B_hnT32])
            V("tensor_copy", [B_hnT32], [B_att[t]], eng="gpsimd", out=ATT[:, t, :, :], in_=hnT32)
            for c in range(8):
                mm(bank(6)[:, 0:36], hnT32[:, c, :], wr[:, c, :], c == 0, c == 7, [B_hnT32, B_["wr"]], [bkB[6]])
            V("tensor_tensor", [bkB[6], B_["rb"]], [B_lgall], out=lgall[:, t, :], in0=bank(6)[:, 0:36], in1=rb[:], op=ALU.add)

        p3_W(0)
        for t in range(NT):
            if t + 1 < NT:
                p3_W(t + 1)
            p3_rest(t)

        ob_ = [162 * KiB]

        def rtile(shape):
            n = 1
            for x_ in shape[1:]:
                n *= x_
            ap = AV(ob_[0], shape, F32)
            ob_[0] += (n * 4 + 31) // 32 * 32
            assert ob_[0] <= 170 * KiB
            return ap
        B_rt = Buf("rtmp"); P.alias(B_rt, B_xr2 + B_hn2)
        Rr = [B_rt]
        m4 = rtile([128, NT]); ohg = rtile([128, NT, 4]); eg = rtile([128, NT, 4]); sg4 = rtile([128, NT]); pg = rtile([128, NT])
        leg = rtile([128, NT, 8]); tmp8 = rtile([128, NT, 8]); t8a = rtile([128, NT, 8]); dd = rtile([128, NT]); w1 = rtile([128, NT]); w2 = rtile([128, NT])
        e1 = rtile([128, NT, 8]); e2 = rtile([128, NT, 8]); gi = rtile([128, NT, 8])
        lgg = lgall[:, :, 0:4]
        lge = lgall[:, :, 4:36].rearrange("p t (g i) -> p t g i", g=4)

        def bc(ap2, n):
            return ap2.unsqueeze(2).to_broadcast([128, NT, n])
        V("tensor_reduce", [B_lgall], Rr, out=m4, in_=lgg, axis=AX.X, op=ALU.max)
        V("tensor_tensor", [B_lgall] + Rr, Rr, out=ohg, in0=lgg, in1=bc(m4, 4), op=ALU.is_ge)
        V("tensor_tensor", [B_lgall] + Rr, Rr, out=eg, in0=lgg, in1=bc(m4, 4), op=ALU.subtract)
        act(eg, eg, AF.Exp, Rr, Rr)
        V("tensor_reduce", Rr, Rr, out=sg4, in_=eg, axis=AX.X, op=ALU.add)
        V("reciprocal", Rr, Rr, out=pg, in_=sg4)
        for g_ in range(4):
            if g_ == 0:
                V("tensor_tensor", [B_lgall] + Rr, Rr, out=leg, in0=lge[:, :, 0, :], in1=bc(ohg[:, :, 0], 8), op=ALU.mult)
            else:
                V("tensor_tensor", [B_lgall] + Rr, Rr, out=tmp8, in0=lge[:, :, g_, :], in1=bc(ohg[:, :, g_], 8), op=ALU.mult)
                V("tensor_tensor", Rr, Rr, out=leg, in0=leg, in1=tmp8, op=ALU.add)
        for t in range(NT):
            V("max", Rr, Rr, out=t8a[:, t, :], in_=leg[:, t, :])
        V("tensor_tensor", Rr, Rr, out=dd, in0=t8a[:, :, 1], in1=t8a[:, :, 0], op=ALU.subtract)
        act(w2, dd, AF.Exp, Rr, Rr)
        V("tensor_scalar", Rr, Rr, out=w1, in0=w2, scalar1=1.0, scalar2=None, op0=ALU.add)
        V("reciprocal", Rr, Rr, out=w1, in_=w1)
        V("tensor_tensor", Rr, Rr, out=w2, in0=w2, in1=w1, op=ALU.mult)
        V("tensor_tensor", Rr, Rr, out=w1, in0=w1, in1=pg, op=ALU.mult)
        V("tensor_tensor", Rr, Rr, out=w2, in0=w2, in1=pg, op=ALU.mult)
        V("tensor_tensor", Rr, Rr, out=e1, in0=leg, in1=bc(t8a[:, :, 0], 8), op=ALU.is_equal)
        V("tensor_tensor", Rr, Rr, out=e1, in0=e1, in1=bc(w1, 8), op=ALU.mult)
        V("tensor_tensor", Rr, Rr, out=e2, in0=leg, in1=bc(t8a[:, :, 1], 8), op=ALU.is_equal)
        V("tensor_tensor", Rr, Rr, out=e2, in0=e2, in1=bc(w2, 8), op=ALU.mult)
        V("tensor_tensor", Rr, Rr, out=gi, in0=e1, in1=e2, op=ALU.add)
        gates4 = gates.rearrange("p t (g i) -> p t g i", g=4)
        for g_ in range(4):
            V("tensor_tensor", Rr, B_gates, out=gates4[:, :, g_, :], in0=gi, in1=bc(ohg[:, :, g_], 8), op=ALU.mult)

        p3_done = p3_w + [B_wstg, B_woc, B_lgall, B_rt] + B_ct
        WB0 = 129 * KiB
        NSTG = 3
        estg = [AV(34 * KiB + i * 4 * KiB, [128, 1024], F32) for i in range(NSTG)]; B_estg = [Buf("estg%d" % i) for i in range(NSTG)]
        Wgu = [AV(WB0 + i * 12 * KiB, [128, 8, 512], BF16) for i in range(4)]
        Wd = [AV(WB0 + i * 12 * KiB + 8 * KiB, [128, 2, 1024], BF16) for i in range(4)]
        B_wgu = [Buf("wgu%d" % i) for i in range(4)]; B_wd = [Buf("wd%d" % i) for i in range(4)]
        o = WB0 + 48 * KiB
        sgb = [AV(o + i * KiB, [128, 256], F32) for i in range(2)]; o += 2 * KiB
        hbb = [AV(o + i * 512, [128, 256], BF16) for i in range(2)]; o += KiB
        hTb = [AV(o + i * 512, [128, 2, 128], BF16) for i in range(2)]; o += KiB
        assert o <= ARENA
        B_sg = [Buf("sg0"), Buf("sg1")]; B_hb = [Buf("hb0"), Buf("hb1")]; B_hT = [Buf("hT0"), Buf("hT1")]
        for b_ in B_estg + B_wgu + B_wd + B_sg + B_hb + B_hT:
            P.alias(b_, p3_done + p2_bufs + all_p1 + SAMPLE_BUFS)
        sti = [0]

        def load_expert(e, slot):
            for (src, coff) in ((w_gate, 0), (w_up, 256)):
                for hf in range(2):
                    i = sti[0] % NSTG; sti[0] += 1
                    dma("sync", estg[i].rearrange("p (c f) -> p c f", c=4),
                        src[e, hf * 512:(hf + 1) * 512, :].rearrange("(c p) f -> p c f", p=128), w=[B_estg[i]])
                    V("tensor_copy", [B_estg[i]], [B_wgu[slot]], eng="gpsimd", out=Wgu[slot][:, hf * 4:(hf + 1) * 4, coff:coff + 256],
                      in_=estg[i].rearrange("p (c f) -> p c f", c=4))
            for f_ in range(2):
                i = sti[0] % NSTG; sti[0] += 1
                dma("sync", estg[i], w_down[e, f_ * 128:(f_ + 1) * 128, :], w=[B_estg[i]])
                V("tensor_copy", [B_estg[i]], [B_wd[slot]], eng="gpsimd", out=Wd[slot][:, f_, :], in_=estg[i])

        pTh4 = bank(3).bitcast(BF16).rearrange("p (a b) -> p a b", a=8)
        B_pth = [Buf("pth%d" % i) for i in range(4)]
        for b_ in B_pth:
            b_.last_w = bkB[3].last_w; b_.readers = list(bkB[3].readers)
        sg3 = [AV(o_, [128, 256], F32) for o_ in (WB0 + 48 * KiB, WB0 + 49 * KiB, WB0 + 52 * KiB)]
        hb3 = [AV(WB0 + 50 * KiB + i * 512, [128, 256], BF16) for i in range(3)]
        hT4 = [AV(WB0 + 53 * KiB + i * 512, [128, 2, 128], BF16) for i in range(4)]
        assert WB0 + 55 * KiB <= ARENA
        B_sg3 = [Buf("sg3_%d" % i) for i in range(3)]; B_hb3 = [Buf("hb3_%d" % i) for i in range(3)]; B_hT4 = [Buf("hT4_%d" % i) for i in range(4)]
        for b_ in B_sg3 + B_hb3 + B_hT4:
            P.alias(b_, p3_done + p2_bufs + all_p1 + SAMPLE_BUFS + B_sg + B_hb + B_hT)
        load_expert(0, 0); load_expert(1, 1)
        msteps = []
        for ep in range(NE // 2):
            for t in range(NT):
                for k in range(2):
                    msteps.append((ep, t, k))

        def emit_G(i):
            ep, t, k = msteps[i]
            e = 2 * ep + k; slot = e % 4; gb = i % 3
            for c in range(8):
                mm(bank(gb), ATT[:, t, c, :], Wgu[slot][:, c, :], c == 0, c == 7, [B_att[t], B_wgu[slot]], [bkB[gb]])

        def emit_A(i):
            ep, t, k = msteps[i]
            e = 2 * ep + k; gb = i % 3
            act(sg3[gb], bank(gb)[:, 0:256], AF.Silu, [bkB[gb]], [B_sg3[gb]])
            V("scalar_tensor_tensor", [bkB[gb], B_sg3[gb], B_gates[t]], [B_hb3[gb]], out=hb3[gb], in0=bank(gb)[:, 256:512],
              scalar=gates[:, t, e:e + 1], in1=sg3[gb], op0=ALU.mult, op1=ALU.mult)

        def emit_T(i):
            gb = i % 3; ts = i % 4
            for f_ in range(2):
                tr(pTh4[:, 2 * ts + f_, :], hb3[gb][:, f_ * 128:(f_ + 1) * 128], identb[:], [B_hb3[gb], B_["identb"]], [B_pth[ts]])
            act(hT4[ts], pTh4[:, 2 * ts:2 * ts + 2, :], AF.Copy, [B_pth[ts]], [B_hT4[ts]])

        def emit_D(i):
            ep, t, k = msteps[i]
            if t == 0 and k == 0 and ep + 1 < NE // 2:
                load_expert(2 * ep + 2, (2 * ep + 2) % 4); load_expert(2 * ep + 3, (2 * ep + 3) % 4)
            e = 2 * ep + k; slot = e % 4; ts = i % 4
            ob = 2 + ((i // 2) % 2)
            outp = PS[ob]; Bout = [bkB[2 * ob], bkB[2 * ob + 1]]
            for half in range(2):
                for f_ in range(2):
                    mm(outp[:, half * 512:(half + 1) * 512], hT4[ts][:, f_, :], Wd[slot][:, f_, half * 512:(half + 1) * 512],
                       k == 0 and f_ == 0, k == 1 and f_ == 1, [B_hT4[ts], B_wd[slot]], Bout)
            if k == 1:
                V("tensor_tensor", Bout + [B_acc[t]], [B_acc[t]], out=ACC[:, t, :], in0=outp[:, :], in1=ACC[:, t, :], op=ALU.add)

        NS = len(msteps)
        emit_G(0)
        for i in range(NS + 1):
            if i + 1 < NS:
                emit_G(i + 1)
            if i < NS:
                emit_A(i)
                emit_T(i)
            if i - 1 >= 0:
                emit_D(i - 1)

        dma("sync", gbc[:], g_final.partition_broadcast(128), w=[B_["gbc"]])
        yb = [estg[0], estg[1]]; B_yb = [B_estg[0], B_estg[1]]
        jk5 = estg[2]; B_jk5 = B_estg[2]
        for t in range(NT):
            col = 2 * NT + t
            i = t % 2
            act(jk5, ACC[:, t, :], AF.Square, [B_acc[t]], [B_jk5, B_ssq[col]], accum_out=ssq[:, col:col + 1])
            act(rt[:, col:col + 1], ssq[:, col:col + 1], AF.Sqrt, [B_ssq[col], B_["epsb"]], [B_ssq[col]], scale=1.0 / 1024.0, bias=epsb[:, 0:1])
            V("reciprocal", [B_ssq[col]], [B_ssq[col]], out=rstd[:, col:col + 1], in_=rt[:, col:col + 1])
            V("scalar_tensor_tensor", [B_acc[t], B_ssq[col], B_["gbc"]], [B_yb[i]], out=yb[i], in0=ACC[:, t, :], scalar=rstd[:, col:col + 1],
              in1=gbc[:], op0=ALU.mult, op1=ALU.mult)
            if t < 16:
                dma("sync", yp[t * 128:(t + 1) * 128, :], yb[i], r=[B_yb[i]])
            else:
                dma("sync", ys, yb[i][0:4, :], r=[B_yb[i]])
        P.run()
    return nc


_NC = None


def kernel(**inputs):
    global _NC
    if _NC is None:
        _NC = build_program()
    nc = _NC
    f = lambda a: np.ascontiguousarray(np.asarray(a))
    consts = host_consts()
    ckf = f(inputs["cache_k"]).reshape(2621440, 64)
    cvf = f(inputs["cache_v"]).reshape(2621440, 64)
    shared = {
        "ck": ckf, "cv": cvf,
        "g_mix": f(inputs["g_mix"]).reshape(1024), "w_in": f(inputs["w_in"]).reshape(1024, 3072),
        "conv_w": f(inputs["conv_w"]).reshape(3, 512), "w_out": f(inputs["w_out"]).reshape(1024, 1024),
        "g_ffn": f(inputs["g_ffn"]).reshape(1024), "w_rg": f(inputs["w_router_group"]).reshape(1024, 4),
        "b_rg": f(inputs["b_router_group"]).reshape(4), "w_re": f(inputs["w_router_expert"]).reshape(1024, 32),
        "b_re": f(inputs["b_router_expert"]).reshape(32), "w_gate": f(inputs["w_gate"]).reshape(32, 1024, 256),
        "w_up": f(inputs["w_up"]).reshape(32, 1024, 256), "w_down": f(inputs["w_down"]).reshape(32, 256, 1024),
        "g_final": f(inputs["g_final"]).reshape(1024),
    }
    shared.update(consts)
    xpr = f(inputs["x_prompt"]); xsm = f(inputs["x_sample"]).reshape(32, 1024)
    scv = f(inputs["state_conv"]).reshape(32, 2, 512); ptb = f(inputs["page_table"]).astype(np.int32)
    in_maps = []
    for c in range(NCORES):
        m = dict(shared)
        m["xp"] = xpr[c]
        m["xs"] = xsm[4 * c:4 * c + 4]
        m["sc"] = scv[4 * c:4 * c + 4].reshape(8, 512)
        m["pt"] = ptb[4 * c:4 * c + 4]
        in_maps.append(m)
    res = run_bass_kernel_spmd(nc, in_maps, core_ids=list(range(NCORES)))
    R = res.results
    y_prompt = np.stack([R[c]["yp"] for c in range(NCORES)]).reshape(8, 2048, 1024)
    y_sample = np.concatenate([R[c]["ys"] for c in range(NCORES)]).reshape(32, 1, 1024)
    k_prompt = np.stack([R[c]["kp"] for c in range(NCORES)]).reshape(1, 8, 2048, 8, 64)
    v_prompt = np.stack([R[c]["vp"] for c in range(NCORES)]).reshape(1, 8, 2048, 8, 64)
    conv_prompt = np.stack([R[c]["cp"] for c in range(NCORES)]).reshape(1, 8, 2, 512)
    k_sample = np.concatenate([R[c]["ks"] for c in range(NCORES)]).reshape(1, 32, 1, 8, 64)
    v_sample = np.concatenate([R[c]["vs"] for c in range(NCORES)]).reshape(1, 32, 1, 8, 64)
    conv_sample = np.concatenate([R[c]["cso"] for c in range(NCORES)]).reshape(1, 32, 2, 512)
    return tuple(np.asarray(a, dtype=np.float32) for a in
                 (y_prompt, y_sample, k_prompt, v_prompt, conv_prompt, k_sample, v_sample, conv_sample))
```

```python
import contextlib
import numpy as np
import ml_dtypes
import concourse.bass as bass
import concourse.mybir as mybir
from concourse.bass_utils import run_bass_kernel_spmd

F32 = mybir.dt.float32
BF16 = mybir.dt.bfloat16
I32 = mybir.dt.int32
U8 = mybir.dt.uint8
AF = mybir.ActivationFunctionType
ALU = mybir.AluOpType
AX = mybir.AxisListType

ENGS = ("sync", "scalar", "vector", "gpsimd", "tensor")
NCORES = 8
NT = 17
NE = 32
BIG = 30000.0
KiB = 1024


class Buf:
    __slots__ = ("name", "last_w", "readers")

    def __init__(self, name):
        self.name = name
        self.last_w = None
        self.readers = []


class Ins:
    __slots__ = ("eng", "fn", "deps", "signal", "sigval", "is_dma", "dsem", "dval")

    def __init__(self, eng, fn, is_dma):
        self.eng = eng
        self.fn = fn
        self.deps = []
        self.signal = False
        self.sigval = None
        self.is_dma = is_dma
        self.dsem = None
        self.dval = None


class Prog:
    def __init__(self, nc, ring=8):
        self.nc = nc
        self.q = {e: [] for e in ENGS}
        self.ringd = {e: ring for e in ENGS}
        self.ringd["gpsimd"] = 8
        self.dma_count = {e: 0 for e in ENGS}
        self.dma_hist = {e: [] for e in ENGS}

    def _add(self, ins, r, w):
        deps = []
        for b in r:
            if b.last_w is not None:
                deps.append(b.last_w)
        for b in w:
            if b.last_w is not None:
                deps.append(b.last_w)
            deps.extend(b.readers)
        for b in w:
            b.last_w = ins
            b.readers = []
        for b in r:
            if b.last_w is not ins:
                b.readers.append(ins)
        seen = set()
        for d in deps:
            if d is ins or id(d) in seen:
                continue
            seen.add(id(d))
            if (not d.is_dma) and d.eng == ins.eng and d.eng == "tensor" and not ins.is_dma:
                continue
            ins.deps.append(d)
            if not d.is_dma:
                d.signal = True
        self.q[ins.eng].append(ins)
        return ins

    def op(self, eng, fn, r=(), w=()):
        return self._add(Ins(eng, fn, False), list(r), list(w))

    def dma(self, eng, fn, r=(), w=()):
        ins = Ins(eng, fn, True)
        k = self.dma_count[eng]
        self.dma_count[eng] += 1
        hist = self.dma_hist[eng]
        ring = self.ringd[eng]
        if k >= ring:
            ins.deps.append(hist[k - ring])
        hist.append(ins)
        ins.dsem = (eng, k % ring)
        ins.dval = 16 * (k // ring + 1)
        return self._add(ins, list(r), list(w))

    def alias(self, new, olds):
        for o in olds:
            if o.last_w is not None:
                new.readers.append(o.last_w)
            new.readers.extend(o.readers)

    def run(self):
        nc = self.nc
        with contextlib.ExitStack() as st:
            esem = {e: st.enter_context(nc.semaphore("es_" + e)) for e in ENGS}
            dsem = {}
            for e in ENGS:
                for i in range(min(self.ringd[e], self.dma_count[e])):
                    dsem[(e, i)] = st.enter_context(nc.semaphore("ds_%s%d" % (e, i)))
            for e in ENGS:
                c = 0
                for ins in self.q[e]:
                    if (not ins.is_dma) and ins.signal:
                        c += 1
                        ins.sigval = c
            allsems = list(esem.values()) + list(dsem.values())
            for s_ in allsems:
                nc.gpsimd.sem_clear(s_)
            nc.all_engine_barrier()
            block = nc.Block()
            block.__enter__()

            def make(ename):
                def body(eng):
                    known = {}
                    for ins in self.q[ename]:
                        need = {}
                        for d in ins.deps:
                            if d.is_dma:
                                s, v = dsem[d.dsem], d.dval
                            else:
                                s, v = esem[d.eng], d.sigval
                            key = id(s)
                            if known.get(key, 0) >= v:
                                continue
                            if key not in need or need[key][1] < v:
                                need[key] = (s, v)
                        for key, (s, v) in need.items():
                            eng.wait_ge(s, v)
                            known[key] = v
                        r = ins.fn(eng)
                        if ins.is_dma:
                            r.then_inc(dsem[ins.dsem], 16)
                        elif ins.signal:
                            r.then_inc(esem[ename], 1)
                    for ins in self.dma_hist[ename][-self.ringd[ename]:]:
                        s, v = dsem[ins.dsem], ins.dval
                        if known.get(id(s), 0) < v:
                            eng.wait_ge(s, v)
                            known[id(s)] = v
                return body

            for e in ENGS:
                if self.q[e]:
                    getattr(block, e)(make(e))
            block.__exit__(None, None, None)
            st.pop_all()
            nc.all_engine_barrier()
            for s_ in allsems:
                nc.gpsimd.sem_clear(s_)
            nc.all_engine_barrier()


NFC = 1068


def host_consts():
    c = {}
    c["c_identb"] = np.eye(128, dtype=np.float32).astype(ml_dtypes.bfloat16)
    c["c_identf"] = np.eye(128, dtype=np.float32)
    kk = np.arange(128)[:, None]
    qq = np.arange(128)[None, :]
    c["c_tri"] = (qq >= kk).astype(np.float32).astype(ml_dtypes.bfloat16)
    inv = (500000.0 ** (-np.arange(0, 16, 2, dtype=np.float32) / np.float32(16))).astype(np.float32)
    cs = np.zeros((NT, 128, 128), np.float32)
    for t in range(NT):
        pos = (t * 128 + np.arange(128)) if t < 16 else np.full(128, 8192)
        ang = pos.astype(np.float32)[:, None] * inv[None, :]
        ang = ang.astype(np.float32).astype(np.float64)
        cs[t, :, 0:64] = np.tile(np.cos(ang), (1, 8))
        cs[t, :, 64:128] = np.tile(np.sin(ang), (1, 8))
    c["c_cs"] = cs
    g = np.zeros((16, 128, 192), np.float32)
    for t in range(16):
        own = (t * 128 + np.arange(128)) // 256
        j = np.arange(8)[None, :]
        past = (j < own[:, None]).astype(np.float32)
        ownm = (j == own[:, None]).astype(np.float32)
        g[t, :, 0:64] = np.tile((past - 1.0) * 1e30, (1, 8))
        g[t, :, 64:128] = np.tile(past, (1, 8))
        g[t, :, 128:192] = np.tile(ownm, (1, 8))
    c["c_g"] = g
    blk = np.zeros((8, 2048), np.float32)
    for j in range(8):
        blk[j, j * 256:(j + 1) * 256] = 1.0
    c["c_blk"] = blk.astype(ml_dtypes.bfloat16)
    f = np.zeros((128, NFC), np.float32)
    p = np.arange(128)
    f[p, p // 2] = 1.0
    for h in range(8):
        for b in range(4):
            for r in range(3):
                for hf in range(2):
                    col = 64 + ((h * 4 + b) * 3 + r) * 2 + hf
                    f[:, col] = p * 8 + h
    f[:, 256:288] = np.arange(32)[None, :]
    for pr in range(2):
        for m in range(64):
            f[2 * pr + m // 32, 288 + pr * 64 + m] = 1.0
    for b in range(4):
        f[b, 416 + b * 128: 416 + (b + 1) * 128] = 1.0
    f[0:8, 928:936] = np.eye(8)
    f[0:4, 936:940] = np.eye(4)
    f[:, 940:1068] = 1.0
    c["c_f"] = f
    return c


def build_program(debug=False):
    nc = bass.Bass("TRN2", target_bir_lowering=False)
    P = Prog(nc)

    def din(name, shape, dt=F32):
        return nc.dram_tensor(name, list(shape), dt, kind="ExternalInput").ap()

    def dout(name, shape, dt=F32):
        return nc.dram_tensor(name, list(shape), dt, kind="ExternalOutput").ap()

    xp = din("xp", [2048, 1024]); xs = din("xs", [4, 1024])
    ck = din("ck", [2621440, 64]); cv = din("cv", [2621440, 64])
    sc = din("sc", [8, 512]); pt = din("pt", [4, 64], I32)
    g_mix = din("g_mix", [1024]); w_in = din("w_in", [1024, 3072]); conv_w = din("conv_w", [3, 512])
    w_out = din("w_out", [1024, 1024]); g_ffn = din("g_ffn", [1024])
    w_rg = din("w_rg", [1024, 4]); b_rg = din("b_rg", [4]); w_re = din("w_re", [1024, 32]); b_re = din("b_re", [32])
    w_gate = din("w_gate", [32, 1024, 256]); w_up = din("w_up", [32, 1024, 256]); w_down = din("w_down", [32, 256, 1024])
    g_final = din("g_final", [1024])
    c_identb = din("c_identb", [128, 128], BF16); c_identf = din("c_identf", [128, 128])
    c_tri = din("c_tri", [128, 128], BF16); c_cs = din("c_cs", [NT, 128, 128]); c_g = din("c_g", [16, 128, 192])
    c_blk = din("c_blk", [8, 2048], BF16); c_f = din("c_f", [128, NFC])
    yp = dout("yp", [2048, 1024]); ys = dout("ys", [4, 1024])
    kp = dout("kp", [2048, 512]); vp = dout("vp", [2048, 512]); cp = dout("cp", [2, 512])
    ks = dout("ks", [4, 512]); vs = dout("vs", [4, 512]); cso = dout("cso", [8, 512])
    ckc = ck.rearrange("(a b) d -> a (b d)", b=16)

    st = contextlib.ExitStack()
    with st:
        ARENA = 190 * KiB
        arena = st.enter_context(nc.sbuf_tensor("arena", [128, ARENA], U8))

        def AV(off, shape, dt, p0=0):
            isz = 2 if dt == BF16 else 4
            n = 1
            for s in shape[1:]:
                n *= s
            nb = n * isz
            assert off + nb <= ARENA, (off, nb)
            ap = arena[p0:p0 + shape[0], off:off + nb].bitcast(dt)
            if len(shape) == 3:
                ap = ap.rearrange("p (a b) -> p a b", a=shape[1])
            elif len(shape) == 4:
                ap = ap.rearrange("p (a b c) -> p a b c", a=shape[1], b=shape[2])
            return ap

        def ST(name, shape, dt):
            return st.enter_context(nc.sbuf_tensor(name, list(shape), dt))

        PS = [st.enter_context(nc.psum_tensor("ps%d" % i, [128, 1024], F32)) for i in range(4)]

        def bank(i):
            return PS[i // 2][:, (i % 2) * 512:(i % 2 + 1) * 512]

        bkB = [P_buf for P_buf in (Buf("bank%d" % i) for i in range(8))]

        identb = ST("identb", [128, 128], BF16); identf = ST("identf", [128, 128], F32); tri = ST("tri", [128, 128], BF16)
        cf = ST("cf", [128, NFC], F32)
        gbc = ST("gbc", [128, 1024], F32)
        ssq = ST("ssq", [128, 3 * NT], F32); rt = ST("rt", [128, 3 * NT], F32); rstd = ST("rstd", [128, 3 * NT], F32)
        convw = ST("convw", [128, 12], F32)
        ksum = ST("ksum", [64, 64], F32); ksumhi = ST("ksumhi", [64, 64], BF16); ksumlo = ST("ksumlo", [64, 64], BF16)
        ksumr = ST("ksumr", [64, 64], F32)
        epsb = ST("epsb", [128, 1], F32)
        rb = ST("rb", [128, 36], F32)
        wr = ST("wr", [128, 8, 36], F32)
        B_ = {n: Buf(n) for n in ("identb identf tri cf gbc convw ksum ksumhi ksumlo ksumr epsb rb wr").split()}
        B_ssq = [Buf("ssq%d" % i) for i in range(3 * NT)]

        def dma(q, out, in_, r=(), w=()):
            return P.dma(q, lambda e: e.dma_start(out=out, in_=in_), r, w)

        def act(out, in_, func, r, w, **kw):
            return P.op("scalar", lambda e: e.activation(out=out, in_=in_, func=func, **kw), r, w)

        def mm(out, lhsT, rhs, start, stop, r, w):
            return P.op("tensor", lambda e: e.matmul(out, lhsT=lhsT, rhs=rhs, start=start, stop=stop), r, w)

        def tr(out, in_, ident, r, w):
            return P.op("tensor", lambda e: e.transpose(out=out, in_=in_, identity=ident), r, w)

        def V(name, r, w, eng="vector", **kw):
            return P.op(eng, lambda e: getattr(e, name)(**kw), r, w)

        dma("sync", identb[:], c_identb, w=[B_["identb"]])
        dma("sync", identf[:], c_identf, w=[B_["identf"]])
        dma("sync", tri[:], c_tri, w=[B_["tri"]])
        dma("sync", cf[:], c_f, w=[B_["cf"]])
        dma("sync", gbc[:], g_mix.partition_broadcast(128), w=[B_["gbc"]])
        V("memset", [], [B_["epsb"]], ap=epsb[:], constant=1e-6)
        PairM = cf[:, 0:64]; poshc = cf[:, 64:256]; chunkid = cf[:, 256:288]
        ones_f = cf[:, 940:1068]

        cw12 = ST("cw12", [12, 128], F32); Bcw12 = Buf("cw12")
        dma("sync", cw12[:], conv_w.rearrange("k (t p) -> (k t) p", p=128), w=[Bcw12])
        tr(bank(0)[:, 0:12], cw12[:], identf[0:12, 0:12], [Bcw12, B_["identf"]], [bkB[0]])
        act(convw[:], bank(0)[:, 0:12], AF.Copy, [bkB[0]], [B_["convw"]])
        dma("sync", wr[:, :, 0:4], w_rg.rearrange("(c p) n -> p c n", p=128), w=[B_["wr"]])
        dma("sync", wr[:, :, 4:36], w_re.rearrange("(c p) n -> p c n", p=128), w=[B_["wr"]])
        dma("sync", rb[:, 0:4], b_rg.partition_broadcast(128), w=[B_["rb"]])
        dma("sync", rb[:, 4:36], b_re.partition_broadcast(128), w=[B_["rb"]])

        WinB = AV(0, [128, 8, 3072], BF16); B_win = [Buf("win%d" % c) for c in range(8)]
        W0 = 146 * KiB
        stg = [AV(W0 + i * 12 * KiB, [128, 3072], F32) for i in range(2)]
        B_stg = [Buf("stg%d" % i) for i in range(2)]
        for c in range(8):
            s = c % 2
            dma("sync", stg[s], w_in[c * 128:(c + 1) * 128, :], w=[B_stg[s]])
            V("tensor_copy", [B_stg[s]], [B_win[c]], eng="vector", out=WinB[:, c, :], in_=stg[s])

        QT = AV(48 * KiB, [72, 8, 2048], BF16); KT = AV(80 * KiB, [72, 8, 2048], BF16)
        Vb = AV(112 * KiB, [128, 16, 8, 65], BF16)
        convT = AV(129 * KiB, [128, NT, 4, 128], BF16)
        B_qt = [Buf("qt%d" % t) for t in range(16)]; B_kt = [Buf("kt%d" % t) for t in range(16)]
        B_vb = [Buf("vb%d" % t) for t in range(16)]; B_ct = [Buf("convT%d" % t) for t in range(NT)]
        B_qtb = [Buf("qtb%d" % t) for t in range(16)]
        V("memset", [], B_vb, eng="gpsimd", ap=Vb[:, :, :, 64:65], constant=1.0)
        for h in range(8):
            dma("sync", KT[64:72, h, :], c_blk, w=B_kt)
        V("memset", [], [B_ct[16]], eng="gpsimd", ap=convT[:, 16, :, :], constant=0.0)

        o = W0
        qf = AV(o, [128, 512], F32); o += 2 * KiB
        kf = [AV(o + i * 2 * KiB, [128, 512], F32) for i in range(2)]; o += 4 * KiB
        vf = [AV(o, [128, 512], F32)] * 2; o += 2 * KiB
        xt = [AV(o + i * 4 * KiB, [128, 1024], F32) for i in range(2)]; o += 8 * KiB
        xnb = AV(o, [128, 1024], BF16); o += 2 * KiB
        xnTg = AV(o, [128, 8, 512], BF16); o += 8 * KiB
        qb = AV(o, [128, 512], BF16); o += 1 * KiB
        kb = AV(o, [128, 512], BF16); o += 1 * KiB
        rtq = [AV(o + i * 256, [128, 8, 8], F32) for i in range(4)]; o += 1 * KiB
        rtk = [AV(o + i * 256, [128, 8, 8], F32) for i in range(4)]; o += 1 * KiB
        Hs = AV(o, [128, 512], F32); o += 2 * KiB
        ctmp = AV(o, [128, 512], F32); o += 2 * KiB
        ug = AV(o, [128, 4, 514], F32); o += 8224 + 32
        cst = [AV(o + i * 512, [128, 128], F32) for i in range(2)]; o += 1 * KiB
        assert o <= ARENA, o
        Bw = {n: Buf(n) for n in "xnb xnTg qf qb kb rtq rtk Hs ctmp ug".split()}
        B_xt = [Buf("xt0"), Buf("xt1")]; B_kf = [Buf("kf0"), Buf("kf1")]; B_vf = [Buf("vf0")] * 2
        B_cst = [Buf("cst0"), Buf("cst1")]
        for b_ in list(Bw.values()) + B_xt + B_kf + B_vf[:1] + B_cst:
            P.alias(b_, B_stg)
        V("memset", [], [Bw["ug"]], eng="gpsimd", ap=ug[:, :, 0:2], constant=0.0)

        def load_x(t):
            s = t % 2
            if t < 16:
                dma("sync", xt[s], xp[t * 128:(t + 1) * 128, :], w=[B_xt[s]])
            else:
                V("memset", [], [B_xt[s]], eng="gpsimd", ap=xt[s], constant=0.0)
                dma("sync", xt[s][0:4, :], xs, w=[B_xt[s]])
            dma("sync", cst[s], c_cs[t], w=[B_cst[s]])

        def rmsnorm_stats(src, col, rbufs):
            act(junk, src, AF.Square, rbufs, B_junkl + [B_ssq[col]], accum_out=ssq[:, col:col + 1])
            act(rt[:, col:col + 1], ssq[:, col:col + 1], AF.Sqrt, [B_ssq[col], B_["epsb"]], [B_ssq[col]],
                scale=1.0 / 1024.0, bias=epsb[:, 0:1])
            V("reciprocal", [B_ssq[col]], [B_ssq[col]], out=rstd[:, col:col + 1], in_=rt[:, col:col + 1])

        junk = AV(W0 + 30 * KiB, [128, 1024], F32); B_junkl = [Bw["Hs"], Bw["ctmp"]]
        us = ST("us", [128, 4, 4], F32); B_us = Buf("us")
        cs4 = ST("cs4", [128, 4, 4], F32); B_cs4 = Buf("cs4")
        prevT = ST("prevT", [128, 4, 4, 2], F32); B_prevT = Buf("prevT")
        csT = ST("csT", [128, 4, 4, 2], F32); B_csT = Buf("csT")
        misc8 = ST("misc8", [8, 512], F32); B_misc8 = Buf("misc8")
        cpo = misc8[0:2, :]; sc8 = misc8; cso8 = misc8
        B_cpo = B_sc8 = B_cso8 = B_misc8

        def rope(tf, tmps, cs_t, Bt, Btmp, Bcs, eng):
            v = tf.rearrange("p (h d) -> p h d", h=8)
            x1 = v[:, :, 0:8]; x2 = v[:, :, 8:16]
            cosv = cs_t[:, 0:64].rearrange("p (h d) -> p h d", h=8); sinv = cs_t[:, 64:128].rearrange("p (h d) -> p h d", h=8)
            t1, t2, t3, t4 = tmps
            V("tensor_tensor", [Bt, Bcs], [Btmp], eng=eng, out=t1, in0=x1, in1=cosv, op=ALU.mult)
            V("tensor_tensor", [Bt, Bcs], [Btmp], eng=eng, out=t2, in0=x2, in1=sinv, op=ALU.mult)
            V("tensor_tensor", [Bt, Bcs], [Btmp], eng=eng, out=t3, in0=x2, in1=cosv, op=ALU.mult)
            V("tensor_tensor", [Bt, Bcs], [Btmp], eng=eng, out=t4, in0=x1, in1=sinv, op=ALU.mult)
            V("tensor_tensor", [Btmp], [Bt], eng=eng, out=x1, in0=t1, in1=t2, op=ALU.subtract)
            V("tensor_tensor", [Btmp], [Bt], eng=eng, out=x2, in0=t3, in1=t4, op=ALU.add)

        pTx = bank(0).bitcast(BF16).rearrange("p (a b) -> p a b", a=8)
        pTqk = bank(4).bitcast(BF16).rearrange("p (a b) -> p a b", a=8)

        load_x(0)
        load_x(1)
        groups = [[0, 1, 2, 3], [4, 5, 6, 7], [8, 9, 10, 11], [12, 13, 14, 15], [16]]

        def FE(t, tt):
            s = t % 2
            rmsnorm_stats(xt[s], t, [B_xt[s]])
            V("scalar_tensor_tensor", [B_xt[s], B_ssq[t], B_["gbc"]], [Bw["xnb"]], out=xnb, in0=xt[s],
              scalar=rstd[:, t:t + 1], in1=gbc[:], op0=ALU.mult, op1=ALU.mult)
            for c in range(8):
                tr(pTx[:, c, :], xnb[:, c * 128:(c + 1) * 128], identb[:], [Bw["xnb"], B_["identb"]], [bkB[0]])
            act(xnTg[:, :, tt * 128:(tt + 1) * 128], pTx, AF.Copy, [bkB[0]], [B_xs[tt]])

        B_xs = [Buf("xnTg%d" % i) for i in range(4)]
        for b_ in B_xs:
            P.alias(b_, B_stg)
        for g, tiles in enumerate(groups):
            ncol = 128 * len(tiles)
            for tt, t in enumerate(tiles):
                s = t % 2
                if tt == 0:
                    FE(t, tt)
                if tt + 1 < len(tiles):
                    FE(tiles[tt + 1], tt + 1)
                if False:
                    rmsnorm_stats(xt[s], t, [B_xt[s]])
                for j in range(3):
                    for c in range(8):
                        mm(bank(1 + j), xnTg[:, c, tt * 128:(tt + 1) * 128], WinB[:, c, j * 512:(j + 1) * 512],
                           c == 0, c == 7, [B_xs[tt], B_win[c]], [bkB[1 + j]])
                tq, tk, tv = qf, kf[s], vf[s]
                Bq, Bk, Bv = Bw["qf"], B_kf[s], B_vf[s]
                act(tq, bank(1), AF.Copy, [bkB[1]], [Bq], scale=0.125)
                act(tk, bank(2), AF.Copy, [bkB[2]], [Bk])
                act(tv, bank(3), AF.Copy, [bkB[3]], [Bv])
                rope(tq, rtq, cst[s], Bq, Bw["rtq"], B_cst[s], "vector")
                rope(tk, rtk, cst[s], Bk, Bw["rtk"], B_cst[s], "gpsimd")
                if t < 16:
                    dma("sync", kp[t * 128:(t + 1) * 128, :], tk, r=[Bk])
                    dma("sync", vp[t * 128:(t + 1) * 128, :], tv, r=[Bv])
                    V("tensor_copy", [Bv], [B_vb[t]], eng="gpsimd", out=Vb[:, t, :, 0:64],
                      in_=tv.rearrange("p (h d) -> p h d", h=8))
                    V("tensor_copy", [Bq], [Bw["qb"]], out=qb, in_=tq)
                    V("tensor_copy", [Bk], [Bw["kb"]], eng="gpsimd", out=kb, in_=tk)
                    for h in range(8):
                        tr(pTqk[0:64, h, :], qb[:, h * 64:(h + 1) * 64], identb[:], [Bw["qb"], B_["identb"]], [bkB[4]])
                    act(QT[0:64, :, t * 128:(t + 1) * 128], pTqk[0:64, :, :], AF.Copy, [bkB[4]], [B_qt[t]])
                    for h in range(8):
                        tr(pTqk[0:64, h, :], kb[:, h * 64:(h + 1) * 64], identb[:], [Bw["kb"], B_["identb"]], [bkB[4]])
                    act(KT[0:64, :, t * 128:(t + 1) * 128], pTqk[0:64, :, :], AF.Copy, [bkB[4]], [B_kt[t]])
                else:
                    dma("sync", ks, tk[0:4, :], r=[Bk])
                    dma("sync", vs, tv[0:4, :], r=[Bv])
                if t + 2 < NT:
                    load_x(t + 2)
            for ct in range(4):
                def colsW(j):
                    return slice(1536 + j * 512 + ct * 128, 1536 + j * 512 + (ct + 1) * 128)
                for c in range(8):
                    mm(bank(5)[:, 0:ncol], WinB[:, c, colsW(2)], xnTg[:, c, 0:ncol], c == 0, c == 7, B_xs + [B_win[c]], [bkB[5]])
                for c in range(8):
                    mm(bank(6)[:, 0:ncol], WinB[:, c, colsW(1)], xnTg[:, c, 0:ncol], c == 0, c == 7, B_xs + [B_win[c]], [bkB[6]])
                act(Hs[:, 0:ncol], bank(5)[:, 0:ncol], AF.Copy, [bkB[5]], [Bw["Hs"]])
                for c in range(8):
                    mm(bank(5)[:, 0:ncol], WinB[:, c, colsW(0)], xnTg[:, c, 0:ncol], c == 0, c == 7, B_xs + [B_win[c]], [bkB[5]])
                w0 = convw[:, 0 * 4 + ct:0 * 4 + ct + 1]; w1 = convw[:, 4 + ct:4 + ct + 1]; w2 = convw[:, 8 + ct:8 + ct + 1]
                if g < 4:
                    V("tensor_tensor", [bkB[6], Bw["Hs"]], [Bw["ug"]], out=ug[:, ct, 2:514], in0=bank(6), in1=Hs, op=ALU.mult)
                    V("tensor_scalar", [Bw["ug"], B_["convw"]], [Bw["ctmp"]], out=ctmp, in0=ug[:, ct, 2:514], scalar1=w2, scalar2=None, op0=ALU.mult)
                    V("scalar_tensor_tensor", [Bw["ug"], Bw["ctmp"], B_["convw"]], [Bw["ctmp"]], out=ctmp, in0=ug[:, ct, 1:513], scalar=w1, in1=ctmp, op0=ALU.mult, op1=ALU.add)
                    V("scalar_tensor_tensor", [Bw["ug"], Bw["ctmp"], B_["convw"]], [Bw["ctmp"]], out=ctmp, in0=ug[:, ct, 0:512], scalar=w0, in1=ctmp, op0=ALU.mult, op1=ALU.add)
                    V("tensor_tensor", [bkB[5], Bw["ctmp"]], [B_ct[t_] for t_ in tiles],
                      out=convT[:, tiles[0]:tiles[0] + 4, ct, :], in0=bank(5).rearrange("p (a b) -> p a b", a=4),
                      in1=ctmp.rearrange("p (a b) -> p a b", a=4), op=ALU.mult)
                    V("tensor_copy", [Bw["ug"]], [Bw["ug"]], out=ug[:, ct, 0:2], in_=ug[:, ct, 512:514])
                else:
                    V("tensor_tensor", [bkB[6], Bw["Hs"]], [B_us], out=us[:, ct, :], in0=bank(6)[:, 0:4], in1=Hs[:, 0:4], op=ALU.mult)
                    V("tensor_scalar", [B_us, B_["convw"]], [B_cs4], out=cs4[:, ct, :], in0=us[:, ct, :], scalar1=w2, scalar2=None, op0=ALU.mult)
                    V("scalar_tensor_tensor", [B_prevT, B_cs4, B_["convw"]], [B_cs4], out=cs4[:, ct, :], in0=prevT[:, ct, :, 1], scalar=w1, in1=cs4[:, ct, :], op0=ALU.mult, op1=ALU.add)
                    V("scalar_tensor_tensor", [B_prevT, B_cs4, B_["convw"]], [B_cs4], out=cs4[:, ct, :], in0=prevT[:, ct, :, 0], scalar=w0, in1=cs4[:, ct, :], op0=ALU.mult, op1=ALU.add)
                    V("tensor_tensor", [bkB[5], B_cs4], [B_ct[16]], out=convT[:, 16, ct, 0:4], in0=bank(5)[:, 0:4], in1=cs4[:, ct, :], op=ALU.mult)
            if g == 3:
                for ct in range(4):
                    tr(bank(0)[0:2, ct * 128:(ct + 1) * 128], ug[:, ct, 0:2], identf[:], [Bw["ug"], B_["identf"]], [bkB[0]])
                act(cpo, bank(0)[0:2, :], AF.Copy, [bkB[0]], [B_cpo])
                dma("sync", cp, cpo, r=[B_cpo])
                dma("sync", sc8[:], sc, w=[B_sc8])
                for ct in range(4):
                    tr(bank(0)[:, 512 - 32 + ct * 8: 512 - 32 + (ct + 1) * 8], sc8[:, ct * 128:(ct + 1) * 128], identf[0:8, 0:8], [B_sc8, B_["identf"]], [bkB[0]])
                act(prevT[:].rearrange("p a b c -> p (a b c)"), bank(0)[:, 480:512], AF.Copy, [bkB[0]], [B_prevT])
            if g == 4:
                V("tensor_copy", [B_prevT], [B_csT], out=csT[:, :, :, 0], in_=prevT[:, :, :, 1])
                V("tensor_copy", [B_us], [B_csT], out=csT[:, :, :, 1], in_=us[:, :, :])
                for ct in range(4):
                    tr(bank(0)[0:8, ct * 128:(ct + 1) * 128], csT[:, ct, :, :].rearrange("p a b -> p (a b)"), identf[:], [B_csT, B_["identf"]], [bkB[0]])
                act(cso8[:], bank(0)[0:8, :], AF.Copy, [bkB[0]], [B_cso8])
                dma("sync", cso, cso8[:], r=[B_cso8])

        all_p1 = list(Bw.values()) + B_xt + B_kf + B_vf[:1] + B_cst + B_win + B_xs
        ATT = AV(0, [128, NT, 8, 128], BF16)
        B_att = [Buf("att%d" % t) for t in range(NT)]
        for b_ in B_att:
            P.alias(b_, B_win)
        Pb = [AV(34 * KiB + i * KiB, [128, 512], BF16) for i in range(4)]; B_P = [Buf("P%d" % i) for i in range(4)]
        osb = [AV(38 * KiB + i * 2 * KiB, [65, 512], F32) for i in range(2)]; B_osb = [Buf("osb%d" % i) for i in range(2)]
        biasp = [AV(42 * KiB + i * 1152, [128, 8, 72], BF16) for i in range(2)]; B_bp = [Buf("bp%d" % i) for i in range(2)]
        gct = [AV(45 * KiB + i * 768, [128, 192], F32) for i in range(2)]; B_gct = [Buf("gct%d" % i) for i in range(2)]
        gm = AV(46 * KiB + 512, [128, 64], F32); top8 = AV(46 * KiB + 768, [128, 8, 8], F32); sel = AV(47 * KiB, [128, 64], F32)
        B_gm = Buf("gm"); B_top8 = Buf("top8"); B_sel = Buf("sel")
        for b_ in B_P + B_osb + B_bp + B_gct + [B_gm, B_top8, B_sel]:
            P.alias(b_, B_win)
        V("memset", [], [B_att[16]], eng="gpsimd", ap=ATT[0:64, 16, :, :], constant=0.0)
        for i in range(2):
            V("memset", [], [B_bp[i]], eng="gpsimd", ap=biasp[i], constant=0.0)
        SAMPLE_BUFS = []

        def sbuf_(name, olds=None):
            b_ = Buf(name)
            P.alias(b_, (all_p1 if olds is None else olds) + SAMPLE_BUFS)
            SAMPLE_BUFS.append(b_)
            return b_
        SB = 154 * KiB
        NG = 5
        G = [AV(SB + i * 4 * KiB, [128, 1024], F32) for i in range(NG)]
        accs = [AV(SB + 20 * KiB + i * 4 * KiB, [128, 1024], F32) for i in range(2)]
        B_G = [sbuf_("G%d" % i) for i in range(NG)]; B_accs = [sbuf_("accs0"), sbuf_("accs1")]
        oA = [186 * KiB]

        def smallA(shape, dt):
            isz = 2 if dt == BF16 else 4
            n = 1
            for x_ in shape[1:]:
                n *= x_
            ap = AV(oA[0], shape, dt)
            oA[0] += (n * isz + 31) // 32 * 32
            assert oA[0] <= ARENA
            return ap
        ptI = smallA([128, 2], I32); ptF = smallA([128, 2], F32); idxf = smallA([128, 128], F32); idxI = smallA([128, 128], I32)
        ptb8 = smallA([8, 256], I32); PTf = smallA([8, 256], F32)
        B_s0 = sbuf_("s0")
        for pr in range(2):
            dma("sync", ptI[:, pr:pr + 1], pt[2 * pr:2 * pr + 2, :].rearrange("b (g o) -> (b g) o", o=1), w=[B_s0])
        dma("sync", ptb8, pt.rearrange("b g -> (b g)").partition_broadcast(8), w=[B_s0])
        V("tensor_copy", [B_s0], [B_s0], out=ptF, in_=ptI)
        V("tensor_copy", [B_s0], [B_s0], out=PTf, in_=ptb8)
        V("tensor_scalar", [B_s0], [B_s0], out=ptF, in0=ptF, scalar1=64.0, scalar2=None, op0=ALU.mult)
        for pr in range(2):
            V("tensor_scalar", [B_s0, B_["cf"]], [B_s0], out=idxf[:, pr * 64:pr * 64 + 32], in0=chunkid, scalar1=ptF[:, pr:pr + 1], scalar2=None, op0=ALU.add)
            V("tensor_scalar", [B_s0], [B_s0], out=idxf[:, pr * 64 + 32:pr * 64 + 64], in0=idxf[:, pr * 64:pr * 64 + 32], scalar1=32.0, scalar2=None, op0=ALU.add)
        V("tensor_scalar", [B_s0], [B_s0], out=idxf, in0=idxf, scalar1=0.0, scalar2=163839.0, op0=ALU.max, op1=ALU.min)
        V("tensor_copy", [B_s0], [B_s0], out=idxI, in_=idxf)

        def s1_steps():
            chunks = [(pr, c) for pr in range(2) for c in range(64)]
            AHEAD = NG - 1

            def gather(k_):
                pr, c = chunks[k_]
                gi_ = k_ % NG
                col = pr * 64 + c
                P.dma("gpsimd", lambda e, gi_=gi_, col=col: e.indirect_dma_start(
                    out=G[gi_], out_offset=None, in_=ckc[:, :], in_offset=bass.IndirectOffsetOnAxis(ap=idxI[:, col:col + 1], axis=0)),
                    [B_s0], [B_G[gi_]])

            def accum(k_):
                pr, c = chunks[k_]
                gi_ = k_ % NG
                en_ = "gpsimd" if s1_pool[0] else "vector"
                if c == 0:
                    V("tensor_copy", [B_G[gi_]], [B_accs[pr]], eng=en_, out=accs[pr], in_=G[gi_])
                else:
                    V("tensor_tensor", [B_G[gi_], B_accs[pr]], [B_accs[pr]], eng=en_, out=accs[pr], in0=accs[pr], in1=G[gi_], op=ALU.add)
            for k_ in range(min(AHEAD, len(chunks))):
                gather(k_)
            for k_ in range(len(chunks)):
                if k_ + AHEAD < len(chunks):
                    gather(k_ + AHEAD)
                accum(k_)
                yield
        s1_pool = [False]
        s1gen = s1_steps()

        def s1_advance(n):
            for _ in range(n):
                try:
                    next(s1gen)
                except StopIteration:
                    return
        for h in range(8):
            V("tensor_reduce", B_kt, [B_["ksum"]], out=ksum[:, h * 8:(h + 1) * 8],
              in_=KT[0:64, h, :].rearrange("p (b k) -> p b k", b=8), axis=AX.X, op=ALU.add)
        V("tensor_copy", [B_["ksum"]], [B_["ksumhi"]], out=ksumhi[:], in_=ksum[:])
        V("tensor_tensor", [B_["ksum"], B_["ksumhi"]], [B_["ksumlo"]], out=ksumlo[:], in0=ksum[:], in1=ksumhi[:], op=ALU.subtract)
        pB = bank(7).bitcast(BF16).rearrange("p (a b) -> p a b", a=8)
        for t in range(16):
            s = t % 2
            s1_advance(4)
            dma("sync", gct[s], c_g[t], w=[B_gct[s]])
            for h in range(8):
                mm(bank(6)[:, h * 8:(h + 1) * 8], QT[0:64, h, t * 128:(t + 1) * 128], ksumhi[:, h * 8:(h + 1) * 8], True, False,
                   [B_qt[t], B_["ksumhi"]], [bkB[6]])
                mm(bank(6)[:, h * 8:(h + 1) * 8], QT[0:64, h, t * 128:(t + 1) * 128], ksumlo[:, h * 8:(h + 1) * 8], False, True,
                   [B_qt[t], B_["ksumlo"]], [bkB[6]])
            V("tensor_tensor", [bkB[6], B_gct[s]], [B_gm], out=gm, in0=bank(6)[:, 0:64], in1=gct[s][:, 0:64], op=ALU.add)
            for h in range(8):
                V("max", [B_gm], [B_top8], out=top8[:, h, :], in_=gm[:, h * 8:(h + 1) * 8])
            for h in range(8):
                V("tensor_scalar", [B_gm, B_top8], [B_sel], out=sel[:, h * 8:(h + 1) * 8], in0=gm[:, h * 8:(h + 1) * 8],
                  scalar1=top8[:, h, 2:3], scalar2=None, op0=ALU.is_ge)
            V("tensor_tensor", [B_sel, B_gct[s]], [B_sel], out=sel, in0=sel, in1=gct[s][:, 64:128], op=ALU.mult)
            V("tensor_tensor", [B_sel, B_gct[s]], [B_sel], out=sel, in0=sel, in1=gct[s][:, 128:192], op=ALU.add)
            V("tensor_scalar", [B_sel], [B_bp[s]], out=biasp[s][:, :, 64:72], in0=sel.rearrange("p (h j) -> p h j", h=8),
              scalar1=-1.0, scalar2=BIG, op0=ALU.add, op1=ALU.mult)
            for h in range(8):
                tr(pB[0:72, h, :], biasp[s][:, h, :], identb[:], [B_bp[s], B_["identb"]], [bkB[7]])
            act(QT[64:72, :, t * 128:(t + 1) * 128], pB[64:72, :, :], AF.Copy, [bkB[7]], [B_qtb[t]])


        def s2_steps():
            def sb_view(off_kib, shape, dt):
                return AV(int(off_kib * KiB), shape, dt)
            pagesum = [sb_view(154 + 2 * i, [128, 512], F32) for i in range(2)]
            B_ps_ = sbuf_("pagesum", [])
            for pr in range(2):
                V("tensor_reduce", [B_accs[pr]], [B_ps_], out=pagesum[pr], in_=accs[pr].rearrange("p (pos f) -> p f pos", pos=2), axis=AX.X, op=ALU.add)
            qbcs = sb_view(158, [64, 512], F32); prod = sb_view(160, [64, 512], F32)
            oB = [162 * KiB]

            def smallB(shape, dt):
                n = 1
                for x_ in shape[1:]:
                    n *= x_
                ap = AV(oB[0], shape, dt)
                oB[0] += (n * 4 + 31) // 32 * 32
                assert oB[0] <= 166 * KiB
                return ap
            gate2 = smallB([64, 16], F32); gateT = smallB([8, 128], F32); top8s = smallB([8, 4, 8], F32); oh = smallB([8, 32], F32)
            junk8 = smallB([8, 32], F32); physf = smallB([8, 24], F32); Dm = smallB([8, 8, 24], F32)
            idxf2 = smallB([128, 192], F32); idxI2 = smallB([128, 192], I32)
            B_s2 = sbuf_("s2", [])
            Bqs = Bw["qf"]; Bks = B_kf[0]; Bvs = B_vf[0]
            for pr in range(2):
                mm(bank(7)[0:64, :], cf[:, 0:64], pagesum[pr], True, True, [B_ps_, B_["cf"]], [bkB[7]])
                mm(bank(6)[0:64, :], cf[0:4, 288 + 64 * pr:288 + 64 * (pr + 1)], qf[0:4, :], True, True, [Bqs, B_["cf"]], [bkB[6]])
                act(qbcs, bank(6)[0:64, :], AF.Copy, [bkB[6]], [B_s2])
                V("tensor_tensor", [bkB[7], B_s2], [B_s2], out=prod, in0=bank(7)[0:64, :], in1=qbcs, op=ALU.mult)
                V("tensor_reduce", [B_s2], [B_s2], out=gate2[:, pr * 8:(pr + 1) * 8], in_=prod.rearrange("p (h d) -> p h d", h=8), axis=AX.X, op=ALU.add)
                tr(bank(7)[0:8, pr * 64:(pr + 1) * 64], gate2[:, pr * 8:(pr + 1) * 8], identf[0:64, 0:64], [B_s2, B_["identf"]], [bkB[7]])
                act(gateT[:, pr * 64:(pr + 1) * 64], bank(7)[0:8, pr * 64:(pr + 1) * 64], AF.Copy, [bkB[7]], [B_s2])
            S2 = [B_s2]
            for b in range(4):
                V("max", S2, S2, out=top8s[:, b, :], in_=gateT[:, b * 32:(b + 1) * 32])
                for r_ in range(3):
                    V("tensor_scalar", S2, S2, out=oh, in0=gateT[:, b * 32:(b + 1) * 32], scalar1=top8s[:, b, r_:r_ + 1], scalar2=None, op0=ALU.is_equal)
                    for hf in range(2):
                        colp = (b * 3 + r_) * 2 + hf
                        V("tensor_tensor", S2 + [B_s0], S2, out=junk8, in0=oh,
                          in1=PTf[:, b * 64:(b + 1) * 64].rearrange("p (j t) -> p j t", t=2)[:, :, hf], op=ALU.mult)
                        V("tensor_reduce", S2, S2, out=physf[:, colp:colp + 1], in_=junk8, axis=AX.X, op=ALU.add)
            for h in range(8):
                V("tensor_scalar", S2 + [B_["cf"]], S2, out=Dm[:, h, :], in0=physf, scalar1=cf[0:8, 928 + h:929 + h], scalar2=None, op0=ALU.mult)
            mm(bank(7)[:, 0:192], cf[0:8, 940:1068], Dm.rearrange("p a b -> p (a b)"), True, True, S2 + [B_["cf"]], [bkB[7]])
            V("scalar_tensor_tensor", [bkB[7], B_["cf"]], S2, out=idxf2, in0=bank(7)[:, 0:192], scalar=1024.0, in1=poshc, op0=ALU.mult, op1=ALU.add)
            V("tensor_scalar", S2, S2, out=idxf2, in0=idxf2, scalar1=0.0, scalar2=2621439.0, op0=ALU.max, op1=ALU.min)
            V("tensor_copy", S2, S2, out=idxI2, in_=idxf2)
            yield
            NSET = 4
            Ks = [sb_view(166 + 6 * i, [128, 12, 64], F32) for i in range(NSET)]
            Vs = [sb_view(169 + 6 * i, [128, 12, 64], F32) for i in range(NSET)]
            B_ks = [sbuf_("ks%d" % i, []) for i in range(NSET)]; B_vs = [sbuf_("vs%d" % i, []) for i in range(NSET)]
            qbu = [sb_view(154 + 0.5 * i, [128, 128], F32) for i in range(2)]; B_qbu = [sbuf_("qbu0", []), sbuf_("qbu1", [])]
            oC = [158 * KiB]

            def smallC(shape):
                n = 1
                for x_ in shape[1:]:
                    n *= x_
                ap = AV(oC[0], shape, F32)
                oC[0] += (n * 4 + 31) // 32 * 32
                assert oC[0] <= 162 * KiB
                return ap
            STs = smallC([128, 192]); Es = smallC([128, 192]); denp = smallC([64, 32]); sself = smallC([4, 8])
            eself = smallC([4, 8]); D2 = smallC([4, 32]); ebvt = smallC([64, 96]); numt = smallC([64, 32]); dent = smallC([64, 32])
            B_s6 = sbuf_("s6", [])
            S6 = [B_s6]
            units = [(b, hp) for b in range(4) for hp in range(4)]

            def u_gather(u):
                b, hp = units[u]
                st_ = u % NSET
                for jj in range(12):
                    h = 2 * hp + jj // 6
                    col = h * 24 + b * 6 + (jj % 6)
                    P.dma("gpsimd", lambda e, jj=jj, col=col, st_=st_: e.indirect_dma_start(
                        out=Ks[st_][:, jj, :], out_offset=None, in_=ck[:, :], in_offset=bass.IndirectOffsetOnAxis(ap=idxI2[:, col:col + 1], axis=0)),
                        S2, [B_ks[st_]])
                    P.dma("gpsimd", lambda e, jj=jj, col=col, st_=st_: e.indirect_dma_start(
                        out=Vs[st_][:, jj, :], out_offset=None, in_=cv[:, :], in_offset=bass.IndirectOffsetOnAxis(ap=idxI2[:, col:col + 1], axis=0)),
                        S2, [B_vs[st_]])

            def u_compute(u):
                b, hp = units[u]
                st_ = u % NSET
                qs_ = u % 2
                mm(bank(6)[:, 0:128], cf[0:4, 416 + 128 * b:416 + 128 * (b + 1)], qf[0:4, hp * 128:(hp + 1) * 128], True, True, [Bqs, B_["cf"]], [bkB[6]])
                act(qbu[qs_], bank(6)[:, 0:128], AF.Copy, [bkB[6]], [B_qbu[qs_]])
                for jj in range(12):
                    hh = jj // 6
                    V("tensor_tensor", [B_ks[st_], B_qbu[qs_]], [B_ks[st_]], out=Ks[st_][:, jj, :], in0=Ks[st_][:, jj, :],
                      in1=qbu[qs_][:, hh * 64:(hh + 1) * 64], op=ALU.mult)
                c0 = b * 48 + hp * 12
                V("tensor_reduce", [B_ks[st_]], S6, out=STs[:, c0:c0 + 12], in_=Ks[st_], axis=AX.X, op=ALU.add)
                act(Es[:, c0:c0 + 12], STs[:, c0:c0 + 12], AF.Exp, S6, S6)
                for hh in range(2):
                    h = 2 * hp + hh
                    for s6 in range(6):
                        cc = c0 + hh * 6 + s6
                        mm(bank(7)[0:64, 480 + b * 8 + h:480 + b * 8 + h + 1], Vs[st_][:, hh * 6 + s6, :], Es[:, cc:cc + 1], s6 == 0, s6 == 5, S6 + [B_vs[st_]], [bkB[7]])
            LAG = NSET - 1
            for u in range(16 + LAG):
                if u < 16:
                    u_gather(u)
                if u - LAG >= 0:
                    u_compute(u - LAG)
                yield
            mm(bank(7)[0:64, 0:192], cf[:, 940:1004], Es, True, True, S6 + [B_["cf"]], [bkB[7]])
            V("tensor_reduce", [bkB[7]], S6, out=denp, in_=bank(7)[0:64, 0:192].rearrange("p (g s) -> p g s", s=6), axis=AX.X, op=ALU.add)
            prodqk = misc8[0:4, :]
            V("tensor_tensor", [Bqs, Bks, B_misc8], [B_misc8], out=prodqk, in0=qf[0:4, :], in1=kf[0][0:4, :], op=ALU.mult)
            V("tensor_reduce", [B_misc8], S6, out=sself, in_=prodqk.rearrange("p (h d) -> p h d", h=8), axis=AX.X, op=ALU.add)
            act(eself, sself, AF.Exp, S6, S6)
            for b in range(4):
                V("tensor_scalar", S6 + [B_["cf"]], S6, out=D2[:, b * 8:(b + 1) * 8], in0=eself, scalar1=cf[0:4, 936 + b:937 + b], scalar2=None, op0=ALU.mult)
            mm(bank(6)[0:64, 0:32], cf[0:4, 940:1004], D2, True, True, S6 + [B_["cf"]], [bkB[6]])
            for h in range(8):
                tr(bank(6)[0:64, 64 + h * 4:64 + h * 4 + 4], vf[0][0:4, h * 64:(h + 1) * 64], identf[0:4, 0:4], [Bvs, B_["identf"]], [bkB[6]])
            act(ebvt, bank(6)[0:64, 0:96], AF.Copy, [bkB[6]], S6)
            ebc3 = ebvt[:, 0:32].rearrange("p (b h) -> p b h", b=4)
            vT3 = ebvt[:, 64:96].rearrange("p (h b) -> p b h", b=4)
            V("tensor_tensor", S6, S6, out=numt.rearrange("p (b h) -> p b h", b=4), in0=vT3, in1=ebc3, op=ALU.mult)
            V("tensor_tensor", S6 + [bkB[7]], S6, out=numt, in0=numt, in1=bank(7)[0:64, 480:512], op=ALU.add)
            V("tensor_tensor", S6, S6, out=dent, in0=denp, in1=ebvt[:, 0:32], op=ALU.add)
            V("reciprocal", S6, S6, out=dent, in_=dent)
            V("tensor_tensor", S6, [B_att[16]], out=ATT[0:64, 16, :, 0:4], in0=numt.rearrange("p (b h) -> p h b", b=4),
              in1=dent.rearrange("p (b h) -> p h b", b=4), op=ALU.mult)
            yield
        s2gen = s2_steps()

        def s2_advance(n):
            for _ in range(n):
                try:
                    next(s2gen)
                except StopIteration:
                    return
        steps = []
        for h in range(8):
            for c in range(4):
                for kt in range(4 * c + 4):
                    steps.append((h, c, kt))
        LOOK = 2
        hc_ob = {}
        for si in range(len(steps) + LOOK):
            if si < len(steps):
                h, c, kt = steps[si]
                if kt == 0:
                    it_ = h * 4 + c
                    s1_pool[0] = True
                    s1_advance(2)
                    hc_ob[(h, c)] = 3 + (len(hc_ob) % 2)
                i = max(0, kt - 4 * c)
                sb = si % 3
                pi = si % 4
                qr = [B_qt[4 * c + i_] for i_ in range(4)] + [B_qtb[4 * c + i_] for i_ in range(4)]
                mm(bank(sb)[:, i * 128:512], KT[0:72, h, kt * 128:(kt + 1) * 128], QT[0:72, h, c * 512 + i * 128:(c + 1) * 512],
                   True, True, [B_kt[kt]] + qr, [bkB[sb]])
                act(Pb[pi][:, i * 128:512], bank(sb)[:, i * 128:512], AF.Exp, [bkB[sb]], [B_P[pi]])
                if kt >= 4 * c:
                    V("tensor_tensor", [B_P[pi], B_["tri"]], [B_P[pi]], out=Pb[pi][:, i * 128:(i + 1) * 128],
                      in0=Pb[pi][:, i * 128:(i + 1) * 128], in1=tri[:], op=ALU.mult)
            sj = si - LOOK
            if sj >= 0:
                h, c, kt = steps[sj]
                nkt = 4 * c + 4
                i = max(0, kt - 4 * c)
                pi = sj % 4
                ob = hc_ob[(h, c)]
                mm(bank(ob)[0:65, i * 128:512], Vb[:, kt, h, :], Pb[pi][:, i * 128:512], kt == 0, kt == nkt - 1,
                   [B_vb[kt], B_P[pi]], [bkB[ob]])
                if kt == nkt - 1:
                    oi = ob - 3
                    V("tensor_copy", [bkB[ob]], [B_osb[oi]], out=osb[oi], in_=bank(ob)[0:65, :])
                    act(osb[oi][64:65, :], osb[oi][64:65, :], AF.Ln, [B_osb[oi]], [B_osb[oi]])
                    act(osb[oi][64:65, :], osb[oi][64:65, :], AF.Exp, [B_osb[oi]], [B_osb[oi]], scale=-1.0)
                    mm(bank(5)[0:64, :], cf[64:65, 940:1004], osb[oi][64:65, :], True, True, [B_osb[oi], B_["cf"]], [bkB[5]])
                    V("tensor_tensor", [B_osb[oi], bkB[5]], [B_att[4 * c + i_] for i_ in range(4)],
                      out=ATT[0:64, 4 * c:4 * c + 4, h, :], in0=osb[oi][0:64, :].rearrange("p (a b) -> p a b", a=4),
                      in1=bank(5)[0:64, :].rearrange("p (a b) -> p a b", a=4), op=ALU.mult)
        s1_advance(1000)
        s2_advance(1000)
        att_done = B_qt + B_kt + B_vb + B_qtb
        ACC = AV(48 * KiB, [128, NT, 1024], F32); B_acc = [Buf("acc%d" % t) for t in range(NT)]
        for b_ in B_acc:
            P.alias(b_, att_done)
        WoC = AV(116 * KiB, [128, 4, 1024], BF16); B_woc = Buf("woc"); P.alias(B_woc, att_done)
        gates = AV(124 * KiB, [128, NT, 32], F32); B_gates = [Buf("gates%d" % t) for t in range(NT)]
        for b_ in B_gates:
            P.alias(b_, att_done)
        WoA = AV(146 * KiB, [64, 8, 1024], BF16); B_woa = Buf("woa")
        xr2 = [AV((162 + 4 * i) * KiB, [128, 1024], F32) for i in range(2)]; B_xr2 = [Buf("xr0"), Buf("xr1")]
        hn2 = [AV((170 + 4 * i) * KiB, [128, 1024], F32) for i in range(2)]; B_hn2 = [Buf("hn0"), Buf("hn1")]
        hnT32_2 = [AV((178 + 4 * i) * KiB, [128, 8, 128], F32) for i in range(2)]; B_hnT2 = [Buf("hnT0"), Buf("hnT1")]
        B_rs2 = []
        wstg = AV(34 * KiB, [128, 3, 1024], F32); B_wstg = Buf("wstg")
        p2_bufs = B_P + B_osb + B_bp + B_gct + [B_gm, B_top8, B_sel]
        p3_w = [B_woa] + B_xr2 + B_hn2 + B_hnT2 + B_rs2
        for b_ in p3_w + [B_wstg]:
            P.alias(b_, all_p1 + SAMPLE_BUFS + p2_bufs)
        for hg in ((0, 1, 2), (3, 4, 5), (6, 7)):
            n = len(hg)
            dma("sync", wstg[0:64, 0:n, :], w_out[hg[0] * 64:(hg[-1] + 1) * 64, :].rearrange("(h r) n -> r h n", r=64), w=[B_wstg])
            V("tensor_copy", [B_wstg], [B_woa], out=WoA[:, hg[0]:hg[0] + n, :], in_=wstg[0:64, 0:n, :])
        for cg in ((0, 1, 2), (3,)):
            n = len(cg)
            dma("sync", wstg[:, 0:n, :], w_out[512 + cg[0] * 128:512 + (cg[-1] + 1) * 128, :].rearrange("(c p) n -> p c n", p=128), w=[B_wstg])
            V("tensor_copy", [B_wstg], [B_woc], out=WoC[:, cg[0]:cg[0] + n, :], in_=wstg[:, 0:n, :])
        dma("sync", gbc[:], g_ffn.partition_broadcast(128), w=[B_["gbc"]])
        pT32 = PS[2].rearrange("p (a b) -> p a b", a=8)
        lgall = AV(186 * KiB, [128, NT, 36], F32); B_lgall = Buf("lgall")
        P.alias(B_lgall, all_p1 + SAMPLE_BUFS + p2_bufs)

        def p3_W(t):
            hps = PS[t % 2]
            Bh = [bkB[2 * (t % 2)], bkB[2 * (t % 2) + 1]]
            for half in range(2):
                for h in range(8):
                    mm(hps[:, half * 512:(half + 1) * 512], ATT[0:64, t, h, :], WoA[:, h, half * 512:(half + 1) * 512], h == 0, False,
                       [B_att[t], B_woa], Bh)
                for ct in range(4):
                    mm(hps[:, half * 512:(half + 1) * 512], convT[:, t, ct, :], WoC[:, ct, half * 512:(half + 1) * 512], False, ct == 3,
                       [B_ct[t], B_woc], Bh)
            i = t % 2
            if t < 16:
                dma("sync", xr2[i], xp[t * 128:(t + 1) * 128, :], w=[B_xr2[i]])
            else:
                V("memset", [], [B_xr2[i]], eng="gpsimd", ap=xr2[i], constant=0.0)
                dma("sync", xr2[i][0:4, :], xs, w=[B_xr2[i]])

        def p3_A(t):
            i = t % 2
            hps = PS[i]; Bh = [bkB[2 * i], bkB[2 * i + 1]]
            hn = hn2[i]; B_hn = B_hn2[i]; hnT32 = hnT32_2[i]; B_hnT32 = B_hnT2[i]
            V("tensor_tensor", Bh + [B_xr2[i]], [B_acc[t]], out=ACC[:, t, :], in0=hps[:, :], in1=xr2[i], op=ALU.add)
            col = NT + t
            act(hn, ACC[:, t, :], AF.Square, [B_acc[t]], [B_hn, B_ssq[col]], accum_out=ssq[:, col:col + 1])
            act(rt[:, col:col + 1], ssq[:, col:col + 1], AF.Sqrt, [B_ssq[col], B_["epsb"]], [B_ssq[col]], scale=1.0 / 1024.0, bias=epsb[:, 0:1])
            V("reciprocal", [B_ssq[col]], [B_ssq[col]], out=rstd[:, col:col + 1], in_=rt[:, col:col + 1])
            V("scalar_tensor_tensor", [B_acc[t], B_ssq[col], B_["gbc"]], [B_hn], out=hn, in0=ACC[:, t, :], scalar=rstd[:, col:col + 1],
              in1=gbc[:], op0=ALU.mult, op1=ALU.mult)

        def p3_B(t):
            i = t % 2
            hn = hn2[i]; B_hn = B_hn2[i]; hnT32 = hnT32_2[i]; B_hnT32 = B_hnT2[i]
            for c in range(8):
                tr(pT32[:, c, :], hn[:, c * 128:(c + 1) * 128], identf[:], [B_hn, B_["identf"]], [bkB[4], bkB[5]])
            act(hnT32, pT32, AF.Copy, [bkB[4], bkB[5]], [B_hnT32])
            V("tensor_copy", [B_hnT32], [B_att[t]], eng="gpsimd", out=ATT[:, t, :, :], in_=hnT32)
            for c in range(8):
                mm(bank(6)[:, 0:36], hnT32[:, c, :], wr[:, c, :], c == 0, c == 7, [B_hnT32, B_["wr"]], [bkB[6]])
            V("tensor_tensor", [bkB[6], B_["rb"]], [B_lgall], out=lgall[:, t, :], in0=bank(6)[:, 0:36], in1=rb[:], op=ALU.add)

        p3_W(0)
        p3_W(1)
        p3_A(0)
        for t in range(NT):
            if t + 2 < NT:
                p3_W(t + 2)
            if t + 1 < NT:
                p3_A(t + 1)
            p3_B(t)

        ob_ = [162 * KiB]

        def rtile(shape):
            n = 1
            for x_ in shape[1:]:
                n *= x_
            ap = AV(ob_[0], shape, F32)
            ob_[0] += (n * 4 + 31) // 32 * 32
            assert ob_[0] <= 170 * KiB
            return ap
        B_rt = Buf("rtmp"); P.alias(B_rt, B_xr2 + B_hn2)
        Rr = [B_rt]
        m4 = rtile([128, NT]); ohg = rtile([128, NT, 4]); eg = rtile([128, NT, 4]); sg4 = rtile([128, NT]); pg = rtile([128, NT])
        leg = rtile([128, NT, 8]); tmp8 = rtile([128, NT, 8]); t8a = rtile([128, NT, 8]); dd = rtile([128, NT]); w1 = rtile([128, NT]); w2 = rtile([128, NT])
        e1 = rtile([128, NT, 8]); e2 = rtile([128, NT, 8]); gi = rtile([128, NT, 8])
        lgg = lgall[:, :, 0:4]
        lge = lgall[:, :, 4:36].rearrange("p t (g i) -> p t g i", g=4)

        def bc(ap2, n):
            return ap2.unsqueeze(2).to_broadcast([128, NT, n])
        V("tensor_reduce", [B_lgall], Rr, out=m4, in_=lgg, axis=AX.X, op=ALU.max)
        V("tensor_tensor", [B_lgall] + Rr, Rr, out=ohg, in0=lgg, in1=bc(m4, 4), op=ALU.is_ge)
        V("tensor_tensor", [B_lgall] + Rr, Rr, out=eg, in0=lgg, in1=bc(m4, 4), op=ALU.subtract)
        act(eg, eg, AF.Exp, Rr, Rr)
        V("tensor_reduce", Rr, Rr, out=sg4, in_=eg, axis=AX.X, op=ALU.add)
        V("reciprocal", Rr, Rr, out=pg, in_=sg4)
        for g_ in range(4):
            if g_ == 0:
                V("tensor_tensor", [B_lgall] + Rr, Rr, out=leg, in0=lge[:, :, 0, :], in1=bc(ohg[:, :, 0], 8), op=ALU.mult)
            else:
                V("tensor_tensor", [B_lgall] + Rr, Rr, out=tmp8, in0=lge[:, :, g_, :], in1=bc(ohg[:, :, g_], 8), op=ALU.mult)
                V("tensor_tensor", Rr, Rr, out=leg, in0=leg, in1=tmp8, op=ALU.add)
        for t in range(NT):
            V("max", Rr, Rr, out=t8a[:, t, :], in_=leg[:, t, :])
        V("tensor_tensor", Rr, Rr, out=dd, in0=t8a[:, :, 1], in1=t8a[:, :, 0], op=ALU.subtract)
        act(w2, dd, AF.Exp, Rr, Rr)
        V("tensor_scalar", Rr, Rr, out=w1, in0=w2, scalar1=1.0, scalar2=None, op0=ALU.add)
        V("reciprocal", Rr, Rr, out=w1, in_=w1)
        V("tensor_tensor", Rr, Rr, out=w2, in0=w2, in1=w1, op=ALU.mult)
        V("tensor_tensor", Rr, Rr, out=w1, in0=w1, in1=pg, op=ALU.mult)
        V("tensor_tensor", Rr, Rr, out=w2, in0=w2, in1=pg, op=ALU.mult)
        V("tensor_tensor", Rr, Rr, out=e1, in0=leg, in1=bc(t8a[:, :, 0], 8), op=ALU.is_equal)
        V("tensor_tensor", Rr, Rr, out=e1, in0=e1, in1=bc(w1, 8), op=ALU.mult)
        V("tensor_tensor", Rr, Rr, out=e2, in0=leg, in1=bc(t8a[:, :, 1], 8), op=ALU.is_equal)
        V("tensor_tensor", Rr, Rr, out=e2, in0=e2, in1=bc(w2, 8), op=ALU.mult)
        V("tensor_tensor", Rr, Rr, out=gi, in0=e1, in1=e2, op=ALU.add)
        gates4 = gates.rearrange("p t (g i) -> p t g i", g=4)
        for g_ in range(4):
            V("tensor_tensor", Rr, B_gates, out=gates4[:, :, g_, :], in0=gi, in1=bc(ohg[:, :, g_], 8), op=ALU.mult)

        p3_done = p3_w + [B_wstg, B_woc, B_lgall, B_rt] + B_ct
        WB0 = 129 * KiB
        NSTG = 3
        estg = [AV(34 * KiB + i * 4 * KiB, [128, 1024], F32) for i in range(NSTG)]; B_estg = [Buf("estg%d" % i) for i in range(NSTG)]
        Wgu = [AV(WB0 + i * 12 * KiB, [128, 8, 512], BF16) for i in range(4)]
        Wd = [AV(WB0 + i * 12 * KiB + 8 * KiB, [128, 2, 1024], BF16) for i in range(4)]
        B_wgu = [Buf("wgu%d" % i) for i in range(4)]; B_wd = [Buf("wd%d" % i) for i in range(4)]
        o = WB0 + 48 * KiB
        sgb = [AV(o + i * KiB, [128, 256], F32) for i in range(2)]; o += 2 * KiB
        hbb = [AV(o + i * 512, [128, 256], BF16) for i in range(2)]; o += KiB
        hTb = [AV(o + i * 512, [128, 2, 128], BF16) for i in range(2)]; o += KiB
        assert o <= ARENA
        B_sg = [Buf("sg0"), Buf("sg1")]; B_hb = [Buf("hb0"), Buf("hb1")]; B_hT = [Buf("hT0"), Buf("hT1")]
        for b_ in B_estg + B_wgu + B_wd + B_sg + B_hb + B_hT:
            P.alias(b_, p3_done + p2_bufs + all_p1 + SAMPLE_BUFS)
        sti = [0]

        def load_expert(e, slot):
            for (src, coff) in ((w_gate, 0), (w_up, 256)):
                for hf in range(2):
                    i = sti[0] % NSTG; sti[0] += 1
                    dma("sync", estg[i].rearrange("p (c f) -> p c f", c=4),
                        src[e, hf * 512:(hf + 1) * 512, :].rearrange("(c p) f -> p c f", p=128), w=[B_estg[i]])
                    V("tensor_copy", [B_estg[i]], [B_wgu[slot]], eng="gpsimd", out=Wgu[slot][:, hf * 4:(hf + 1) * 4, coff:coff + 256],
                      in_=estg[i].rearrange("p (c f) -> p c f", c=4))
            for f_ in range(2):
                i = sti[0] % NSTG; sti[0] += 1
                dma("sync", estg[i], w_down[e, f_ * 128:(f_ + 1) * 128, :], w=[B_estg[i]])
                V("tensor_copy", [B_estg[i]], [B_wd[slot]], eng="gpsimd", out=Wd[slot][:, f_, :], in_=estg[i])

        pTh4 = bank(3).bitcast(BF16).rearrange("p (a b) -> p a b", a=8)
        B_pth = [Buf("pth%d" % i) for i in range(4)]
        for b_ in B_pth:
            b_.last_w = bkB[3].last_w; b_.readers = list(bkB[3].readers)
        sg3 = [AV(o_, [128, 256], F32) for o_ in (WB0 + 48 * KiB, WB0 + 49 * KiB, WB0 + 52 * KiB)]
        hb3 = [AV(WB0 + 50 * KiB + i * 512, [128, 256], BF16) for i in range(3)]
        hT4 = [AV(WB0 + 53 * KiB + i * 512, [128, 2, 128], BF16) for i in range(4)]
        assert WB0 + 55 * KiB <= ARENA
        B_sg3 = [Buf("sg3_%d" % i) for i in range(3)]; B_hb3 = [Buf("hb3_%d" % i) for i in range(3)]; B_hT4 = [Buf("hT4_%d" % i) for i in range(4)]
        for b_ in B_sg3 + B_hb3 + B_hT4:
            P.alias(b_, p3_done + p2_bufs + all_p1 + SAMPLE_BUFS + B_sg + B_hb + B_hT)
        load_expert(0, 0); load_expert(1, 1)
        msteps = []
        for ep in range(NE // 2):
            for t in range(NT):
                for k in range(2):
                    msteps.append((ep, t, k))

        def emit_G(i):
            ep, t, k = msteps[i]
            e = 2 * ep + k; slot = e % 4; gb = i % 3
            for c in range(8):
                mm(bank(gb), ATT[:, t, c, :], Wgu[slot][:, c, :], c == 0, c == 7, [B_att[t], B_wgu[slot]], [bkB[gb]])

        def emit_A(i):
            ep, t, k = msteps[i]
            e = 2 * ep + k; gb = i % 3
            act(sg3[gb], bank(gb)[:, 0:256], AF.Silu, [bkB[gb]], [B_sg3[gb]])
            V("scalar_tensor_tensor", [bkB[gb], B_sg3[gb], B_gates[t]], [B_hb3[gb]], out=hb3[gb], in0=bank(gb)[:, 256:512],
              scalar=gates[:, t, e:e + 1], in1=sg3[gb], op0=ALU.mult, op1=ALU.mult)

        def emit_T(i):
            gb = i % 3; ts = i % 4
            for f_ in range(2):
                tr(pTh4[:, 2 * ts + f_, :], hb3[gb][:, f_ * 128:(f_ + 1) * 128], identb[:], [B_hb3[gb], B_["identb"]], [B_pth[ts]])
            act(hT4[ts], pTh4[:, 2 * ts:2 * ts + 2, :], AF.Copy, [B_pth[ts]], [B_hT4[ts]])

        def emit_D(i):
            ep, t, k = msteps[i]
            if t == 0 and k == 0 and ep + 1 < NE // 2:
                load_expert(2 * ep + 2, (2 * ep + 2) % 4); load_expert(2 * ep + 3, (2 * ep + 3) % 4)
            e = 2 * ep + k; slot = e % 4; ts = i % 4
            ob = 2 + ((i // 2) % 2)
            outp = PS[ob]; Bout = [bkB[2 * ob], bkB[2 * ob + 1]]
            for half in range(2):
                for f_ in range(2):
                    mm(outp[:, half * 512:(half + 1) * 512], hT4[ts][:, f_, :], Wd[slot][:, f_, half * 512:(half + 1) * 512],
                       k == 0 and f_ == 0, k == 1 and f_ == 1, [B_hT4[ts], B_wd[slot]], Bout)
            if k == 1:
                V("tensor_tensor", Bout + [B_acc[t]], [B_acc[t]], out=ACC[:, t, :], in0=outp[:, :], in1=ACC[:, t, :], op=ALU.add)

        NS = len(msteps)
        emit_G(0)
        for i in range(NS + 1):
            if i + 1 < NS:
                emit_G(i + 1)
            if i < NS:
                emit_A(i)
                emit_T(i)
            if i - 1 >= 0:
                emit_D(i - 1)

        dma("sync", gbc[:], g_final.partition_broadcast(128), w=[B_["gbc"]])
        yb = [estg[0], estg[1]]; B_yb = [B_estg[0], B_estg[1]]
        jk5 = estg[2]; B_jk5 = B_estg[2]
        for t in range(NT):
            col = 2 * NT + t
            i = t % 2
            act(jk5, ACC[:, t, :], AF.Square, [B_acc[t]], [B_jk5, B_ssq[col]], accum_out=ssq[:, col:col + 1])
            act(rt[:, col:col + 1], ssq[:, col:col + 1], AF.Sqrt, [B_ssq[col], B_["epsb"]], [B_ssq[col]], scale=1.0 / 1024.0, bias=epsb[:, 0:1])
            V("reciprocal", [B_ssq[col]], [B_ssq[col]], out=rstd[:, col:col + 1], in_=rt[:, col:col + 1])
            V("scalar_tensor_tensor", [B_acc[t], B_ssq[col], B_["gbc"]], [B_yb[i]], out=yb[i], in0=ACC[:, t, :], scalar=rstd[:, col:col + 1],
              in1=gbc[:], op0=ALU.mult, op1=ALU.mult)
            if t < 16:
                dma("sync", yp[t * 128:(t + 1) * 128, :], yb[i], r=[B_yb[i]])
            else:
                dma("sync", ys, yb[i][0:4, :], r=[B_yb[i]])
        P.run()
    return nc


_NC = None


def kernel(**inputs):
    global _NC
    if _NC is None:
        _NC = build_program()
    nc = _NC
    f = lambda a: np.ascontiguousarray(np.asarray(a))
    consts = host_consts()
    ckf = f(inputs["cache_k"]).reshape(2621440, 64)
    cvf = f(inputs["cache_v"]).reshape(2621440, 64)
    shared = {
        "ck": ckf, "cv": cvf,
        "g_mix": f(inputs["g_mix"]).reshape(1024), "w_in": f(inputs["w_in"]).reshape(1024, 3072),
        "conv_w": f(inputs["conv_w"]).reshape(3, 512), "w_out": f(inputs["w_out"]).reshape(1024, 1024),
        "g_ffn": f(inputs["g_ffn"]).reshape(1024), "w_rg": f(inputs["w_router_group"]).reshape(1024, 4),
        "b_rg": f(inputs["b_router_group"]).reshape(4), "w_re": f(inputs["w_router_expert"]).reshape(1024, 32),
        "b_re": f(inputs["b_router_expert"]).reshape(32), "w_gate": f(inputs["w_gate"]).reshape(32, 1024, 256),
        "w_up": f(inputs["w_up"]).reshape(32, 1024, 256), "w_down": f(inputs["w_down"]).reshape(32, 256, 1024),
        "g_final": f(inputs["g_final"]).reshape(1024),
    }
    shared.update(consts)
    xpr = f(inputs["x_prompt"]); xsm = f(inputs["x_sample"]).reshape(32, 1024)
    scv = f(inputs["state_conv"]).reshape(32, 2, 512); ptb = f(inputs["page_table"]).astype(np.int32)
    in_maps = []
    for c in range(NCORES):
        m = dict(shared)
        m["xp"] = xpr[c]
        m["xs"] = xsm[4 * c:4 * c + 4]
        m["sc"] = scv[4 * c:4 * c + 4].reshape(8, 512)
        m["pt"] = ptb[4 * c:4 * c + 4]
        in_maps.append(m)
    res = run_bass_kernel_spmd(nc, in_maps, core_ids=list(range(NCORES)))
    R = res.results
    y_prompt = np.stack([R[c]["yp"] for c in range(NCORES)]).reshape(8, 2048, 1024)
    y_sample = np.concatenate([R[c]["ys"] for c in range(NCORES)]).reshape(32, 1, 1024)
    k_prompt = np.stack([R[c]["kp"] for c in range(NCORES)]).reshape(1, 8, 2048, 8, 64)
    v_prompt = np.stack([R[c]["vp"] for c in range(NCORES)]).reshape(1, 8, 2048, 8, 64)
    conv_prompt = np.stack([R[c]["cp"] for c in range(NCORES)]).reshape(1, 8, 2, 512)
    k_sample = np.concatenate([R[c]["ks"] for c in range(NCORES)]).reshape(1, 32, 1, 8, 64)
    v_sample = np.concatenate([R[c]["vs"] for c in range(NCORES)]).reshape(1, 32, 1, 8, 64)
    conv_sample = np.concatenate([R[c]["cso"] for c in range(NCORES)]).reshape(1, 32, 2, 512)
    return tuple(np.asarray(a, dtype=np.float32) for a in
                 (y_prompt, y_sample, k_prompt, v_prompt, conv_prompt, k_sample, v_sample, conv_sample))
```
